# Optimizing a Trainium2 kernel written in Bass

```python
import jax, jax.numpy as jnp
from jax import lax
import numpy as np

D_MODEL = 1024
BATCH = 4
SEQ = 8192
DEPTH = 2

PLE_DIM = 256
NORM_EPS = 1e-6
NEG_INF = -1e30

MOBA_HEADS = 8
MOBA_HEAD_DIM = 64
MOBA_BLOCK = 256
MOBA_TOPK = 3
MOBA_Q_CHUNK = 32
POOL_GROUPS = 4
POOL_GROUP_DIM = 128
POOL_WINDOWS = (2, 4, 8, 16)
MLSTM_HEADS = 4
MLSTM_HEAD_DIM = 128
MLSTM_CHUNK = 64
CONV_WIDTH = 4
MLA_HEADS = 4
MLA_Q_RANK = 256
MLA_KV_RANK = 128
MLA_NOPE_DIM = 64
MLA_ROPE_DIM = 32
MLA_V_DIM = 128
ROPE_BASE = 10000.0
ATTN_Q_BLOCK = 128
FFN_DIM = 2816
N_EXPERTS = 8
MOE_TOPK = 2
EXPERT_DIM = 3584

A_WIDTH = MOBA_HEADS * MOBA_HEAD_DIM
B_WIDTH = POOL_GROUPS * POOL_GROUP_DIM
C_WIDTH = MLSTM_HEADS * MLSTM_HEAD_DIM
D_WIDTH = MLA_HEADS * MLA_V_DIM
EVEN_IN = 3 * A_WIDTH + B_WIDTH
ODD_IN = 4 * C_WIDTH + 2 * MLSTM_HEADS + MLA_Q_RANK + MLA_KV_RANK + MLA_ROPE_DIM
EVEN_MIX = A_WIDTH + B_WIDTH
ODD_MIX = C_WIDTH + D_WIDTH

kernel_name = "hybrid_moba_pool_mlstm_mla_moe"


def rmsnorm(x, g):
    xf = x.astype(jnp.float32)
    y = xf * lax.rsqrt(jnp.mean(xf * xf, axis=-1, keepdims=True) + NORM_EPS)
    return (y * g.astype(jnp.float32)).astype(x.dtype)


def swiglu(x, wg, wu, wd):
    return (jax.nn.silu(x @ wg) * (x @ wu)) @ wd


def alibi_slopes(n):
    return jnp.asarray(2.0 ** (-8.0 * np.arange(1, n + 1) / n), dtype=jnp.float32)


def moba_attention(q, k, v):
    B, H, S, dh = q.shape
    n_blk = -(-S // MOBA_BLOCK)
    s_pad = n_blk * MOBA_BLOCK
    pad = ((0, 0), (0, 0), (0, s_pad - S), (0, 0))
    q = jnp.pad(q, pad).astype(jnp.float32)
    k = jnp.pad(k, pad).astype(jnp.float32)
    v = jnp.pad(v, pad).astype(jnp.float32)
    k_blocks = k.reshape(B, H, n_blk, MOBA_BLOCK, dh)
    v_blocks = v.reshape(B, H, n_blk, MOBA_BLOCK, dh)
    k_mean = jnp.mean(k_blocks, axis=3)
    gate = jnp.einsum('bhsd,bhnd->bhsn', q, k_mean)
    q_blk = jnp.arange(s_pad) // MOBA_BLOCK
    past = jnp.arange(n_blk)[None, :] < q_blk[:, None]
    gate = jnp.where(past, gate, NEG_INF)
    n_sel = min(MOBA_TOPK, n_blk)
    _, sel_idx = lax.top_k(gate, n_sel)
    sel_valid = sel_idx < q_blk[:, None]
    slopes = alibi_slopes(H)[None, :, None, None]
    scale = dh ** -0.5
    b_ix = jnp.arange(B)[:, None, None, None]
    h_ix = jnp.arange(H)[None, :, None, None]
    key_off = jnp.arange(MOBA_BLOCK)

    def chunk(c):
        t0 = c * MOBA_Q_CHUNK
        qc = lax.dynamic_slice_in_dim(q, t0, MOBA_Q_CHUNK, axis=2)
        idx = lax.dynamic_slice_in_dim(sel_idx, t0, MOBA_Q_CHUNK, axis=2)
        ok = lax.dynamic_slice_in_dim(sel_valid, t0, MOBA_Q_CHUNK, axis=2)
        own0 = (t0 // MOBA_BLOCK) * MOBA_BLOCK
        k_own = lax.dynamic_slice_in_dim(k, own0, MOBA_BLOCK, axis=2)
        v_own = lax.dynamic_slice_in_dim(v, own0, MOBA_BLOCK, axis=2)
        k_sel = k_blocks[b_ix, h_ix, idx]
        v_sel = v_blocks[b_ix, h_ix, idx]
        t_pos = t0 + jnp.arange(MOBA_Q_CHUNK)
        d_own = t_pos[:, None] - (own0 + key_off)[None, :]
        s_own = jnp.einsum('bhqd,bhkd->bhqk', qc, k_own) * scale - slopes * d_own.astype(jnp.float32)
        s_own = jnp.where(d_own >= 0, s_own, NEG_INF)
        d_sel = t_pos[:, None, None] - (idx[..., None] * MOBA_BLOCK + key_off)
        s_sel = jnp.einsum('bhqd,bhqnkd->bhqnk', qc, k_sel) * scale - slopes[..., None] * d_sel.astype(jnp.float32)
        s_sel = jnp.where(ok[..., None], s_sel, NEG_INF)
        scores = jnp.concatenate([s_sel.reshape(B, H, MOBA_Q_CHUNK, n_sel * MOBA_BLOCK), s_own], axis=-1)
        probs = jax.nn.softmax(scores, axis=-1)
        p_sel = probs[..., :n_sel * MOBA_BLOCK].reshape(B, H, MOBA_Q_CHUNK, n_sel, MOBA_BLOCK)
        p_own = probs[..., n_sel * MOBA_BLOCK:]
        return (jnp.einsum('bhqnk,bhqnkd->bhqd', p_sel, v_sel)
                + jnp.einsum('bhqk,bhkd->bhqd', p_own, v_own))

    out = lax.map(chunk, jnp.arange(s_pad // MOBA_Q_CHUNK))
    out = jnp.moveaxis(out, 0, 2).reshape(B, H, s_pad, dh)
    return out[:, :, :S]


def multiscale_pool(x, w_group, scale):
    B, S, _ = x.shape
    xg = x.reshape(B, S, POOL_GROUPS, POOL_GROUP_DIM).astype(jnp.float32)
    csum = jnp.concatenate([jnp.zeros_like(xg[:, :1]), jnp.cumsum(xg, axis=1)], axis=1)
    t = jnp.arange(S)[:, None]
    win = jnp.asarray(POOL_WINDOWS, dtype=jnp.int32)[None, :]
    lo = jnp.maximum(t + 1 - win, 0)
    count = (t + 1 - lo).astype(jnp.float32)
    lagged = csum[:, lo, jnp.arange(POOL_GROUPS)[None, :]]
    pooled = (csum[:, 1:] - lagged) / count[None, :, :, None]
    mixed = jnp.einsum('bsgc,gcd->bsgd', pooled - xg, w_group.astype(jnp.float32))
    return (mixed.reshape(B, S, B_WIDTH) * scale).astype(x.dtype)


def causal_conv(x, w):
    K, C = w.shape
    return lax.conv_general_dilated(
        x, w[:, None, :].astype(x.dtype), window_strides=(1,), padding=[(K - 1, 0)],
        dimension_numbers=('NWC', 'WIO', 'NWC'), feature_group_count=C)


def mlstm(q, k, v, i_pre, f_pre):
    B, H, S, d = q.shape
    L = MLSTM_CHUNK
    nc = S // L

    def to_chunks(t):
        return jnp.moveaxis(t.reshape((B, H, nc, L) + t.shape[3:]), 2, 0)

    logf = jax.nn.log_sigmoid(f_pre)
    tril = jnp.tril(jnp.ones((L, L), dtype=bool))

    def step(carry, xs):
        C, n, m = carry
        qc, kc, vc, ic, fc = xs
        b = jnp.cumsum(fc, axis=-1)
        intra = jnp.where(tril, b[..., :, None] - b[..., None, :] + ic[..., None, :], NEG_INF)
        m_inter = b + m[..., None]
        m_t = jnp.maximum(m_inter, jnp.max(intra, axis=-1))
        w_inter = jnp.exp(m_inter - m_t)
        a = jnp.exp(intra - m_t[..., None]) * jnp.einsum('bhtd,bhsd->bhts', qc, kc)
        num = (w_inter[..., None] * jnp.einsum('bhvk,bhtk->bhtv', C, qc)
               + jnp.einsum('bhts,bhsv->bhtv', a, vc))
        den = w_inter * jnp.einsum('bhk,bhtk->bht', n, qc) + jnp.sum(a, axis=-1)
        h = num / jnp.maximum(jnp.abs(den), jnp.exp(-m_t))[..., None]
        b_end = b[..., -1]
        g = b_end[..., None] - b + ic
        m_new = jnp.maximum(b_end + m, jnp.max(g, axis=-1))
        decay = jnp.exp(b_end + m - m_new)
        w_s = jnp.exp(g - m_new[..., None])
        C_new = decay[..., None, None] * C + jnp.einsum('bhs,bhsv,bhsk->bhvk', w_s, vc, kc)
        n_new = decay[..., None] * n + jnp.einsum('bhs,bhsk->bhk', w_s, kc)
        return (C_new, n_new, m_new), h

    init = (jnp.zeros((B, H, d, d), jnp.float32), jnp.zeros((B, H, d), jnp.float32),
            jnp.zeros((B, H), jnp.float32))
    _, hs = lax.scan(step, init, (to_chunks(q), to_chunks(k), to_chunks(v),
                                  to_chunks(i_pre), to_chunks(logf)))
    return jnp.moveaxis(hs, 0, 2).reshape(B, H, S, d)


def rope(x, ang):
    x1, x2 = jnp.split(x, 2, axis=-1)
    cos = jnp.cos(ang).astype(x.dtype)
    sin = jnp.sin(ang).astype(x.dtype)
    return jnp.concatenate([x1 * cos - x2 * sin, x1 * sin + x2 * cos], axis=-1)


def causal_attention(q, k, v, scale):
    B, H, S, _ = q.shape
    k_pos = jnp.arange(S)

    def block(i):
        t0 = i * ATTN_Q_BLOCK
        qb = lax.dynamic_slice_in_dim(q, t0, ATTN_Q_BLOCK, axis=2)
        s = jnp.einsum('bhqd,bhkd->bhqk', qb, k).astype(jnp.float32) * scale
        q_pos = t0 + jnp.arange(ATTN_Q_BLOCK)
        s = jnp.where(k_pos[None, :] <= q_pos[:, None], s, NEG_INF)
        p = jax.nn.softmax(s, axis=-1)
        return jnp.einsum('bhqk,bhkd->bhqd', p.astype(v.dtype), v)

    out = lax.map(block, jnp.arange(S // ATTN_Q_BLOCK))
    return jnp.moveaxis(out, 0, 2).reshape(B, H, S, v.shape[-1])


def mla(c_q, c_kv, k_rope_in, q_norm, w_uq, kv_norm, w_ukv):
    B, S, _ = c_q.shape
    q = (rmsnorm(c_q, q_norm) @ w_uq).reshape(B, S, MLA_HEADS, MLA_NOPE_DIM + MLA_ROPE_DIM)
    kv = (rmsnorm(c_kv, kv_norm) @ w_ukv).reshape(B, S, MLA_HEADS, MLA_NOPE_DIM + MLA_V_DIM)
    half = MLA_ROPE_DIM // 2
    inv_freq = ROPE_BASE ** (-jnp.arange(half, dtype=jnp.float32) / half)
    ang = jnp.arange(S, dtype=jnp.float32)[:, None] * inv_freq[None, :]
    q_rot = rope(q[..., MLA_NOPE_DIM:], ang[:, None, :])
    k_rot = rope(k_rope_in, ang)
    q_full = jnp.concatenate([q[..., :MLA_NOPE_DIM], q_rot], axis=-1)
    k_full = jnp.concatenate(
        [kv[..., :MLA_NOPE_DIM], jnp.broadcast_to(k_rot[:, :, None, :], (B, S, MLA_HEADS, MLA_ROPE_DIM))], axis=-1)
    v = kv[..., MLA_NOPE_DIM:]
    out = causal_attention(q_full.transpose(0, 2, 1, 3), k_full.transpose(0, 2, 1, 3),
                           v.transpose(0, 2, 1, 3), (MLA_NOPE_DIM + MLA_ROPE_DIM) ** -0.5)
    return out.transpose(0, 2, 1, 3).reshape(B, S, D_WIDTH)


def moe_swiglu(x, router_w, router_b, wg, wu, wd):
    B, S, D = x.shape
    xt = x.reshape(B * S, D)
    logits = (xt @ router_w).astype(jnp.float32) + router_b.astype(jnp.float32)
    top_val, top_idx = lax.top_k(logits, MOE_TOPK)
    top_w = jax.nn.softmax(top_val, axis=-1)
    gates = jnp.einsum('tk,tke->te', top_w, jax.nn.one_hot(top_idx, N_EXPERTS, dtype=jnp.float32))
    y = jnp.zeros_like(xt)
    for e in range(N_EXPERTS):
        y = y + gates[:, e:e + 1].astype(x.dtype) * swiglu(xt, wg[e], wu[e], wd[e])
    return y.reshape(B, S, D)


def per_layer_embedding(h, p_i, g, w_gate, w_proj):
    gate = jax.nn.sigmoid(rmsnorm(h, g) @ w_gate)
    return h + gate * (p_i @ w_proj)


def even_layer(h, norm_mix, w_in, pool_w, pool_scale, w_out, norm_ffn, wg, wu, wd):
    B, S, _ = h.shape
    u = rmsnorm(h, norm_mix) @ w_in
    qa, ka, va, ub = jnp.split(u, [A_WIDTH, 2 * A_WIDTH, 3 * A_WIDTH], axis=-1)

    def heads(t):
        return t.reshape(B, S, MOBA_HEADS, MOBA_HEAD_DIM).transpose(0, 2, 1, 3)

    a_out = moba_attention(heads(qa), heads(ka), heads(va))
    a_out = a_out.transpose(0, 2, 1, 3).reshape(B, S, A_WIDTH).astype(h.dtype)
    b_out = multiscale_pool(ub, pool_w, pool_scale)
    h = h + jnp.concatenate([a_out, b_out], axis=-1) @ w_out
    return h + swiglu(rmsnorm(h, norm_ffn), wg, wu, wd)


def odd_layer(h, norm_mix, w_in, conv_w, b_i, b_f, head_norm, q_norm, w_uq, kv_norm, w_ukv,
              w_out, norm_ffn, router_w, router_b, wg, wu, wd):
    B, S, _ = h.shape
    u = rmsnorm(h, norm_mix) @ w_in
    cuts = np.cumsum([C_WIDTH, C_WIDTH, C_WIDTH, C_WIDTH, MLSTM_HEADS, MLSTM_HEADS,
                      MLA_Q_RANK, MLA_KV_RANK]).tolist()
    q_raw, k_raw, v_c, o_pre, i_pre, f_pre, c_q, c_kv, k_rope_in = jnp.split(u, cuts, axis=-1)
    qk = jax.nn.silu(causal_conv(jnp.concatenate([q_raw, k_raw], axis=-1), conv_w))
    q_c, k_c = jnp.split(qk, 2, axis=-1)

    def heads(t):
        return t.reshape(B, S, MLSTM_HEADS, MLSTM_HEAD_DIM).transpose(0, 2, 1, 3).astype(jnp.float32)

    i_g = (i_pre + b_i).astype(jnp.float32).transpose(0, 2, 1)
    f_g = (f_pre + b_f).astype(jnp.float32).transpose(0, 2, 1)
    hc = mlstm(heads(q_c), heads(k_c) * (MLSTM_HEAD_DIM ** -0.5), heads(v_c), i_g, f_g)
    hc = rmsnorm(hc.transpose(0, 2, 1, 3), head_norm.reshape(MLSTM_HEADS, MLSTM_HEAD_DIM))
    c_out = hc.reshape(B, S, C_WIDTH).astype(h.dtype) * jax.nn.sigmoid(o_pre)
    d_out = mla(c_q, c_kv, k_rope_in, q_norm, w_uq, kv_norm, w_ukv)
    h = h + jnp.concatenate([c_out, d_out], axis=-1) @ w_out
    return h + moe_swiglu(rmsnorm(h, norm_ffn), router_w, router_b, wg, wu, wd)


def setup_inputs(seed: int = 0) -> dict:
    key = jax.random.key(seed)
    ks = iter(jax.random.split(key, 40))
    ne = (DEPTH + 1) // 2
    no = DEPTH // 2
    f32 = jnp.float32

    def w(shape, fan_in):
        return jax.random.normal(next(ks), shape, f32) * fan_in ** -0.5

    def gain(shape):
        return 1.0 + 0.1 * jax.random.normal(next(ks), shape, f32)

    return {
        "x": jax.random.normal(next(ks), (BATCH, SEQ, D_MODEL), f32),
        "p": jax.random.normal(next(ks), (DEPTH, BATCH, SEQ, PLE_DIM), f32),
        "ev_norm_mix": gain((ne, D_MODEL)),
        "ev_w_in": w((ne, D_MODEL, EVEN_IN), D_MODEL),
        "pool_w": w((ne, POOL_GROUPS, POOL_GROUP_DIM, POOL_GROUP_DIM), POOL_GROUP_DIM),
        "pool_scale": gain((ne, B_WIDTH)),
        "ev_w_out": w((ne, EVEN_MIX, D_MODEL), EVEN_MIX),
        "ev_norm_ffn": gain((ne, D_MODEL)),
        "ffn_w_gate": w((ne, D_MODEL, FFN_DIM), D_MODEL),
        "ffn_w_up": w((ne, D_MODEL, FFN_DIM), D_MODEL),
        "ffn_w_down": w((ne, FFN_DIM, D_MODEL), FFN_DIM),
        "od_norm_mix": gain((no, D_MODEL)),
        "od_w_in": w((no, D_MODEL, ODD_IN), D_MODEL),
        "conv_w": w((no, CONV_WIDTH, 2 * C_WIDTH), CONV_WIDTH),
        "gate_b_i": 0.1 * jax.random.normal(next(ks), (no, MLSTM_HEADS), f32),
        "gate_b_f": 3.0 + 0.5 * jax.random.normal(next(ks), (no, MLSTM_HEADS), f32),
        "mlstm_norm": gain((no, C_WIDTH)),
        "mla_q_norm": gain((no, MLA_Q_RANK)),
        "mla_w_uq": w((no, MLA_Q_RANK, MLA_HEADS * (MLA_NOPE_DIM + MLA_ROPE_DIM)), MLA_Q_RANK),
        "mla_kv_norm": gain((no, MLA_KV_RANK)),
        "mla_w_ukv": w((no, MLA_KV_RANK, MLA_HEADS * (MLA_NOPE_DIM + MLA_V_DIM)), MLA_KV_RANK),
        "od_w_out": w((no, ODD_MIX, D_MODEL), ODD_MIX),
        "od_norm_ffn": gain((no, D_MODEL)),
        "router_w": w((no, D_MODEL, N_EXPERTS), D_MODEL),
        "router_b": 0.01 * jax.random.normal(next(ks), (no, N_EXPERTS), f32),
        "moe_w_gate": w((no, N_EXPERTS, D_MODEL, EXPERT_DIM), D_MODEL),
        "moe_w_up": w((no, N_EXPERTS, D_MODEL, EXPERT_DIM), D_MODEL),
        "moe_w_down": w((no, N_EXPERTS, EXPERT_DIM, D_MODEL), EXPERT_DIM),
        "ple_norm": gain((DEPTH, D_MODEL)),
        "ple_w_gate": w((DEPTH, D_MODEL, D_MODEL), D_MODEL),
        "ple_w_proj": w((DEPTH, PLE_DIM, D_MODEL), PLE_DIM),
        "final_norm": gain((D_MODEL,)),
    }


def reference(x, p, ev_norm_mix, ev_w_in, pool_w, pool_scale, ev_w_out, ev_norm_ffn,
              ffn_w_gate, ffn_w_up, ffn_w_down, od_norm_mix, od_w_in, conv_w, gate_b_i, gate_b_f,
              mlstm_norm, mla_q_norm, mla_w_uq, mla_kv_norm, mla_w_ukv, od_w_out, od_norm_ffn,
              router_w, router_b, moe_w_gate, moe_w_up, moe_w_down, ple_norm, ple_w_gate,
              ple_w_proj, final_norm):
    h = x
    for layer in range(DEPTH):
        j = layer // 2
        if layer % 2 == 0:
            h = even_layer(h, ev_norm_mix[j], ev_w_in[j], pool_w[j], pool_scale[j], ev_w_out[j],
                           ev_norm_ffn[j], ffn_w_gate[j], ffn_w_up[j], ffn_w_down[j])
        else:
            h = odd_layer(h, od_norm_mix[j], od_w_in[j], conv_w[j], gate_b_i[j], gate_b_f[j],
                          mlstm_norm[j], mla_q_norm[j], mla_w_uq[j], mla_kv_norm[j], mla_w_ukv[j],
                          od_w_out[j], od_norm_ffn[j], router_w[j], router_b[j],
                          moe_w_gate[j], moe_w_up[j], moe_w_down[j])
        h = per_layer_embedding(h, p[layer], ple_norm[layer], ple_w_gate[layer], ple_w_proj[layer])
    return rmsnorm(h, final_norm)
```

```python
import os
import numpy as np
from contextlib import ExitStack
import concourse.bass as bass
import concourse.mybir as mybir
from concourse.bass_utils import run_bass_kernel_spmd

F32 = mybir.dt.float32
BF16 = mybir.dt.bfloat16
I32 = mybir.dt.int32
AF = mybir.ActivationFunctionType
ALU = mybir.AluOpType
AX = mybir.AxisListType

D = 1024
NEG = -1.0e30


class Buf:
    __slots__ = ("name", "w", "r")

    def __init__(self, name=""):
        self.name = name
        self.w = None
        self.r = {}


class T:
    def __init__(self, t, buf):
        self.t = t
        self.b = buf

    def __getitem__(self, idx):
        return self.t[idx]


class Prog:
    NDMASEM = 12

    def __init__(self, nc, es):
        self.nc = nc
        self.es = es
        self.eng = {"pe": nc.tensor, "act": nc.scalar, "dve": nc.vector, "pool": nc.gpsimd, "sp": nc.sync}
        self.sems = {}
        self.cnt = {}
        for e in ("pe", "act", "dve", "pool"):
            self.sems[e] = es.enter_context(nc.semaphore("sem_" + e))
            self.cnt[e] = 0
        self.dq = {}
        for q in ("sp", "pool"):
            ss = []
            for i in range(self.NDMASEM):
                k = "dma_%s_%d" % (q, i)
                self.sems[k] = es.enter_context(nc.semaphore(k))
                ss.append(k)
            self.dq[q] = [ss, 0]
        self.waited = {e: {} for e in self.eng}
        self.n_inst = 0
        self.dbufs = {}
        self.scope = None
        self.nuniq = 0
        self.deferred = None

    def sb(self, name, shape, dtype):
        es = self.scope if self.scope is not None else self.es
        self.nuniq += 1
        t = es.enter_context(self.nc.sbuf_tensor("sb%d_%s" % (self.nuniq, name), list(shape), dtype))
        return T(t, Buf(name))

    def begin_phase(self):
        self.scope = ExitStack()

    def end_phase(self):
        self.barrier()
        self.scope.close()
        self.scope = None

    def barrier(self):
        toks = [(e, self.cnt[e]) for e in self.cnt if self.cnt[e] > 0]
        for q in self.dq:
            ss, j = self.dq[q]
            for i, k in enumerate(ss):
                n = (j - i + self.NDMASEM - 1) // self.NDMASEM if j > i else 0
                if n > 0:
                    toks.append((k, 16 * n))
        for e in self.eng:
            for tok in toks:
                self._wait(e, tok)

    def ps(self, name, shape, dtype=F32):
        t = self.es.enter_context(self.nc.psum_tensor(name, list(shape), dtype))
        return T(t, Buf(name))

    def dram(self, name, shape, dtype, kind="Internal"):
        return self.nc.dram_tensor(name, list(shape), dtype, kind=kind).ap()

    def db(self, *key):
        b = self.dbufs.get(key)
        if b is None:
            b = Buf(str(key))
            self.dbufs[key] = b
        return b

    def _wait(self, eng, tok):
        k, v = tok
        if self.waited[eng].get(k, 0) >= v:
            return
        self.eng[eng].wait_ge(self.sems[k], v)
        self.waited[eng][k] = v
        self.n_inst += 1

    def _deps(self, eng, reads, writes):
        toks = {}

        def add(tok):
            if tok is None:
                return
            k, v = tok
            if eng == "pe" and k == "pe":
                return
            if toks.get(k, 0) < v:
                toks[k] = v
        for b in reads:
            add(b.w)
        for b in writes:
            add(b.w)
            for k, v in b.r.items():
                add((k, v))
        for k, v in toks.items():
            self._wait(eng, (k, v))

    @staticmethod
    def _mark(tok, reads, writes):
        k, v = tok
        for b in reads:
            if b.r.get(k, 0) < v:
                b.r[k] = v
        for b in writes:
            b.w = tok
            b.r = {}

    @staticmethod
    def _bufs(lst):
        out = []
        for x in lst:
            if x is None:
                continue
            out.append(x.b if isinstance(x, T) else x)
        return out

    def pump(self, thunks, n):
        while n > 0 and thunks:
            f, a, kw = thunks.pop(0)
            f(*a, **kw)
            n -= 1

    def op(self, eng, fn, reads=(), writes=()):
        if self.deferred is not None:
            self.deferred.append((self.op, (eng, fn, list(reads), list(writes)), {}))
            return None
        reads = self._bufs(reads)
        writes = self._bufs(writes)
        self._deps(eng, reads, writes)
        inst = fn()
        self.cnt[eng] += 1
        inst.then_inc(self.sems[eng], 1)
        self._mark((eng, self.cnt[eng]), reads, writes)
        self.n_inst += 1
        return inst

    def mm(self, fns, reads, writes):
        if self.deferred is not None:
            self.deferred.append((self.mm, (list(fns), list(reads), list(writes)), {}))
            return
        reads = self._bufs(reads)
        writes = self._bufs(writes)
        self._deps("pe", reads, writes)
        inst = None
        for f in fns:
            inst = f()
            self.n_inst += 1
        self.cnt["pe"] += 1
        inst.then_inc(self.sems["pe"], 1)
        self._mark(("pe", self.cnt["pe"]), reads, writes)

    def dma(self, q, out, in_, reads=(), writes=(), **kw):
        if self.deferred is not None:
            self.deferred.append((self.dma, (q, out, in_, list(reads), list(writes)), dict(kw)))
            return
        reads = self._bufs(reads)
        writes = self._bufs(writes)
        ss, j = self.dq[q]
        k = ss[j % self.NDMASEM]
        v = 16 * (j // self.NDMASEM + 1)
        self.dq[q][1] = j + 1
        if v > 16:
            self._wait(q, (k, v - 16))
        self._deps(q, reads, writes)
        self.eng[q].dma_start(out=out, in_=in_, **kw).then_inc(self.sems[k], 16)
        self._mark((k, v), reads, writes)
        self.n_inst += 1

    def idma(self, out, in_, out_off=None, in_off=None, bounds=None, reads=(), writes=()):
        q = "pool"
        reads = self._bufs(reads)
        writes = self._bufs(writes)
        ss, j = self.dq[q]
        k = ss[j % self.NDMASEM]
        v = 16 * (j // self.NDMASEM + 1)
        self.dq[q][1] = j + 1
        if v > 16:
            self._wait(q, (k, v - 16))
        self._deps(q, reads, writes)
        self.eng[q].indirect_dma_start(out=out, out_offset=out_off, in_=in_, in_offset=in_off, bounds_check=None,
                                       oob_is_err=False).then_inc(self.sems[k], 16)
        self._mark((k, v), reads, writes)
        self.n_inst += 1

    def finish(self):
        for q in self.dq:
            ss, j = self.dq[q]
            for i, k in enumerate(ss):
                n = (j - i + self.NDMASEM - 1) // self.NDMASEM if j > i else 0
                if n > 0:
                    self._wait("sp", (k, 16 * n))


class Ring:
    def __init__(self, tiles):
        self.tiles = tiles
        self.i = 0

    def next(self):
        t = self.tiles[self.i % len(self.tiles)]
        self.i += 1
        return t


class Cfg:
    def __init__(self, CTX=8192, OWN=4096, stop_after=None, debug=False):
        self.CTX = CTX
        self.OWN = OWN
        self.NBLK = CTX // 256
        self.NTT = OWN // 128
        self.NST = (2 * OWN) // 512 + 7
        self.NSLOT = self.NST * 512
        self.stop_after = stop_after
        self.debug = debug


def _vec_layout():
    names = [("ev_norm_mix", 8), ("ev_norm_ffn", 8), ("pool_scale", 4), ("od_norm_mix", 8), ("od_norm_ffn", 8),
             ("ple_norm0", 8), ("ple_norm1", 8), ("final_norm", 8), ("mla_q_norm", 2), ("mla_kv_norm", 1),
             ("conv_w", 32)]
    off = {}
    o = 0
    for n, c in names:
        off[n] = o
        o += c
    return off, o


VOFF, NV = _vec_layout()


class Ctx:
    pass


def build(cfg):
    nc = bass.Bass("TRN2", target_bir_lowering=False)
    es = ExitStack()
    P = Prog(nc, es)
    CTX, OWN, NBLK = cfg.CTX, cfg.OWN, cfg.NBLK
    NCH = CTX // 512
    g = Ctx()
    g.P, g.nc, g.cfg = P, nc, cfg

    def ein(name, shape, dt=F32):
        return nc.dram_tensor(name, list(shape), dt, kind="ExternalInput").ap()

    def eout(name, shape, dt=F32):
        return nc.dram_tensor(name, list(shape), dt, kind="ExternalOutput").ap()

    I = Ctx()
    g.I = I
    I.xc = ein("xc", [CTX, D])
    I.vecs = ein("vecs", [128, NV])
    I.ev_w_in = ein("ev_w_in", [D, 2048])
    I.pool_w = ein("pool_w", [4, 128, 128])
    I.invc = ein("invc", [4, CTX])
    I.ev_tab = ein("ev_tab", [128, 8])
    I.rowt = ein("rowt", [128, 8])
    I.pastb2 = ein("pastb2", [NBLK, NBLK])
    I.pastb = ein("pastb", [NBLK, NBLK])
    I.alib = ein("alib", [8, CTX // 128])
    I.ev_w_out = ein("ev_w_out", [D, D])
    I.ffn_w_gate = ein("ffn_w_gate", [1, D, 2816])
    I.ffn_w_up = ein("ffn_w_up", [1, D, 2816])
    I.ffn_w_down = ein("ffn_w_down", [1, 2816, D])
    I.ple_w_gate = ein("ple_w_gate", [2, D, D])
    I.ple_w_proj = ein("ple_w_proj", [2, 256, D])
    I.p0c = ein("p0c", [CTX, 256])
    I.p1c = ein("p1c", [OWN, 256])
    I.w1 = ein("w1", [D, 2632])
    I.wuq = ein("wuq", [256, 8 * 96])
    I.mla_w_ukv = ein("mla_w_ukv", [128, 768])
    I.rope = ein("rope", [32, 2, CTX])
    I.bif = ein("bif", [8])
    I.od_w_out = ein("od_w_out", [D, D])
    I.router_w = ein("router_w", [D, 8])
    I.router_b = ein("router_b", [8])
    I.moe_w = ein("moe_w", [8 * 7 * 128, 12288])
    I.widx_base = ein("widx_base", [128, 7])
    I.tilepos = ein("tilepos", [cfg.NST])
    I.hnorm = ein("hnorm", [512])
    I.l1flags = ein("l1flags", [4])
    g.OUT = eout("out", [OWN, D])
    S = Ctx()
    g.S = S
    dbg = cfg.debug
    kind = "ExternalOutput" if dbg else "Internal"
    S.HT = P.dram("HT", [128, 8, CTX], F32, kind)
    S.QT = P.dram("QT", [128, 4, CTX], BF16, kind)
    S.KT = P.dram("KT", [128, 4, CTX], BF16, kind)
    S.VA = P.dram("VA", [CTX // 128, 128, 8 * 65], BF16, kind)
    S.MIXT = P.dram("MIXT", [128, 8, CTX], BF16, kind)
    S.KM = P.dram("KM", [128, 4, NBLK], F32, kind)
    S.KMAX = P.dram("KMAX", [128, 4], F32, kind)
    S.QKC = P.dram("QKC", [128, 8, CTX], BF16, kind)
    S.VC = P.dram("VC", [CTX // 128, 128, 4 * 129], BF16, kind)
    S.OS = P.dram("OS", [CTX // 128, 128, 512], BF16, kind)
    S.IF = P.dram("IF", [CTX // 128, 128, 8], F32, kind)
    S.QM = P.dram("QM", [128, 4, CTX], BF16, kind)
    S.KMT = P.dram("KMT", [128, 4, CTX], BF16, kind)
    S.VM = P.dram("VM", [CTX // 128, 128, 4 * 129], BF16, kind)
    S.KMAX1 = P.dram("KMAX1", [128, 4], F32, kind)
    S.XNT = P.dram("XNT", [OWN, D], BF16, kind)
    S.XS = P.dram("XS", [cfg.NSLOT, D], BF16, kind)
    S.Y = P.dram("Y", [cfg.NSLOT, D], F32, kind)
    S.XN = P.dram("XN", [128, 8, CTX], BF16, kind)
    S.XNF = P.dram("XNF", [128, 8, CTX], F32, kind)

    C = Ctx()
    g.C = C
    C.vecs = P.sb("vecs", [128, NV], F32)
    P.dma("sp", C.vecs[:, :], I.vecs[:, :], writes=[C.vecs])
    C.ones = P.sb("ones", [128, 128], F32)
    P.op("pool", lambda: nc.gpsimd.memset(C.ones[:, :], 1.0), writes=[C.ones])
    C.onesb = P.sb("onesb", [128, 128], BF16)
    P.op("pool", lambda: nc.gpsimd.memset(C.onesb[:, :], 1.0), writes=[C.onesb])
    C.eps = P.sb("eps", [128, 1], F32)
    P.op("pool", lambda: nc.gpsimd.memset(C.eps[:, :], 1e-6), writes=[C.eps])
    C.ident = P.sb("ident", [128, 128], F32)
    P.op("pool", lambda: nc.gpsimd.memset(C.ident[:, :], 1.0), writes=[C.ident])
    P.op("pool", lambda: nc.gpsimd.affine_select(out=C.ident[:, :], in_=C.ident[:, :], pattern=[[-1, 128]],
                                                 compare_op=ALU.is_equal, fill=0.0, base=0, channel_multiplier=1),
         reads=[C.ident], writes=[C.ident])
    C.blk2 = P.sb("blk2", [128, 128], F32)
    P.op("pool", lambda: nc.gpsimd.memset(C.blk2[:, :], 0.0), writes=[C.blk2])
    P.op("pool", lambda: nc.gpsimd.memset(C.blk2[0:64, 0:64], 1.0), reads=[C.blk2], writes=[C.blk2])
    P.op("pool", lambda: nc.gpsimd.memset(C.blk2[64:128, 64:128], 1.0), reads=[C.blk2], writes=[C.blk2])

    g.psf = Ring([P.ps("psf%d" % i, [128, 512], F32) for i in range(4)])
    g.pacc = Ring([P.ps("pacc%d" % i, [128, 512], F32) for i in range(2)])
    g.psb = Ring([P.ps("psb%d" % i, [128, 1024], BF16) for i in range(2)])
    STOP = int(os.environ.get('KSTOP', '99'))

    P.begin_phase()
    phase_l0_a(g)
    P.end_phase()
    if STOP >= 2:
        P.begin_phase()
        phase_l0_b(g)
        P.end_phase()
    if STOP >= 3:
        P.begin_phase()
        phase_resid_norm(g, I.ev_w_out, VOFF["ev_norm_ffn"], 0, CTX)
        P.end_phase()
        P.begin_phase()
        phase_mlp(g, I.ffn_w_gate, I.ffn_w_up, I.ffn_w_down, 1, 2816, 0, CTX, None)
        P.end_phase()
        P.begin_phase()
        phase_ple(g, 0, I.p0c, VOFF["ple_norm0"], 0, CTX, final=False)
        P.end_phase()
    if STOP >= 4:
        P.begin_phase()
        phase_l1_in(g)
        P.end_phase()
    if STOP >= 6:
        full_psf, full_psb = g.psf, g.psb
        P.begin_phase()
        g.psf, g.psb = Ring(full_psf.tiles[3:4]), Ring(full_psb.tiles[1:2])
        P.deferred = []
        phase_mlstm(g)
        g.side = P.deferred
        P.deferred = None
        g.psf, g.psb = Ring(full_psf.tiles[0:3]), Ring(full_psb.tiles[0:1])
        phase_mla(g)
        P.pump(g.side, len(g.side))
        P.end_phase()
        g.psf, g.psb = full_psf, full_psb
    if STOP >= 7:
        T0 = CTX - OWN
        P.begin_phase()
        phase_resid_norm(g, I.od_w_out, VOFF["od_norm_ffn"], T0, OWN, want_f32=True, mixd=True)
        P.end_phase()
        R = Ctx()
        g.R = R
        NTT, NST = cfg.NTT, cfg.NST
        R.s1i = P.sb("R_s1i", [128, NTT], I32)
        R.s2i = P.sb("R_s2i", [128, NTT], I32)
        R.g1 = P.sb("R_g1", [128, NTT], F32)
        R.g2 = P.sb("R_g2", [128, NTT], F32)
        R.widx = P.sb("R_widx", [128, NST * 7], I32)
        P.begin_phase()
        phase_router(g)
        P.end_phase()
        P.begin_phase()
        phase_scatter(g)
        P.end_phase()
        P.begin_phase()
        phase_experts(g)
        P.end_phase()
        P.begin_phase()
        phase_combine(g)
        P.end_phase()
        P.begin_phase()
        phase_ple(g, 1, I.p1c, VOFF["ple_norm1"], T0, OWN, final=True)
        P.end_phase()

    P.finish()
    es.close()
    return nc


def rmsnorm_fm(g, hT, xn, gcol, n, sqr, rstd, tmp, nk=8, dim=D):
    P, nc, C = g.P, g.nc, g.C
    ps = g.psf.next()
    for k in range(nk):
        sq = sqr.next()
        P.op("act", lambda k=k, sq=sq: nc.scalar.activation(out=sq[:, :n], in_=hT[:, k, :n], func=AF.Square),
             reads=[hT], writes=[sq])
        P.mm([lambda k=k, sq=sq: nc.tensor.matmul(ps[:, :n], lhsT=C.onesb[:, :], rhs=sq[:, :n], start=(k == 0),
                                                  stop=(k == nk - 1))], reads=[C.onesb, sq], writes=[ps])
    P.op("act", lambda: nc.scalar.activation(out=tmp[:, :n], in_=ps[:, :n], func=AF.Sqrt, bias=C.eps[:, 0:1],
                                             scale=1.0 / dim), reads=[ps, C.eps], writes=[tmp])
    P.op("dve", lambda: nc.vector.reciprocal(out=rstd[:, :n], in_=tmp[:, :n]), reads=[tmp], writes=[rstd])
    for k in range(nk):
        P.op("dve", lambda k=k: nc.vector.scalar_tensor_tensor(
            out=xn[:, k, :n], in0=hT[:, k, :n], scalar=C.vecs[:, gcol + k:gcol + k + 1], in1=rstd[:, :n],
            op0=ALU.mult, op1=ALU.mult), reads=[hT, rstd, C.vecs], writes=[xn])


def phase_l0_a(g):
    P, nc, C, I, S, cfg = g.P, g.nc, g.C, g.I, g.S, g.cfg
    CTX, NBLK = cfg.CTX, cfg.NBLK
    NCH = CTX // 512
    win = P.sb("a_win", [128, 8, 2048], BF16)
    P.dma("pool", win[:, :, :], I.ev_w_in.rearrange("(k p) f -> p k f", p=128), writes=[win])
    pw = P.sb("a_pw", [128, 4, 128], BF16)
    P.dma("pool", pw[:, :, :], I.pool_w.rearrange("g c d -> c g d"), writes=[pw])
    evt = P.sb("a_evt", [128, 8], F32)
    P.dma("sp", evt[:, :], I.ev_tab[:, :], writes=[evt])
    xtok = Ring([P.sb("a_xtok%d" % i, [128, 4, D], F32) for i in range(2)])
    hT = Ring([P.sb("a_hT%d" % i, [128, 8, 512], F32) for i in range(2)])
    sq = Ring([P.sb("a_sq%d" % i, [128, 512], BF16) for i in range(2)])
    xnr = Ring([P.sb("a_xn%d" % i, [128, 8, 512], BF16) for i in range(2)])
    rstd = P.sb("a_rstd", [128, 512], F32)
    tmp = P.sb("a_tmp", [128, 512], F32)
    qT = Ring([P.sb("a_qT%d" % i, [128, 4, 512], BF16) for i in range(2)])
    kT = Ring([P.sb("a_kT%d" % i, [128, 4, 512], BF16) for i in range(2)])
    ksq = P.sb("a_ksq", [128, 512], F32)
    km = P.sb("a_km", [128, 4, NBLK], F32)
    kmx = P.sb("a_kmx", [128, 4], F32)
    kmx1 = P.sb("a_kmx1", [128, 1], F32)
    P.op("pool", lambda: nc.gpsimd.memset(kmx[:, :], 0.0), writes=[kmx])
    ub = P.sb("a_ub", [128, 4, 528], F32)
    P.op("pool", lambda: nc.gpsimd.memset(ub[:, :, :], 0.0), writes=[ub])
    s_a = P.sb("a_sa", [128, 528], F32)
    s_b = P.sb("a_sb", [128, 528], F32)
    invc = Ring([P.sb("a_invc%d" % i, [128, 4, 512], F32) for i in range(2)])
    pm = P.sb("a_pm", [128, 512], BF16)
    bout = Ring([P.sb("a_bout%d" % i, [128, 4, 512], BF16) for i in range(2)])
    vaug = Ring([P.sb("a_vaug%d" % i, [128, 8, 65], BF16) for i in range(2)])
    vc = VOFF
    STG = int(os.environ.get('KSTAGE', '99'))

    def load(c):
        t = xtok.next()
        P.dma("sp", t[:, :, :], I.xc[c * 512:(c + 1) * 512, :].rearrange("(j p) d -> p j d", p=128), writes=[t])
        iv = invc.next()
        for gi in range(4):
            P.dma("sp", iv[:, gi, :], I.invc[gi:gi + 1, c * 512:(c + 1) * 512].partition_broadcast(128),
                  writes=[iv])
        return t, iv

    nxt = load(0)
    for c in range(NCH):
        xt, iv = nxt
        if c + 1 < NCH:
            nxt = load(c + 1)
        h = hT.next()
        xn = xnr.next()
        for k in range(8):
            ps = g.psf.next()
            P.mm([lambda j=j, k=k: nc.tensor.transpose(out=ps[:, j * 128:(j + 1) * 128],
                                                       in_=xt[:, j, k * 128:(k + 1) * 128], identity=C.ident[:, :])
                  for j in range(4)], reads=[xt, C.ident], writes=[ps])
            P.op("dve", lambda k=k, ps=ps: nc.vector.tensor_copy(out=h[:, k, :], in_=ps[:, :]), reads=[ps], writes=[h])
        P.dma("sp", S.HT[:, :, c * 512:(c + 1) * 512], h[:, :, :], reads=[h], writes=[P.db("HT", c)])
        if STG < 2:
            continue
        rmsnorm_fm(g, h, xn, vc["ev_norm_mix"], 512, sq, rstd, tmp)
        if STG < 3:
            continue
        q = qT.next()
        for fc in range(4):
            ps = g.psf.next()
            P.mm([lambda k=k, fc=fc: nc.tensor.matmul(ps[:, :], lhsT=win[:, k, fc * 128:(fc + 1) * 128],
                                                      rhs=xn[:, k, :], start=(k == 0), stop=(k == 7))
                  for k in range(8)], reads=[win, xn], writes=[ps])
            P.op("act", lambda fc=fc, ps=ps: nc.scalar.mul(out=q[:, fc, :], in_=ps[:, :], mul=0.125),
                 reads=[ps], writes=[q])
        P.dma("sp", S.QT[:, :, c * 512:(c + 1) * 512], q[:, :, :], reads=[q], writes=[P.db("QT", c)])
        if STG < 4:
            continue
        kk = kT.next()
        for fc in range(4):
            ps = g.psf.next()
            P.mm([lambda k=k, fc=fc: nc.tensor.matmul(ps[:, :], lhsT=win[:, k, 512 + fc * 128:512 + (fc + 1) * 128],
                                                      rhs=xn[:, k, :], start=(k == 0), stop=(k == 7))
                  for k in range(8)], reads=[win, xn], writes=[ps])
            P.op("act", lambda fc=fc, ps=ps: nc.scalar.copy(out=kk[:, fc, :], in_=ps[:, :]), reads=[ps], writes=[kk])
            SUB = int(os.environ.get('KSUB', '99'))
            if SUB < 2:
                continue
            for bb in range(2):
                P.op("dve", lambda fc=fc, ps=ps, bb=bb: nc.vector.reduce_sum(
                    out=km[:, fc, 2 * c + bb:2 * c + bb + 1], in_=kk[:, fc, bb * 256:(bb + 1) * 256], axis=AX.X),
                    reads=[kk], writes=[km])
            if SUB < 3:
                continue
            P.op("act", lambda ps=ps: nc.scalar.activation(out=ksq[:, :], in_=ps[:, :], func=AF.Square),
                 reads=[ps], writes=[ksq])
            if SUB < 4:
                continue
            ps2 = g.psf.next()
            P.mm([lambda: nc.tensor.matmul(ps2[:, :], lhsT=C.blk2[:, :], rhs=ksq[:, :], start=True, stop=True)],
                 reads=[C.blk2, ksq], writes=[ps2])
            if SUB < 5:
                continue
            P.op("dve", lambda ps2=ps2: nc.vector.reduce_max(out=kmx1[:, :], in_=ps2[:, :], axis=AX.X),
                 reads=[ps2], writes=[kmx1])
            if SUB < 6:
                continue
            P.op("dve", lambda fc=fc: nc.vector.tensor_max(out=kmx[:, fc:fc + 1], in0=kmx[:, fc:fc + 1], in1=kmx1[:, :]),
                 reads=[kmx, kmx1], writes=[kmx])
        P.dma("sp", S.KT[:, :, c * 512:(c + 1) * 512], kk[:, :, :], reads=[kk], writes=[P.db("KT", c)])
        if STG < 5:
            continue
        for j in range(4):
            ps = g.psf.next()
            P.mm([lambda k=k, j=j: nc.tensor.matmul(ps[:, :], lhsT=xn[:, k, j * 128:(j + 1) * 128],
                                                    rhs=win[:, k, 1024:1536], start=(k == 0), stop=(k == 7))
                  for k in range(8)], reads=[win, xn], writes=[ps])
            va = vaug.next()
            par = j % 2
            P.op("dve", lambda ps=ps, va=va, par=par: nc.vector.tensor_tensor(
                out=va[:, :, 0:64], in0=ps[:, :].rearrange("p (h d) -> p h d", h=8),
                in1=evt[:, :].unsqueeze(2).broadcast_to([128, 8, 64]), op=ALU.mult),
                reads=[ps, evt], writes=[va])
            P.op("act", lambda va=va, par=par: nc.scalar.copy(out=va[:, :, 64:65], in_=evt[:, :].unsqueeze(2)),
                 reads=[evt, va], writes=[va])
            P.dma("sp", S.VA[c * 4 + j, :, :], va[:, :, :].rearrange("p h d -> p (h d)"), reads=[va],
                  writes=[P.db("VA", c * 4 + j)])
        if STG < 6:
            continue
        bo = bout.next()
        for gi in range(4):
            ps = g.psf.next()
            P.mm([lambda k=k, gi=gi: nc.tensor.matmul(ps[:, :], lhsT=win[:, k, 1536 + gi * 128:1536 + (gi + 1) * 128],
                                                      rhs=xn[:, k, :], start=(k == 0), stop=(k == 7))
                  for k in range(8)], reads=[win, xn], writes=[ps])
            P.op("act", lambda gi=gi, ps=ps: nc.scalar.copy(out=ub[:, gi, 16:528], in_=ps[:, :]), reads=[ps], writes=[ub])
            src = None
            cur, oth = s_a, s_b
            for st in range(gi + 1):
                sh = 1 << st
                if st == 0:
                    P.op("dve", lambda gi=gi, cur=cur, sh=sh: nc.vector.tensor_add(
                        out=cur[:, sh:528], in0=ub[:, gi, sh:528], in1=ub[:, gi, 0:528 - sh]),
                        reads=[ub], writes=[cur])
                else:
                    v = 2 * sh - 1
                    P.op("dve", lambda cur=cur, oth=oth, sh=sh, v=v: nc.vector.tensor_add(
                        out=oth[:, v:528], in0=cur[:, v:528], in1=cur[:, v - sh:528 - sh]),
                        reads=[cur], writes=[oth])
                    cur, oth = oth, cur
            P.op("dve", lambda gi=gi, cur=cur, oth=oth: nc.vector.tensor_mul(
                out=oth[:, 16:528], in0=cur[:, 16:528], in1=iv[:, gi, :]), reads=[cur, iv], writes=[oth])
            P.op("dve", lambda gi=gi, oth=oth: nc.vector.tensor_sub(
                out=pm[:, :], in0=oth[:, 16:528], in1=ub[:, gi, 16:528]), reads=[oth, ub], writes=[pm])
            P.op("pool", lambda gi=gi: nc.gpsimd.tensor_copy(out=ub[:, gi, 0:16], in_=ub[:, gi, 512:528]),
                 reads=[ub], writes=[ub])
            ps3 = g.psf.next()
            P.mm([lambda gi=gi, ps3=ps3: nc.tensor.matmul(ps3[:, :], lhsT=pw[:, gi, :], rhs=pm[:, :], start=True, stop=True)],
                 reads=[pw, pm], writes=[ps3])
            P.op("act", lambda gi=gi, ps3=ps3: nc.scalar.mul(out=bo[:, gi, :], in_=ps3[:, :],
                                                             mul=C.vecs[:, vc["pool_scale"] + gi:vc["pool_scale"] + gi + 1]),
                 reads=[ps3, C.vecs], writes=[bo])
        P.dma("sp", S.MIXT[:, 4:8, c * 512:(c + 1) * 512], bo[:, :, :], reads=[bo], writes=[P.db("MIXT_B", c)])
    P.op("dve", lambda: nc.vector.tensor_scalar_mul(out=km[:, :, :], in0=km[:, :, :], scalar1=1.0 / 256.0),
         reads=[km], writes=[km])
    P.dma("sp", S.KM[:, :, :], km[:, :, :], reads=[km], writes=[P.db("KM")])
    P.dma("sp", S.KMAX[:, :], kmx[:, :], reads=[kmx], writes=[P.db("KMAX")])


def phase_l0_b(g):
    P, nc, C, I, S, cfg = g.P, g.nc, g.C, g.I, g.S, g.cfg
    CTX, NBLK = cfg.CTX, cfg.NBLK
    NT = CTX // 128
    NB8 = max(NBLK, 8)
    KT = P.sb("b_KT", [128, 4, CTX], BF16)
    NCH = CTX // 512
    kbufs = [Buf("KTc%d" % c) for c in range(NCH)]
    for c in range(NCH):
        P.dma("sp", KT[:, :, c * 512:(c + 1) * 512], S.KT[:, :, c * 512:(c + 1) * 512], reads=[P.db("KT", c)],
              writes=[kbufs[c]])
    VA = P.sb("b_VA", [128, NT, 520], BF16)
    vbufs = [Buf("VAt%d" % t) for t in range(NT)]
    for t in range(NT):
        P.dma("sp", VA[:, t, :], S.VA[t, :, :], reads=[P.db("VA", t)], writes=[vbufs[t]])
    kmf = P.sb("b_kmf", [128, 4, NBLK], F32)
    P.dma("sp", kmf[:, :, :], S.KM[:, :, :], reads=[P.db("KM")], writes=[kmf])
    kmb = P.sb("b_kmb", [128, 4, 2, NBLK], BF16)
    P.op("dve", lambda: nc.vector.memset(kmb[:, :, :, :], 0.0), writes=[kmb])
    P.op("dve", lambda: nc.vector.tensor_copy(out=kmb[0:64, :, 0, :], in_=kmf[0:64, :, :]), reads=[kmf, kmb], writes=[kmb])
    P.op("dve", lambda: nc.vector.tensor_copy(out=kmb[64:128, :, 1, :], in_=kmf[64:128, :, :]), reads=[kmf, kmb], writes=[kmb])
    kmx = P.sb("b_kmx", [128, 4], F32)
    P.dma("sp", kmx[:, :], S.KMAX[:, :], reads=[P.db("KMAX")], writes=[kmx])
    pastb = P.sb("b_pastb", [128, NBLK, NBLK], F32)
    P.dma("sp", pastb[:, :, :].rearrange("p a b -> p (a b)"),
          I.pastb.rearrange("a b -> (a b)").partition_broadcast(128),
          writes=[pastb])
    alib = P.sb("b_alib", [128, 8, NT], F32)
    P.dma("sp", alib[:, :, :].rearrange("p a b -> p (a b)"),
          I.alib.rearrange("a b -> (a b)").partition_broadcast(128),
          writes=[alib])
    pastb2 = P.sb("b_pastb2", [128, NBLK, NBLK], F32)
    P.dma("sp", pastb2[:, :, :].rearrange("p a b -> p (a b)"),
          I.pastb2.rearrange("a b -> (a b)").partition_broadcast(128), writes=[pastb2])
    rowt = P.sb("b_rowt", [128, 8], F32)
    P.dma("sp", rowt[:, :], I.rowt[:, :], writes=[rowt])
    rowc = P.sb("b_rowc", [128, 8], F32)
    e0 = P.sb("b_e0", [128, 2, 128], F32)
    P.op("pool", lambda: nc.gpsimd.memset(e0[:, :, :], 0.0), writes=[e0])
    P.op("pool", lambda: nc.gpsimd.memset(e0[0:1, 0, :], 1.0), reads=[e0], writes=[e0])
    P.op("pool", lambda: nc.gpsimd.memset(e0[64:65, 1, :], 1.0), reads=[e0], writes=[e0])
    hsel = P.sb("b_hsel", [128, 2], BF16)
    P.op("pool", lambda: nc.gpsimd.memset(hsel[:, :], 0.0), writes=[hsel])
    P.op("pool", lambda: nc.gpsimd.memset(hsel[0:64, 0:1], 1.0), reads=[hsel], writes=[hsel])
    P.op("pool", lambda: nc.gpsimd.memset(hsel[64:128, 1:2], 1.0), reads=[hsel], writes=[hsel])
    tri = P.sb("b_tri", [128, 128], BF16)
    P.op("pool", lambda: nc.gpsimd.memset(tri[:, :], 1.0), writes=[tri])
    P.op("pool", lambda: nc.gpsimd.affine_select(out=tri[:, :], in_=tri[:, :], pattern=[[-1, 128]],
                                                 compare_op=ALU.is_ge, fill=0.0, base=0, channel_multiplier=1),
         reads=[tri], writes=[tri])
    identb = P.sb("b_identb", [128, 128], BF16)
    P.op("dve", lambda: nc.vector.tensor_copy(out=identb[:, :], in_=C.ident[:, :]), reads=[C.ident], writes=[identb])
    kx = P.sb("b_kx", [128, 8], F32)
    ps = g.psf.next()
    for par in range(2):
        P.mm([lambda par=par: nc.tensor.matmul(ps[:, par * 4:par * 4 + 4], lhsT=e0[:, par, :], rhs=kmx[:, :],
                                               start=True, stop=True)], reads=[e0, kmx], writes=[ps])
    P.op("dve", lambda: nc.vector.tensor_copy(out=kx[:, :].rearrange("p (hp par) -> p par hp", par=2),
                                              in_=ps[:, 0:8].rearrange("p (par hp) -> p par hp", par=2)),
         reads=[ps], writes=[kx])

    qT = Ring([P.sb("b_qT%d" % i, [128, 4, 128], BF16) for i in range(2)])
    qsq = P.sb("b_qsq", [128, 4, 128], BF16)
    gs = P.sb("b_gs", [128, 8, NB8], F32)
    P.op("pool", lambda: nc.gpsimd.memset(gs[:, :, :], -3.0e30), writes=[gs])
    top8 = P.sb("b_top8", [128, 8, 8], F32)
    sel = P.sb("b_sel", [128, 8, NBLK], F32)
    bq = Ring([P.sb("b_bq%d" % i, [128, 8, 2 * NBLK], F32) for i in range(2)])
    fq = Ring([P.sb("b_fq%d" % i, [128, 8, 2 * NBLK], F32) for i in range(2)])
    mb = Ring([P.sb("b_mb%d" % i, [128, 8], F32) for i in range(2)])
    nmb = Ring([P.sb("b_nmb%d" % i, [128, 8], F32) for i in range(2)])
    qn2 = P.sb("b_qn2", [128, 8], F32)
    Pp = Ring([P.sb("b_Pp%d" % i, [128, 1024], BF16) for i in range(6)])
    PT = Ring([P.sb("b_PT%d" % i, [128, 8, 128], BF16) for i in range(4)])
    atok = Ring([P.sb("b_atok%d" % i, [128, 512], BF16) for i in range(2)])
    aT = Ring([P.sb("b_aT%d" % i, [128, 4, 128], BF16) for i in range(2)])
    rden = P.sb("b_rden", [128, 1], F32)

    def loadq(i):
        t = qT.next()
        P.dma("sp", t[:, :, :], S.QT[:, :, i * 128:(i + 1) * 128], reads=[P.db("QT", i // 4)], writes=[t])
        return t

    def tile(i, q):
        ob, par = i // 2, i % 2
        P.op("act", lambda q=q: nc.scalar.activation(out=qsq[:, :, :], in_=q[:, :, :], func=AF.Square),
             reads=[q], writes=[qsq])
        ps = g.psf.next()
        for hp in range(4):
            P.mm([lambda hp=hp: nc.tensor.matmul(ps[:, 2 * hp:2 * hp + 2], lhsT=qsq[:, hp, :], rhs=hsel[:, :],
                                                 start=True, stop=True)], reads=[qsq, hsel], writes=[ps])
        m_ = mb.next()
        P.op("dve", lambda ps=ps: nc.vector.tensor_mul(out=qn2[:, :], in0=ps[:, 0:8], in1=kx[:, :]),
             reads=[ps, kx], writes=[qn2])
        P.op("act", lambda m_=m_: nc.scalar.activation(out=m_[:, :], in_=qn2[:, :], func=AF.Sqrt),
             reads=[qn2], writes=[m_])
        b_ = bq.next()
        if ob > 0:
            psg = g.psf.next()
            for hp in range(4):
                P.mm([lambda hp=hp: nc.tensor.matmul(
                    psg[:, hp * 2 * NBLK:(hp + 1) * 2 * NBLK], lhsT=q[:, hp, :],
                    rhs=kmb[:, hp, :, :].rearrange("p a b -> p (a b)"), start=True, stop=True)],
                    reads=[q, kmb], writes=[psg])
            P.op("dve", lambda psg=psg, ob=ob: nc.vector.tensor_tensor(
                out=gs[:, :, 0:NBLK], in0=psg[:, 0:8 * NBLK].rearrange("p (h n) -> p h n", h=8),
                in1=pastb[:, ob, :].unsqueeze(1).broadcast_to([128, 8, NBLK]), op=ALU.add),
                reads=[psg, pastb], writes=[gs])
            for h in range(8):
                P.op("dve", lambda h=h: nc.vector.max(out=top8[:, h, :], in_=gs[:, h, :]), reads=[gs], writes=[top8])
            P.op("dve", lambda: nc.vector.tensor_tensor(
                out=sel[:, :, :], in0=gs[:, :, 0:NBLK], in1=top8[:, :, 2:3].broadcast_to([128, 8, NBLK]), op=ALU.is_ge),
                reads=[gs, top8], writes=[sel])
            P.op("dve", lambda: nc.vector.tensor_scalar(out=sel[:, :, :], in0=sel[:, :, :], scalar1=-1.0, scalar2=1.0e30,
                                                        op0=ALU.add, op1=ALU.mult), reads=[sel], writes=[sel])
            P.op("dve", lambda ob=ob: nc.vector.memset(sel[:, :, ob:ob + 1], 0.0), reads=[sel], writes=[sel])
        else:
            P.op("dve", lambda: nc.vector.memset(sel[:, :, :], 0.0), reads=[sel], writes=[sel])
        P.op("dve", lambda ob=ob: nc.vector.tensor_tensor(
            out=sel[:, :, :], in0=sel[:, :, :], in1=pastb2[:, ob, :].unsqueeze(1).broadcast_to([128, 8, NBLK]),
            op=ALU.add), reads=[sel, pastb2], writes=[sel])
        nm_ = nmb.next()
        P.op("dve", lambda m_=m_, nm_=nm_: nc.vector.tensor_scalar_mul(out=nm_[:, :], in0=m_[:, :], scalar1=-1.0),
             reads=[m_], writes=[nm_])
        P.op("dve", lambda i=i: nc.vector.tensor_sub(out=rowc[:, :], in0=rowt[:, :], in1=alib[:, :, i]),
             reads=[rowt, alib], writes=[rowc])
        P.op("dve", lambda b_=b_: nc.vector.tensor_tensor(
            out=b_[:, :, :].rearrange("p h (n two) -> p h n two", two=2),
            in0=alib[:, :, :].rearrange("p h (n two) -> p h n two", two=2),
            in1=sel[:, :, :].unsqueeze(3).broadcast_to([128, 8, NBLK, 2]), op=ALU.add),
            reads=[sel, alib], writes=[b_])
        P.op("dve", lambda b_=b_: nc.vector.tensor_tensor(
            out=b_[:, :, :], in0=b_[:, :, :], in1=rowc[:, :].unsqueeze(2).broadcast_to([128, 8, 2 * NBLK]), op=ALU.add),
            reads=[b_, rowc], writes=[b_])
        nkt = i + 1
        f_ = fq.next()
        P.op("act", lambda b_=b_, f_=f_, nkt=nkt: nc.scalar.activation(out=f_[:, :, 0:nkt], in_=b_[:, :, 0:nkt], func=AF.Exp),
             reads=[b_], writes=[f_])
        ngr = (nkt + 7) // 8
        items = [(h, gi) for h in range(8) for gi in range(ngr)]
        at = atok.next()
        st = {}

        def stage_s(h, gi):
            hp, p2 = h // 2, h % 2
            k0 = gi * 8
            k1 = min(nkt, k0 + 8)
            pp = Pp.next()
            st[(h, gi)] = [pp, None]
            for half in range(2):
                a0 = k0 + half * 4
                a1 = min(k1, a0 + 4)
                if a1 <= a0:
                    continue
                w = (a1 - a0) * 128
                ps = g.psf.next()
                kb = [kbufs[c] for c in range(a0 // 4, (a1 - 1) // 4 + 1)]
                P.mm([lambda ps=ps, a0=a0, w=w, hp=hp, p2=p2: nc.tensor.matmul(
                    ps[:, 0:w], lhsT=q[64 * p2:64 * p2 + 64, hp, :],
                    rhs=KT[64 * p2:64 * p2 + 64, hp, a0 * 128:a0 * 128 + w], start=True, stop=True)],
                    reads=[q] + kb, writes=[ps])
                o0 = (a0 - k0) * 128
                P.op("act", lambda ps=ps, pp=pp, o0=o0, w=w: nc.scalar.activation(
                    out=pp[:, o0:o0 + w], in_=ps[:, 0:w], func=AF.Exp, bias=nm_[:, h:h + 1], scale=1.0),
                    reads=[ps, nm_], writes=[pp])
            nk = k1 - k0
            P.op("dve", lambda pp=pp, nk=nk, k0=k0: nc.vector.tensor_tensor(
                out=pp[:, 0:nk * 128].rearrange("p (a b) -> p a b", b=128),
                in0=pp[:, 0:nk * 128].rearrange("p (a b) -> p a b", b=128),
                in1=f_[:, h, k0:k0 + nk].unsqueeze(2).broadcast_to([128, nk, 128]), op=ALU.mult),
                reads=[pp, f_], writes=[pp])
            if k1 == nkt:
                o0 = (nkt - 1 - k0) * 128
                P.op("pool", lambda pp=pp, o0=o0: nc.gpsimd.tensor_mul(out=pp[:, o0:o0 + 128], in0=pp[:, o0:o0 + 128],
                                                                      in1=tri[:, :]), reads=[pp, tri], writes=[pp])

        def stage_t(h, gi):
            k0 = gi * 8
            k1 = min(nkt, k0 + 8)
            pp = st[(h, gi)][0]
            pb = g.psb.next()
            P.mm([lambda j=j, pb=pb, pp=pp: nc.tensor.transpose(out=pb[:, j * 128:(j + 1) * 128],
                                                                in_=pp[:, j * 128:(j + 1) * 128], identity=identb[:, :])
                  for j in range(k1 - k0)], reads=[pp, identb], writes=[pb])
            pt = PT.next()
            st[(h, gi)][1] = pt
            w = (k1 - k0) * 128
            if (h + gi) % 2 == 0:
                P.op("dve", lambda pb=pb, pt=pt, w=w: nc.vector.tensor_copy(
                    out=pt[:, :, :].rearrange("p a b -> p (a b)")[:, 0:w], in_=pb[:, 0:w]), reads=[pb], writes=[pt])
            else:
                P.op("act", lambda pb=pb, pt=pt, w=w: nc.scalar.copy(
                    out=pt[:, :, :].rearrange("p a b -> p (a b)")[:, 0:w], in_=pb[:, 0:w]), reads=[pb], writes=[pt])

        acc = {}

        def stage_v(h, gi):
            k0 = gi * 8
            k1 = min(nkt, k0 + 8)
            pt = st[(h, gi)][1]
            if gi == 0:
                acc[h] = g.pacc.next()
            pa = acc[h]
            vb = [vbufs[t] for t in range(k0, k1)]
            fns = [lambda j=j, pa=pa, pt=pt, k0=k0, h=h: nc.tensor.matmul(
                pa[:, 0:65], lhsT=pt[:, j, :], rhs=VA[:, k0 + j, h * 65:(h + 1) * 65],
                start=(k0 + j == 0), stop=(k0 + j == nkt - 1)) for j in range(k1 - k0)]
            P.mm(fns, reads=[pt] + vb, writes=[pa])
            if k1 == nkt:
                P.op("dve", lambda pa=pa: nc.vector.reciprocal(out=rden[:, :], in_=pa[:, 64:65]), reads=[pa], writes=[rden])
                P.op("act", lambda pa=pa, h=h: nc.scalar.mul(out=at[:, h * 64:(h + 1) * 64], in_=pa[:, 0:64], mul=rden[:, 0:1]),
                     reads=[pa, rden], writes=[at])
            del st[(h, gi)]

        def epilogue():
            pb = g.psb.next()
            P.mm([lambda j=j, pb=pb: nc.tensor.transpose(out=pb[:, j * 128:(j + 1) * 128], in_=at[:, j * 128:(j + 1) * 128],
                                                         identity=identb[:, :]) for j in range(4)],
                 reads=[at, identb], writes=[pb])
            a_ = aT.next()
            P.op("dve", lambda pb=pb, a_=a_: nc.vector.tensor_copy(out=a_[:, :, :].rearrange("p a b -> p (a b)"), in_=pb[:, 0:512]),
                 reads=[pb], writes=[a_])
            P.dma("sp", S.MIXT[:, 0:4, i * 128:(i + 1) * 128], a_[:, :, :], reads=[a_], writes=[P.db("MIXT_A", i)])

        return items, stage_s, stage_t, stage_v, epilogue

    LEAD = 5
    sched = []
    n_items = [8 * ((i + 1 + 7) // 8) for i in range(NT)]
    first = []
    for i in range(NT):
        first.append(len(sched))
        sched += [(i, j) for j in range(n_items[i])]
    tiles = {0: tile(0, loadq(0))}
    nxt_tile = 1
    N = len(sched)
    for z in range(N + 3):
        if z < N:
            ti, j = sched[z]
            tiles[ti][1](*tiles[ti][0][j])
        if 0 <= z - 2 < N:
            ti, j = sched[z - 2]
            tiles[ti][2](*tiles[ti][0][j])
        if 0 <= z - 3 < N:
            ti, j = sched[z - 3]
            tiles[ti][3](*tiles[ti][0][j])
            if j == n_items[ti] - 1:
                tiles[ti][4]()
                del tiles[ti]
        if nxt_tile < NT and z + 1 >= first[nxt_tile] - LEAD:
            tiles[nxt_tile] = tile(nxt_tile, loadq(nxt_tile))
            nxt_tile += 1


def phase_resid_norm(g, w_out, gcol, t0, n_tok, want_f32=False, mixd=False):
    P, nc, C, I, S, cfg = g.P, g.nc, g.C, g.I, g.S, g.cfg
    wo = P.sb("c_wo", [128, 8, D], BF16)
    P.dma("pool", wo[:, :, :], w_out.rearrange("(k p) f -> p k f", p=128), writes=[wo])
    hT = Ring([P.sb("c_hT%d" % i, [128, 8, 512], F32) for i in range(2)])
    mx = Ring([P.sb("c_mx%d" % i, [128, 8, 512], BF16) for i in range(2)])
    xn = Ring([P.sb("c_xn%d" % i, [128, 8, 512], BF16) for i in range(2)])
    xnf = Ring([P.sb("c_xnf%d" % i, [128, 8, 512], F32) for i in range(2)]) if want_f32 else None
    sq = Ring([P.sb("c_sq%d" % i, [128, 512], BF16) for i in range(2)])
    rstd = P.sb("c_rstd", [128, 512], F32)
    tmp = P.sb("c_tmp", [128, 512], F32)
    nch = n_tok // 512
    if want_f32:
        identb = P.sb("c_identb", [128, 128], BF16)
        P.op("dve", lambda: nc.vector.tensor_copy(out=identb[:, :], in_=C.ident[:, :]), reads=[C.ident], writes=[identb])
        xtm = Ring([P.sb("c_xtm%d" % i, [128, D], BF16) for i in range(3)])

    def load(c):
        a = t0 + c * 512
        h = hT.next()
        P.dma("sp", h[:, :, :], S.HT[:, :, a:a + 512], reads=[P.db("HT", a // 512)], writes=[h])
        m = mx.next()
        rd = [P.db("MIXT_B", a // 512)] + [P.db("MIXT_A", a // 128 + j) for j in range(4)]
        if mixd:
            rd += [P.db("MIXT_D", a // 128 + j) for j in range(4)]
        P.dma("sp", m[:, :, :], S.MIXT[:, :, a:a + 512], reads=rd, writes=[m])
        return h, m

    nxt = load(0)
    for c in range(nch):
        h, m = nxt
        if c + 1 < nch:
            nxt = load(c + 1)
        a = t0 + c * 512
        for d in range(8):
            ps = g.psf.next()
            P.mm([lambda k=k, d=d, ps=ps: nc.tensor.matmul(ps[:, :], lhsT=wo[:, k, d * 128:(d + 1) * 128], rhs=m[:, k, :],
                                                          start=(k == 0), stop=(k == 7)) for k in range(8)],
                 reads=[wo, m], writes=[ps])
            P.op("dve", lambda d=d, ps=ps: nc.vector.tensor_add(out=h[:, d, :], in0=ps[:, :], in1=h[:, d, :]),
                 reads=[ps, h], writes=[h])
        P.dma("sp", S.HT[:, :, a:a + 512], h[:, :, :], reads=[h], writes=[P.db("HT", a // 512)])
        x_ = xn.next()
        if want_f32:
            xf = xnf.next()
            rmsnorm_fm(g, h, xf, gcol, 512, sq, rstd, tmp)
            P.op("act", lambda xf=xf, x_=x_: nc.scalar.copy(out=x_[:, :, :], in_=xf[:, :, :]), reads=[xf], writes=[x_])
            P.dma("sp", S.XNF[:, :, a:a + 512], xf[:, :, :], reads=[xf], writes=[P.db("XNF", a // 512)])
            for jj in range(4):
                pb = g.psb.next()
                P.mm([lambda k=k, jj=jj, pb=pb, x_=x_: nc.tensor.transpose(
                    out=pb[:, k * 128:(k + 1) * 128], in_=x_[:, k, jj * 128:(jj + 1) * 128], identity=identb[:, :])
                    for k in range(8)], reads=[x_, identb], writes=[pb])
                xt_ = xtm.next()
                P.op("act", lambda pb=pb, xt_=xt_: nc.scalar.copy(out=xt_[:, :], in_=pb[:, 0:1024]), reads=[pb], writes=[xt_])
                r0 = a - t0 + jj * 128
                P.dma("sp", S.XNT[r0:r0 + 128, :], xt_[:, :], reads=[xt_], writes=[P.db("XNT", r0 // 128)])
        else:
            rmsnorm_fm(g, h, x_, gcol, 512, sq, rstd, tmp)
        P.dma("sp", S.XN[:, :, a:a + 512], x_[:, :, :], reads=[x_], writes=[P.db("XN", a // 512)])


def phase_mlp(g, wg_all, wu_all, wd_all, n_exp, F, t0, n_tok, gates):
    P, nc, C, I, S, cfg = g.P, g.nc, g.C, g.I, g.S, g.cfg
    TS = min(2048, n_tok)
    NTC = TS // 512
    yacc = P.sb("m_yacc", [128, 8, TS], F32)
    xn = P.sb("m_xn", [128, 8, TS], BF16)
    wgr = Ring([P.sb("m_wg%d" % i, [128, 8, 512], BF16) for i in range(2)])
    wur = Ring([P.sb("m_wu%d" % i, [128, 8, 512], BF16) for i in range(2)])
    wdr = Ring([P.sb("m_wd%d" % i, [128, 4, D], BF16) for i in range(2)])
    t1r = Ring([P.sb("m_t1%d" % i, [128, 512], BF16) for i in range(3)])
    actr = Ring([P.sb("m_act%d" % i, [128, 4, 512], BF16) for i in range(2)])
    ger = Ring([P.sb("m_ge%d" % i, [128, TS], BF16) for i in range(2)]) if gates is not None else None
    fbs = []
    f = 0
    while f < F:
        fbs.append((f, min(512, F - f)))
        f += 512
    work = [(e, f0, fw) for e in range(n_exp) for (f0, fw) in fbs]

    def loadw(e, f0, fw):
        wg, wu, wd = wgr.next(), wur.next(), wdr.next()
        P.dma("pool", wg[:, :, 0:fw], wg_all[e, :, f0:f0 + fw].rearrange("(k p) f -> p k f", p=128), writes=[wg])
        P.dma("pool", wu[:, :, 0:fw], wu_all[e, :, f0:f0 + fw].rearrange("(k p) f -> p k f", p=128), writes=[wu])
        P.dma("pool", wd[:, 0:fw // 128, :], wd_all[e, f0:f0 + fw, :].rearrange("(j p) d -> p j d", p=128), writes=[wd])
        return wg, wu, wd

    for sc in range(n_tok // TS):
        a = t0 + sc * TS
        for c in range(NTC):
            P.dma("sp", yacc[:, :, c * 512:(c + 1) * 512], S.HT[:, :, a + c * 512:a + (c + 1) * 512],
                  reads=[P.db("HT", (a + c * 512) // 512)], writes=[yacc])
            P.dma("sp", xn[:, :, c * 512:(c + 1) * 512], S.XN[:, :, a + c * 512:a + (c + 1) * 512],
                  reads=[P.db("XN", (a + c * 512) // 512)], writes=[xn])
        nw = loadw(*work[0])
        ge = None
        for wi, (e, f0, fw) in enumerate(work):
            wg, wu, wd = nw
            if wi + 1 < len(work):
                nw = loadw(*work[wi + 1])
            nj = fw // 128
            if gates is not None and (wi == 0 or work[wi - 1][0] != e):
                ge = ger.next()
                o = a - t0
                P.dma("sp", ge[:, :], gates[e, :, o:o + TS], reads=[P.db("GATES")], writes=[ge])

            def up_stage(tc):
                act = actr.next()
                for j in range(nj):
                    psg = g.psf.next()
                    P.mm([lambda k=k, j=j, psg=psg: nc.tensor.matmul(
                        psg[:, :], lhsT=wg[:, k, j * 128:(j + 1) * 128], rhs=xn[:, k, tc * 512:(tc + 1) * 512],
                        start=(k == 0), stop=(k == 7)) for k in range(8)], reads=[wg, xn], writes=[psg])
                    psu = g.psf.next()
                    P.mm([lambda k=k, j=j, psu=psu: nc.tensor.matmul(
                        psu[:, :], lhsT=wu[:, k, j * 128:(j + 1) * 128], rhs=xn[:, k, tc * 512:(tc + 1) * 512],
                        start=(k == 0), stop=(k == 7)) for k in range(8)], reads=[wu, xn], writes=[psu])
                    t1 = t1r.next()
                    P.op("act", lambda psg=psg, t1=t1: nc.scalar.activation(out=t1[:, :], in_=psg[:, :], func=AF.Silu),
                         reads=[psg], writes=[t1])
                    P.op("dve", lambda psu=psu, t1=t1, act=act, j=j: nc.vector.tensor_mul(
                        out=act[:, j, :], in0=psu[:, :], in1=t1[:, :]), reads=[psu, t1], writes=[act])
                    if ge is not None:
                        P.op("pool", lambda act=act, j=j, ge=ge: nc.gpsimd.tensor_mul(
                            out=act[:, j, :], in0=act[:, j, :], in1=ge[:, tc * 512:(tc + 1) * 512]),
                            reads=[act, ge], writes=[act])
                return act

            def down_stage(tc, act):
                for d in range(8):
                    pd = g.pacc.next()
                    P.mm([lambda j=j, d=d, pd=pd: nc.tensor.matmul(
                        pd[:, :], lhsT=wd[:, j, d * 128:(d + 1) * 128], rhs=act[:, j, :],
                        start=(j == 0), stop=(j == nj - 1)) for j in range(nj)], reads=[wd, act], writes=[pd])
                    P.op("dve", lambda d=d, pd=pd: nc.vector.tensor_add(
                        out=yacc[:, d, tc * 512:(tc + 1) * 512], in0=pd[:, :], in1=yacc[:, d, tc * 512:(tc + 1) * 512]),
                        reads=[pd, yacc], writes=[yacc])

            prev = None
            for tc in range(NTC):
                act = up_stage(tc)
                if prev is not None:
                    down_stage(*prev)
                prev = (tc, act)
            down_stage(*prev)
        for c in range(NTC):
            P.dma("sp", S.HT[:, :, a + c * 512:a + (c + 1) * 512], yacc[:, :, c * 512:(c + 1) * 512],
                  reads=[yacc], writes=[P.db("HT", (a + c * 512) // 512)])


def phase_ple(g, layer, p_in, gcol, t0, n_tok, final):
    P, nc, C, I, S, cfg = g.P, g.nc, g.C, g.I, g.S, g.cfg
    wgt = P.sb("p_wg", [128, 8, D], BF16)
    P.dma("pool", wgt[:, :, :], I.ple_w_gate[layer].rearrange("(k p) f -> p k f", p=128), writes=[wgt])
    wpj = P.sb("p_wp", [128, 2, D], BF16)
    P.dma("pool", wpj[:, :, :], I.ple_w_proj[layer].rearrange("(k p) f -> p k f", p=128), writes=[wpj])
    hT = Ring([P.sb("p_hT%d" % i, [128, 8, 512], F32) for i in range(2)])
    pt = Ring([P.sb("p_pt%d" % i, [128, 4, 256], F32) for i in range(2)])
    pT = P.sb("p_pT", [128, 2, 512], BF16)
    xnr = Ring([P.sb("p_xn%d" % i, [128, 8, 512], BF16) for i in range(2)])
    sq = Ring([P.sb("p_sq%d" % i, [128, 512], BF16) for i in range(2)])
    rstd = P.sb("p_rstd", [128, 512], F32)
    tmp = P.sb("p_tmp", [128, 512], F32)
    sg = Ring([P.sb("p_sg%d" % i, [128, 512], F32) for i in range(2)])
    if final:
        xo = P.sb("p_xo", [128, 8, 512], F32)
        ot = Ring([P.sb("p_ot%d" % i, [128, D], F32) for i in range(2)])
    nch = n_tok // 512

    def load(c):
        a = t0 + c * 512
        h = hT.next()
        P.dma("sp", h[:, :, :], S.HT[:, :, a:a + 512], reads=[P.db("HT", a // 512)], writes=[h])
        p_ = pt.next()
        P.dma("sp", p_[:, :, :], p_in[c * 512:(c + 1) * 512, :].rearrange("(j p) d -> p j d", p=128), writes=[p_])
        return h, p_

    nxt = load(0)
    for c in range(nch):
        h, p_ = nxt
        if c + 1 < nch:
            nxt = load(c + 1)
        a = t0 + c * 512
        for k in range(2):
            ps = g.psf.next()
            P.mm([lambda j=j, k=k, ps=ps: nc.tensor.transpose(out=ps[:, j * 128:(j + 1) * 128],
                                                             in_=p_[:, j, k * 128:(k + 1) * 128], identity=C.ident[:, :])
                  for j in range(4)], reads=[p_, C.ident], writes=[ps])
            P.op("act", lambda k=k, ps=ps: nc.scalar.copy(out=pT[:, k, :], in_=ps[:, :]), reads=[ps], writes=[pT])
        xn = xnr.next()
        rmsnorm_fm(g, h, xn, gcol, 512, sq, rstd, tmp)
        for d in range(8):
            ps = g.psf.next()
            P.mm([lambda k=k, d=d, ps=ps: nc.tensor.matmul(ps[:, :], lhsT=wgt[:, k, d * 128:(d + 1) * 128], rhs=xn[:, k, :],
                                                          start=(k == 0), stop=(k == 7)) for k in range(8)],
                 reads=[wgt, xn], writes=[ps])
            s_ = sg.next()
            P.op("act", lambda ps=ps, s_=s_: nc.scalar.activation(out=s_[:, :], in_=ps[:, :], func=AF.Sigmoid),
                 reads=[ps], writes=[s_])
            ps2 = g.pacc.next()
            P.mm([lambda k=k, d=d, ps2=ps2: nc.tensor.matmul(ps2[:, :], lhsT=wpj[:, k, d * 128:(d + 1) * 128], rhs=pT[:, k, :],
                                                            start=(k == 0), stop=(k == 1)) for k in range(2)],
                 reads=[wpj, pT], writes=[ps2])
            P.op("dve", lambda ps2=ps2, s_=s_: nc.vector.tensor_mul(out=s_[:, :], in0=ps2[:, :], in1=s_[:, :]),
                 reads=[ps2, s_], writes=[s_])
            P.op("dve", lambda d=d, s_=s_: nc.vector.tensor_add(out=h[:, d, :], in0=h[:, d, :], in1=s_[:, :]),
                 reads=[s_, h], writes=[h])
        if not final:
            P.dma("sp", S.HT[:, :, a:a + 512], h[:, :, :], reads=[h], writes=[P.db("HT", a // 512)])
        else:
            rmsnorm_fm(g, h, xo, VOFF["final_norm"], 512, sq, rstd, tmp)
            for j in range(4):
                o_ = ot.next()
                for k in range(8):
                    ps = g.psf.next()
                    P.mm([lambda j=j, k=k, ps=ps: nc.tensor.transpose(out=ps[:, 0:128], in_=xo[:, k, j * 128:(j + 1) * 128],
                                                                     identity=C.ident[:, :])], reads=[xo, C.ident], writes=[ps])
                    P.op("act" if k % 2 else "dve",
                         (lambda k=k, ps=ps, o_=o_: nc.scalar.copy(out=o_[:, k * 128:(k + 1) * 128], in_=ps[:, 0:128])) if k % 2 else
                         (lambda k=k, ps=ps, o_=o_: nc.vector.tensor_copy(out=o_[:, k * 128:(k + 1) * 128], in_=ps[:, 0:128])),
                         reads=[ps], writes=[o_])
                r0 = c * 512 + j * 128
                P.dma("sp", g.OUT[r0:r0 + 128, :], o_[:, :], reads=[o_], writes=[P.db("OUT", c * 4 + j)])


def phase_l1_in(g):
    P, nc, C, I, S, cfg = g.P, g.nc, g.C, g.I, g.S, g.cfg
    CTX, OWN = cfg.CTX, cfg.OWN
    NCH = CTX // 512
    vc = VOFF
    W1 = 2632
    win = P.sb("d_win", [128, 8, W1], BF16)
    P.dma("pool", win[:, :, :], I.w1.rearrange("(k p) f -> p k f", p=128), writes=[win])
    wuq = P.sb("d_wuq", [128, 2, 8, 96], BF16)
    P.dma("pool", wuq[:, :, :, :].rearrange("p k a b -> p k (a b)"), I.wuq.rearrange("(k p) f -> p k f", p=128), writes=[wuq])
    wukv = P.sb("d_wukv", [128, 768], BF16)
    P.dma("pool", wukv[:, :], I.mla_w_ukv[:, :], writes=[wukv])
    ropet = Ring([P.sb("d_rope%d" % i, [128, 2, 512], F32) for i in range(2)])
    bif = P.sb("d_bif", [128, 8], F32)
    P.dma("sp", bif[:, :], I.bif.partition_broadcast(128), writes=[bif])
    hT = Ring([P.sb("d_hT%d" % i, [128, 8, 512], F32) for i in range(2)])
    xnr = Ring([P.sb("d_xn%d" % i, [128, 8, 512], BF16) for i in range(2)])
    xn = None
    sq = Ring([P.sb("d_sq%d" % i, [128, 512], BF16) for i in range(2)])
    rstd = P.sb("d_rstd", [128, 512], F32)
    tmp = P.sb("d_tmp", [128, 512], F32)
    uqk = P.sb("d_uqk", [128, 8, 515], F32)
    P.op("pool", lambda: nc.gpsimd.memset(uqk[:, :, :], 0.0), writes=[uqk])
    cacc = P.sb("d_cacc", [128, 512], F32)
    qkc = Ring([P.sb("d_qkc%d" % i, [128, 8, 512], BF16) for i in range(2)])
    vt = Ring([P.sb("d_vt%d" % i, [128, 4, 129], BF16) for i in range(2)])
    ot = Ring([P.sb("d_ot%d" % i, [128, 512], BF16) for i in range(2)])
    ift = Ring([P.sb("d_ift%d" % i, [128, 8], F32) for i in range(2)])
    cq = P.sb("d_cq", [128, 2, 512], F32)
    cqn = P.sb("d_cqn", [128, 2, 512], BF16)
    ckv = P.sb("d_ckv", [128, 1, 512], F32)
    ckvn = P.sb("d_ckvn", [128, 1, 512], BF16)
    qm = Ring([P.sb("d_qm%d" % i, [128, 4, 512], BF16) for i in range(2)])
    kmt = Ring([P.sb("d_kmt%d" % i, [128, 4, 512], BF16) for i in range(2)])
    for t_ in qm.tiles + kmt.tiles:
        P.op("pool", lambda t_=t_: nc.gpsimd.memset(t_[:, :, :], 0.0), writes=[t_])
    kr = P.sb("d_kr", [128, 512], F32)
    kr2 = P.sb("d_kr2", [128, 512], F32)
    ksq = P.sb("d_ksq", [128, 512], F32)
    kmx = P.sb("d_kmx", [128, 4], F32)
    kmx1 = P.sb("d_kmx1", [128, 1], F32)
    P.op("pool", lambda: nc.gpsimd.memset(kmx[:, :], 0.0), writes=[kmx])
    vm = Ring([P.sb("d_vm%d" % i, [128, 4, 129], BF16) for i in range(2)])
    cw = vc["conv_w"]

    def load(c):
        h = hT.next()
        P.dma("sp", h[:, :, :], S.HT[:, :, c * 512:(c + 1) * 512], reads=[P.db("HT", c)], writes=[h])
        r_ = ropet.next()
        P.dma("sp", r_[64:96, :, :], I.rope[:, :, c * 512:(c + 1) * 512], writes=[r_])
        return h, r_

    def fm_mm(ps, col0, ncols, rhs_tile=None, nk=8, w=None):
        w = win if w is None else w
        P.mm([lambda k=k: nc.tensor.matmul(ps[0:ncols, :], lhsT=w[:, k, col0:col0 + ncols], rhs=xn[:, k, :],
                                           start=(k == 0), stop=(k == nk - 1)) for k in range(nk)], reads=[w, xn], writes=[ps])

    nxt = load(0)
    for c in range(NCH):
        h, rp = nxt
        if c + 1 < NCH:
            nxt = load(c + 1)
        xn = xnr.next()
        rmsnorm_fm(g, h, xn, vc["od_norm_mix"], 512, sq, rstd, tmp)
        qk = qkc.next()
        for f in range(8):
            ps = g.psf.next()
            fm_mm(ps, f * 128, 128)
            P.op("act", lambda f=f, ps=ps: nc.scalar.copy(out=uqk[:, f, 3:515], in_=ps[:, :]), reads=[ps], writes=[uqk])
            P.op("dve", lambda f=f: nc.vector.tensor_scalar_mul(out=cacc[:, :], in0=uqk[:, f, 0:512],
                                                                scalar1=C.vecs[:, cw + f * 4:cw + f * 4 + 1]),
                 reads=[uqk, C.vecs], writes=[cacc])
            for j in range(1, 4):
                P.op("dve", lambda f=f, j=j: nc.vector.scalar_tensor_tensor(
                    out=cacc[:, :], in0=uqk[:, f, j:j + 512], scalar=C.vecs[:, cw + f * 4 + j:cw + f * 4 + j + 1],
                    in1=cacc[:, :], op0=ALU.mult, op1=ALU.add), reads=[uqk, cacc, C.vecs], writes=[cacc])
            P.op("pool", lambda f=f: nc.gpsimd.tensor_copy(out=uqk[:, f, 0:3], in_=uqk[:, f, 512:515]), reads=[uqk], writes=[uqk])
            if f < 4:
                P.op("act", lambda f=f, qk=qk: nc.scalar.activation(out=qk[:, f, :], in_=cacc[:, :], func=AF.Silu),
                     reads=[cacc], writes=[qk])
            else:
                P.op("act", lambda f=f: nc.scalar.activation(out=tmp[:, :], in_=cacc[:, :], func=AF.Silu),
                     reads=[cacc], writes=[tmp])
                P.op("dve", lambda f=f, qk=qk: nc.vector.tensor_scalar_mul(out=qk[:, f, :], in0=tmp[:, :], scalar1=128.0 ** -0.5),
                     reads=[tmp], writes=[qk])
        P.dma("sp", S.QKC[:, :, c * 512:(c + 1) * 512], qk[:, :, :], reads=[qk], writes=[P.db("QKC", c)])
        for j in range(4):
            ps = g.psf.next()
            P.mm([lambda k=k, j=j, ps=ps: nc.tensor.matmul(ps[:, :], lhsT=xn[:, k, j * 128:(j + 1) * 128],
                                                          rhs=win[:, k, 1024:1536], start=(k == 0), stop=(k == 7))
                  for k in range(8)], reads=[win, xn], writes=[ps])
            v_ = vt.next()
            P.op("act", lambda ps=ps, v_=v_: nc.scalar.copy(out=v_[:, :, 0:128], in_=ps[:, :].rearrange("p (h d) -> p h d", h=4)),
                 reads=[ps], writes=[v_])
            P.op("pool", lambda v_=v_: nc.gpsimd.memset(v_[:, :, 128:129], 1.0), reads=[v_], writes=[v_])
            P.dma("sp", S.VC[c * 4 + j, :, :], v_[:, :, :].rearrange("p h d -> p (h d)"), reads=[v_], writes=[P.db("VC", c * 4 + j)])
            ps = g.psf.next()
            P.mm([lambda k=k, j=j, ps=ps: nc.tensor.matmul(ps[:, :], lhsT=xn[:, k, j * 128:(j + 1) * 128],
                                                          rhs=win[:, k, 1536:2048], start=(k == 0), stop=(k == 7))
                  for k in range(8)], reads=[win, xn], writes=[ps])
            o_ = ot.next()
            P.op("act", lambda ps=ps, o_=o_: nc.scalar.activation(out=o_[:, :], in_=ps[:, :], func=AF.Sigmoid),
                 reads=[ps], writes=[o_])
            P.dma("sp", S.OS[c * 4 + j, :, :], o_[:, :], reads=[o_], writes=[P.db("OS", c * 4 + j)])
            ps = g.psf.next()
            P.mm([lambda k=k, j=j, ps=ps: nc.tensor.matmul(ps[:, 0:8], lhsT=xn[:, k, j * 128:(j + 1) * 128],
                                                          rhs=win[:, k, 2048:2056], start=(k == 0), stop=(k == 7))
                  for k in range(8)], reads=[win, xn], writes=[ps])
            i_ = ift.next()
            P.op("dve", lambda ps=ps, i_=i_: nc.vector.tensor_add(out=i_[:, :], in0=ps[:, 0:8], in1=bif[:, :]),
                 reads=[ps, bif], writes=[i_])
            P.dma("sp", S.IF[c * 4 + j, :, :], i_[:, :], reads=[i_], writes=[P.db("IF", c * 4 + j)])
        for k2 in range(2):
            ps = g.psf.next()
            fm_mm(ps, 2056 + k2 * 128, 128)
            P.op("act", lambda k2=k2, ps=ps: nc.scalar.copy(out=cq[:, k2, :], in_=ps[:, :]), reads=[ps], writes=[cq])
        rmsnorm_fm(g, cq, cqn, vc["mla_q_norm"], 512, sq, rstd, tmp, nk=2, dim=256)
        ps = g.psf.next()
        fm_mm(ps, 2312, 128)
        P.op("act", lambda ps=ps: nc.scalar.copy(out=ckv[:, 0, :], in_=ps[:, :]), reads=[ps], writes=[ckv])
        rmsnorm_fm(g, ckv, ckvn, vc["mla_kv_norm"], 512, sq, rstd, tmp, nk=1, dim=128)

        def rope_rows(dst, psA, psB, scale):
            P.op("dve", lambda: nc.vector.tensor_mul(out=kr[64:96, :], in0=psA[64:96, :], in1=rp[64:96, 0, :]),
                 reads=[psA, rp], writes=[kr])
            P.op("dve", lambda: nc.vector.tensor_mul(out=kr2[64:96, :], in0=psB[64:96, :], in1=rp[64:96, 1, :]),
                 reads=[psB, rp], writes=[kr2])
            P.op("dve", lambda: nc.vector.scalar_tensor_tensor(out=dst, in0=kr[64:96, :], scalar=scale, in1=kr2[64:96, :],
                                                               op0=ALU.mult, op1=ALU.add), reads=[kr, kr2], writes=[])
        q_ = qm.next()
        for hh in range(4):
            psA = g.psf.next()
            P.mm([lambda k=k, hh=hh, psA=psA: nc.tensor.matmul(psA[0:96, :], lhsT=wuq[:, k, hh * 2, :], rhs=cqn[:, k, :],
                                                              start=(k == 0), stop=(k == 1)) for k in range(2)],
                 reads=[wuq, cqn], writes=[psA])
            psB = g.psf.next()
            P.mm([lambda k=k, hh=hh, psB=psB: nc.tensor.matmul(psB[0:96, :], lhsT=wuq[:, k, hh * 2 + 1, :], rhs=cqn[:, k, :],
                                                              start=(k == 0), stop=(k == 1)) for k in range(2)],
                 reads=[wuq, cqn], writes=[psB])
            sc = 96.0 ** -0.5
            P.op("act", lambda hh=hh, psA=psA, q_=q_: nc.scalar.mul(out=q_[0:64, hh, :], in_=psA[0:64, :], mul=sc),
                 reads=[psA], writes=[q_])
            P.op("dve", lambda psA=psA: nc.vector.tensor_mul(out=kr[64:96, :], in0=psA[64:96, :], in1=rp[64:96, 0, :]),
                 reads=[psA, rp], writes=[kr])
            P.op("dve", lambda psB=psB: nc.vector.tensor_mul(out=kr2[64:96, :], in0=psB[64:96, :], in1=rp[64:96, 1, :]),
                 reads=[psB, rp], writes=[kr2])
            P.op("dve", lambda: nc.vector.tensor_add(out=kr[64:96, :], in0=kr[64:96, :], in1=kr2[64:96, :]),
                 reads=[kr, kr2], writes=[kr])
            P.op("act", lambda hh=hh, q_=q_: nc.scalar.mul(out=q_[64:96, hh, :], in_=kr[64:96, :], mul=sc),
                 reads=[kr], writes=[q_])
        P.dma("sp", S.QM[:, :, c * 512:(c + 1) * 512], q_[:, :, :], reads=[q_], writes=[P.db("QM", c)])
        k_ = kmt.next()
        psA = g.psf.next()
        fm_mm(psA, 2440, 96)
        psB = g.psf.next()
        fm_mm(psB, 2536, 96)
        P.op("dve", lambda psA=psA: nc.vector.tensor_mul(out=kr[64:96, :], in0=psA[64:96, :], in1=rp[64:96, 0, :]),
             reads=[psA, rp], writes=[kr])
        P.op("dve", lambda psB=psB: nc.vector.tensor_mul(out=kr2[64:96, :], in0=psB[64:96, :], in1=rp[64:96, 1, :]),
             reads=[psB, rp], writes=[kr2])
        P.op("dve", lambda: nc.vector.tensor_add(out=kr[64:96, :], in0=kr[64:96, :], in1=kr2[64:96, :]),
             reads=[kr, kr2], writes=[kr])
        for hh in range(4):
            P.op("act", lambda hh=hh, k_=k_: nc.scalar.copy(out=k_[64:96, hh, :], in_=kr[64:96, :]), reads=[kr], writes=[k_])
            ps = g.psf.next()
            P.mm([lambda hh=hh, ps=ps: nc.tensor.matmul(ps[0:64, :], lhsT=wukv[:, hh * 192:hh * 192 + 64], rhs=ckvn[:, 0, :],
                                                       start=True, stop=True)], reads=[wukv, ckvn], writes=[ps])
            P.op("act", lambda hh=hh, ps=ps, k_=k_: nc.scalar.copy(out=k_[0:64, hh, :], in_=ps[0:64, :]), reads=[ps], writes=[k_])
            P.op("act", lambda hh=hh, k_=k_: nc.scalar.activation(out=ksq[0:96, :], in_=k_[0:96, hh, :], func=AF.Square),
                 reads=[k_], writes=[ksq])
            ps2 = g.psf.next()
            P.mm([lambda ps2=ps2: nc.tensor.matmul(ps2[:, :], lhsT=C.ones[0:96, :], rhs=ksq[0:96, :], start=True, stop=True)],
                 reads=[C.ones, ksq], writes=[ps2])
            P.op("dve", lambda ps2=ps2: nc.vector.reduce_max(out=kmx1[:, :], in_=ps2[:, :], axis=AX.X), reads=[ps2], writes=[kmx1])
            P.op("dve", lambda hh=hh: nc.vector.tensor_max(out=kmx[:, hh:hh + 1], in0=kmx[:, hh:hh + 1], in1=kmx1[:, :]),
                 reads=[kmx, kmx1], writes=[kmx])
        P.dma("sp", S.KMT[:, :, c * 512:(c + 1) * 512], k_[:, :, :], reads=[k_], writes=[P.db("KMT", c)])
        for j in range(4):
            ps = g.psf.next()
            P.mm([lambda j=j, ps=ps, hh=hh: nc.tensor.matmul(ps[:, hh * 128:(hh + 1) * 128], lhsT=ckvn[:, 0, j * 128:(j + 1) * 128],
                                                            rhs=wukv[:, hh * 192 + 64:hh * 192 + 192], start=True, stop=True)
                  for hh in range(4)], reads=[wukv, ckvn], writes=[ps])
            v_ = vm.next()
            P.op("act", lambda ps=ps, v_=v_: nc.scalar.copy(out=v_[:, :, 0:128], in_=ps[:, :].rearrange("p (h d) -> p h d", h=4)),
                 reads=[ps], writes=[v_])
            P.op("pool", lambda v_=v_: nc.gpsimd.memset(v_[:, :, 128:129], 1.0), reads=[v_], writes=[v_])
            P.dma("sp", S.VM[c * 4 + j, :, :], v_[:, :, :].rearrange("p h d -> p (h d)"), reads=[v_], writes=[P.db("VM", c * 4 + j)])
    P.dma("sp", S.KMAX1[:, :], kmx[:, :], reads=[kmx], writes=[P.db("KMAX1")])


def phase_mlstm(g):
    P, nc, C, I, S, cfg = g.P, g.nc, g.C, g.I, g.S, g.cfg
    CTX, OWN = cfg.CTX, cfg.OWN
    NT = CTX // 128
    T0 = (CTX - OWN) // 128
    ut = P.sb("l_ut", [128, 128], F32)
    P.op("pool", lambda: nc.gpsimd.memset(ut[:, :], 1.0), writes=[ut])
    P.op("pool", lambda: nc.gpsimd.affine_select(out=ut[:, :], in_=ut[:, :], pattern=[[1, 128]], compare_op=ALU.is_ge,
                                                 fill=0.0, base=0, channel_multiplier=-1), reads=[ut], writes=[ut])
    maskb = P.sb("l_maskb", [128, 128], F32)
    P.op("pool", lambda: nc.gpsimd.memset(maskb[:, :], 0.0), writes=[maskb])
    P.op("pool", lambda: nc.gpsimd.affine_select(out=maskb[:, :], in_=maskb[:, :], pattern=[[-1, 128]], compare_op=ALU.is_ge,
                                                 fill=NEG, base=0, channel_multiplier=1), reads=[maskb], writes=[maskb])
    identb = P.sb("l_identb", [128, 128], BF16)
    P.op("dve", lambda: nc.vector.tensor_copy(out=identb[:, :], in_=C.ident[:, :]), reads=[C.ident], writes=[identb])
    hn = P.sb("l_hn", [128, 512], F32)
    P.dma("sp", hn[:, :], I.hnorm.partition_broadcast(128), writes=[hn])
    flg = P.sb("l_flg", [128, 4], F32)
    P.dma("sp", flg[:, :], I.l1flags.partition_broadcast(128), writes=[flg])
    CT = P.sb("l_CT", [128, 4, 129], F32)
    CTb = P.sb("l_CTb", [128, 4, 129], BF16)
    mprev = P.sb("l_mprev", [128, 4], F32)
    P.op("pool", lambda: nc.gpsimd.memset(CT[:, :, :], 0.0), writes=[CT])
    P.op("pool", lambda: nc.gpsimd.memset(CTb[:, :, :], 0.0), writes=[CTb])
    P.op("pool", lambda: nc.gpsimd.memset(mprev[:, :], 0.0), writes=[mprev])
    qk = Ring([P.sb("l_qk%d" % i, [128, 8, 128], BF16) for i in range(2)])
    va = Ring([P.sb("l_va%d" % i, [128, 4, 129], BF16) for i in range(2)])
    os_ = Ring([P.sb("l_os%d" % i, [128, 512], BF16) for i in range(2)])
    ift = Ring([P.sb("l_if%d" % i, [128, 8], F32) for i in range(2)])
    sm = {n: P.sb("l_" + n, [128, 4], F32) for n in
          ("e", "logf", "b", "bend", "cc", "rowmax", "bm", "mt", "nmt", "wint", "gg", "gmax", "mnew", "nmnew", "dec", "ws",
           "emt", "den", "rden")}
    diag4 = P.sb("l_diag4", [128, 4, 128], F32)
    intra = P.sb("l_intra", [128, 4, 128], F32)
    Dm = P.sb("l_D", [128, 4, 128], F32)
    am = P.sb("l_a", [128, 4, 128], BF16)
    aT = P.sb("l_aT", [128, 4, 128], BF16)
    kw = P.sb("l_kw", [128, 4, 128], BF16)
    atmp = P.sb("l_atmp", [128, 129], F32)
    nd = P.sb("l_nd", [128, 129], F32)
    hsq = P.sb("l_hsq", [128, 128], F32)
    ss = P.sb("l_ss", [128, 4], F32)
    hh_ = P.sb("l_hh", [128, 4, 128], F32)
    cout = Ring([P.sb("l_cout%d" % i, [128, 512], BF16) for i in range(2)])
    coT = Ring([P.sb("l_coT%d" % i, [128, 4, 128], BF16) for i in range(2)])

    def load(c):
        q_ = qk.next()
        P.dma("sp", q_[:, :, :], S.QKC[:, :, c * 128:(c + 1) * 128], reads=[P.db("QKC", c // 4)], writes=[q_])
        v_ = va.next()
        P.dma("sp", v_[:, :, :].rearrange("p h d -> p (h d)"), S.VC[c, :, :], reads=[P.db("VC", c)], writes=[v_])
        o_ = os_.next()
        P.dma("sp", o_[:, :], S.OS[c, :, :], reads=[P.db("OS", c)], writes=[o_])
        i_ = ift.next()
        P.dma("sp", i_[:, :], S.IF[c, :, :], reads=[P.db("IF", c)], writes=[i_])
        return q_, v_, o_, i_

    def dv(fn, reads, writes):
        P.op("dve", fn, reads=reads, writes=writes)

    def ac(fn, reads, writes):
        P.op("act", fn, reads=reads, writes=writes)

    def chunk(c, q_, v_, o_, i_):
        if c == T0 and T0 > 0:
            dv(lambda: nc.vector.tensor_scalar_mul(out=CT[:, :, :], in0=CT[:, :, :], scalar1=flg[:, 0:1]), [CT, flg], [CT])
            dv(lambda: nc.vector.tensor_scalar_mul(out=CTb[:, :, :], in0=CTb[:, :, :], scalar1=flg[:, 0:1]), [CTb, flg], [CTb])
            dv(lambda: nc.vector.tensor_scalar_mul(out=mprev[:, :], in0=mprev[:, :], scalar1=flg[:, 0:1]), [mprev, flg], [mprev])
        ipre = i_[:, 0:4]
        ac(lambda: nc.scalar.activation(out=sm["e"][:, :], in_=i_[:, 4:8], func=AF.Exp, scale=-1.0), [i_], [sm["e"]])
        ac(lambda: nc.scalar.activation(out=sm["logf"][:, :], in_=sm["e"][:, :], func=AF.Ln, bias=1.0), [sm["e"]], [sm["logf"]])
        dv(lambda: nc.vector.tensor_scalar_mul(out=sm["logf"][:, :], in0=sm["logf"][:, :], scalar1=-1.0), [sm["logf"]], [sm["logf"]])
        ps = g.psf.next()
        P.mm([lambda: nc.tensor.matmul(ps[:, 0:4], lhsT=ut[:, :], rhs=sm["logf"][:, :], start=True, stop=True)],
             reads=[ut, sm["logf"]], writes=[ps])
        P.mm([lambda: nc.tensor.matmul(ps[:, 4:8], lhsT=C.ones[:, :], rhs=sm["logf"][:, :], start=True, stop=True)],
             reads=[C.ones, sm["logf"]], writes=[ps])
        dv(lambda: nc.vector.tensor_copy(out=sm["b"][:, :], in_=ps[:, 0:4]), [ps], [sm["b"]])
        dv(lambda: nc.vector.tensor_copy(out=sm["bend"][:, :], in_=ps[:, 4:8]), [ps], [sm["bend"]])
        dv(lambda: nc.vector.tensor_sub(out=sm["cc"][:, :], in0=ipre, in1=sm["b"][:, :]), [i_, sm["b"]], [sm["cc"]])
        dv(lambda: nc.vector.tensor_tensor(out=diag4[:, :, :], in0=C.ident[:, :].unsqueeze(1).broadcast_to([128, 4, 128]),
                                           in1=sm["cc"][:, :].unsqueeze(2).broadcast_to([128, 4, 128]), op=ALU.mult),
           [C.ident, sm["cc"]], [diag4])
        psr = g.psf.next()
        P.mm([lambda: nc.tensor.matmul(psr[:, :], lhsT=C.ones[:, :], rhs=diag4[:, :, :].rearrange("p a b -> p (a b)"),
                                       start=True, stop=True)], reads=[C.ones, diag4], writes=[psr])
        dv(lambda: nc.vector.tensor_tensor(out=intra[:, :, :], in0=psr[:, :].rearrange("p (a b) -> p a b", a=4),
                                           in1=maskb[:, :].unsqueeze(1).broadcast_to([128, 4, 128]), op=ALU.add),
           [psr, maskb], [intra])
        dv(lambda: nc.vector.tensor_tensor(out=intra[:, :, :], in0=intra[:, :, :],
                                           in1=sm["b"][:, :].unsqueeze(2).broadcast_to([128, 4, 128]), op=ALU.add),
           [intra, sm["b"]], [intra])
        dv(lambda: nc.vector.reduce_max(out=sm["rowmax"][:, :], in_=intra[:, :, :], axis=AX.X), [intra], [sm["rowmax"]])
        dv(lambda: nc.vector.tensor_add(out=sm["bm"][:, :], in0=sm["b"][:, :], in1=mprev[:, :]), [sm["b"], mprev], [sm["bm"]])
        dv(lambda: nc.vector.tensor_max(out=sm["mt"][:, :], in0=sm["bm"][:, :], in1=sm["rowmax"][:, :]),
           [sm["bm"], sm["rowmax"]], [sm["mt"]])
        dv(lambda: nc.vector.tensor_scalar_mul(out=sm["nmt"][:, :], in0=sm["mt"][:, :], scalar1=-1.0), [sm["mt"]], [sm["nmt"]])
        for hd in range(4):
            ac(lambda hd=hd: nc.scalar.activation(out=Dm[:, hd, :], in_=intra[:, hd, :], func=AF.Exp,
                                                  bias=sm["nmt"][:, hd:hd + 1], scale=1.0), [intra, sm["nmt"]], [Dm])
        dv(lambda: nc.vector.tensor_sub(out=sm["wint"][:, :], in0=sm["bm"][:, :], in1=sm["mt"][:, :]), [sm["bm"], sm["mt"]], [sm["wint"]])
        ac(lambda: nc.scalar.activation(out=sm["wint"][:, :], in_=sm["wint"][:, :], func=AF.Exp), [sm["wint"]], [sm["wint"]])
        ac(lambda: nc.scalar.activation(out=sm["emt"][:, :], in_=sm["mt"][:, :], func=AF.Exp, scale=-1.0), [sm["mt"]], [sm["emt"]])
        psq = g.psf.next()
        for hd in range(4):
            P.mm([lambda hd=hd: nc.tensor.matmul(psq[:, hd * 128:(hd + 1) * 128], lhsT=q_[:, hd, :], rhs=q_[:, 4 + hd, :],
                                                 start=True, stop=True)], reads=[q_], writes=[psq])
        dv(lambda: nc.vector.tensor_tensor(out=am[:, :, :], in0=psq[:, :].rearrange("p (a b) -> p a b", a=4), in1=Dm[:, :, :],
                                           op=ALU.mult), [psq, Dm], [am])
        pb = g.psb.next()
        P.mm([lambda hd=hd: nc.tensor.transpose(out=pb[:, hd * 128:(hd + 1) * 128], in_=am[:, hd, :], identity=identb[:, :])
              for hd in range(4)], reads=[am, identb], writes=[pb])
        dv(lambda: nc.vector.tensor_copy(out=aT[:, :, :].rearrange("p a b -> p (a b)"), in_=pb[:, 0:512]), [pb], [aT])
        own = c >= T0
        if own:
            co = cout.next()
            dv(lambda: nc.vector.memset(ss[:, :], 0.0), [ss], [ss])
            for hd in range(4):
                pA = g.psf.next()
                P.mm([lambda hd=hd, pA=pA: nc.tensor.matmul(pA[:, 0:129], lhsT=aT[:, hd, :], rhs=v_[:, hd, :],
                                                           start=True, stop=True)], reads=[aT, v_], writes=[pA])
                pB = g.psf.next()
                P.mm([lambda hd=hd, pB=pB: nc.tensor.matmul(pB[:, 256:385], lhsT=q_[:, hd, :], rhs=CTb[:, hd, :],
                                                           start=True, stop=True)], reads=[q_, CTb], writes=[pB])
                ac(lambda pA=pA: nc.scalar.copy(out=atmp[:, :], in_=pA[:, 0:129]), [pA], [atmp])
                dv(lambda pB=pB, hd=hd: nc.vector.scalar_tensor_tensor(out=nd[:, :], in0=pB[:, 256:385], scalar=sm["wint"][:, hd:hd + 1],
                                                                       in1=atmp[:, :], op0=ALU.mult, op1=ALU.add),
                   [pB, sm["wint"], atmp], [nd])
                ac(lambda hd=hd: nc.scalar.activation(out=sm["den"][:, hd:hd + 1], in_=nd[:, 128:129], func=AF.Abs),
                   [nd], [sm["den"]])
                dv(lambda hd=hd: nc.vector.tensor_max(out=sm["den"][:, hd:hd + 1], in0=sm["den"][:, hd:hd + 1],
                                                      in1=sm["emt"][:, hd:hd + 1]), [sm["den"], sm["emt"]], [sm["den"]])
                dv(lambda hd=hd: nc.vector.reciprocal(out=sm["rden"][:, hd:hd + 1], in_=sm["den"][:, hd:hd + 1]),
                   [sm["den"]], [sm["rden"]])
                dv(lambda hd=hd: nc.vector.tensor_scalar_mul(out=hh_[:, hd, :], in0=nd[:, 0:128], scalar1=sm["rden"][:, hd:hd + 1]),
                   [nd, sm["rden"]], [hh_])
                ac(lambda hd=hd: nc.scalar.activation(out=hsq[:, :], in_=hh_[:, hd, :], func=AF.Square, accum_out=ss[:, hd:hd + 1]),
                   [hh_], [hsq, ss])
            ac(lambda: nc.scalar.activation(out=ss[:, :], in_=ss[:, :], func=AF.Sqrt, bias=C.eps[:, 0:1], scale=1.0 / 128.0),
               [ss, C.eps], [ss])
            dv(lambda: nc.vector.reciprocal(out=ss[:, :], in_=ss[:, :]), [ss], [ss])
            dv(lambda: nc.vector.tensor_tensor(out=hh_[:, :, :], in0=hh_[:, :, :],
                                               in1=ss[:, :].unsqueeze(2).broadcast_to([128, 4, 128]), op=ALU.mult), [hh_, ss], [hh_])
            dv(lambda: nc.vector.tensor_mul(out=hh_[:, :, :].rearrange("p a b -> p (a b)"),
                                            in0=hh_[:, :, :].rearrange("p a b -> p (a b)"), in1=hn[:, :]), [hh_, hn], [hh_])
            dv(lambda co=co: nc.vector.tensor_mul(out=co[:, :], in0=hh_[:, :, :].rearrange("p a b -> p (a b)"), in1=o_[:, :]),
               [hh_, o_], [co])
            pb2 = g.psb.next()
            P.mm([lambda hd=hd, pb2=pb2, co=co: nc.tensor.transpose(out=pb2[:, hd * 128:(hd + 1) * 128],
                                                                   in_=co[:, hd * 128:(hd + 1) * 128], identity=identb[:, :])
                  for hd in range(4)], reads=[co, identb], writes=[pb2])
            ct_ = coT.next()
            dv(lambda pb2=pb2, ct_=ct_: nc.vector.tensor_copy(out=ct_[:, :, :].rearrange("p a b -> p (a b)"), in_=pb2[:, 0:512]),
               [pb2], [ct_])
            P.dma("sp", S.MIXT[:, 0:4, c * 128:(c + 1) * 128], ct_[:, :, :], reads=[ct_], writes=[P.db("MIXT_A", c)])
        dv(lambda: nc.vector.tensor_sub(out=sm["gg"][:, :], in0=sm["bend"][:, :], in1=sm["b"][:, :]), [sm["bend"], sm["b"]], [sm["gg"]])
        dv(lambda: nc.vector.tensor_add(out=sm["gg"][:, :], in0=sm["gg"][:, :], in1=ipre), [sm["gg"], i_], [sm["gg"]])
        dv(lambda: nc.vector.tensor_tensor(out=diag4[:, :, :], in0=C.ident[:, :].unsqueeze(1).broadcast_to([128, 4, 128]),
                                           in1=sm["gg"][:, :].unsqueeze(2).broadcast_to([128, 4, 128]), op=ALU.mult),
           [C.ident, sm["gg"]], [diag4])
        psg = g.psf.next()
        P.mm([lambda: nc.tensor.matmul(psg[:, :], lhsT=C.ones[:, :], rhs=diag4[:, :, :].rearrange("p a b -> p (a b)"),
                                       start=True, stop=True)], reads=[C.ones, diag4], writes=[psg])
        dv(lambda: nc.vector.reduce_max(out=sm["gmax"][:, :], in_=psg[:, :].rearrange("p (a b) -> p a b", a=4), axis=AX.X),
           [psg], [sm["gmax"]])
        dv(lambda: nc.vector.tensor_add(out=sm["mnew"][:, :], in0=sm["bend"][:, :], in1=mprev[:, :]), [sm["bend"], mprev], [sm["mnew"]])
        dv(lambda: nc.vector.tensor_sub(out=sm["dec"][:, :], in0=sm["mnew"][:, :], in1=sm["mnew"][:, :]), [sm["mnew"]], [sm["dec"]])
        dv(lambda: nc.vector.tensor_copy(out=sm["dec"][:, :], in_=sm["mnew"][:, :]), [sm["mnew"]], [sm["dec"]])
        dv(lambda: nc.vector.tensor_max(out=sm["mnew"][:, :], in0=sm["mnew"][:, :], in1=sm["gmax"][:, :]),
           [sm["mnew"], sm["gmax"]], [sm["mnew"]])
        dv(lambda: nc.vector.tensor_sub(out=sm["dec"][:, :], in0=sm["dec"][:, :], in1=sm["mnew"][:, :]), [sm["dec"], sm["mnew"]], [sm["dec"]])
        ac(lambda: nc.scalar.activation(out=sm["dec"][:, :], in_=sm["dec"][:, :], func=AF.Exp), [sm["dec"]], [sm["dec"]])
        dv(lambda: nc.vector.tensor_sub(out=sm["ws"][:, :], in0=sm["gg"][:, :], in1=sm["mnew"][:, :]), [sm["gg"], sm["mnew"]], [sm["ws"]])
        ac(lambda: nc.scalar.activation(out=sm["ws"][:, :], in_=sm["ws"][:, :], func=AF.Exp), [sm["ws"]], [sm["ws"]])
        pbk = g.psb.next()
        P.mm([lambda hd=hd: nc.tensor.transpose(out=pbk[:, hd * 128:(hd + 1) * 128], in_=q_[:, 4 + hd, :], identity=identb[:, :])
              for hd in range(4)], reads=[q_, identb], writes=[pbk])
        dv(lambda: nc.vector.tensor_tensor(out=kw[:, :, :], in0=pbk[:, 0:512].rearrange("p (a b) -> p a b", a=4),
                                           in1=sm["ws"][:, :].unsqueeze(2).broadcast_to([128, 4, 128]), op=ALU.mult),
           [pbk, sm["ws"]], [kw])
        for hd in range(4):
            pU = g.psf.next()
            P.mm([lambda hd=hd, pU=pU: nc.tensor.matmul(pU[:, 0:129], lhsT=kw[:, hd, :], rhs=v_[:, hd, :], start=True, stop=True)],
                 reads=[kw, v_], writes=[pU])
            dv(lambda hd=hd, pU=pU: nc.vector.scalar_tensor_tensor(out=CT[:, hd, :], in0=CT[:, hd, :], scalar=sm["dec"][:, hd:hd + 1],
                                                                   in1=pU[:, 0:129], op0=ALU.mult, op1=ALU.add),
               [CT, sm["dec"], pU], [CT])
        ac(lambda: nc.scalar.copy(out=CTb[:, :, :], in_=CT[:, :, :]), [CT], [CTb])
        dv(lambda: nc.vector.tensor_copy(out=mprev[:, :], in_=sm["mnew"][:, :]), [sm["mnew"]], [mprev])

    nxt = load(0)
    for c in range(NT):
        cur = nxt
        if c + 1 < NT:
            nxt = load(c + 1)
        chunk(c, *cur)


def phase_mla(g):
    P, nc, C, I, S, cfg = g.P, g.nc, g.C, g.I, g.S, g.cfg
    CTX, OWN = cfg.CTX, cfg.OWN
    NT = CTX // 128
    T0 = (CTX - OWN) // 128
    NCH = CTX // 512
    KT = P.sb("e_KT", [128, 4, CTX], BF16)
    kbufs = [Buf("eKT%d" % c) for c in range(NCH)]
    for c in range(NCH):
        P.dma("sp", KT[:, :, c * 512:(c + 1) * 512], S.KMT[:, :, c * 512:(c + 1) * 512], reads=[P.db("KMT", c)], writes=[kbufs[c]])
    VA = P.sb("e_VA", [128, NT, 516], BF16)
    vbufs = [Buf("eVA%d" % t) for t in range(NT)]
    for t in range(NT):
        P.dma("sp", VA[:, t, :], S.VM[t, :, :], reads=[P.db("VM", t)], writes=[vbufs[t]])
    kx = P.sb("e_kx", [128, 4], F32)
    P.dma("sp", kx[:, :], S.KMAX1[:, :], reads=[P.db("KMAX1")], writes=[kx])
    flg = P.sb("e_flg", [128, 4], F32)
    P.dma("sp", flg[:, :], I.l1flags.partition_broadcast(128), writes=[flg])
    tri = P.sb("e_tri", [128, 128], BF16)
    P.op("pool", lambda: nc.gpsimd.memset(tri[:, :], 1.0), writes=[tri])
    P.op("pool", lambda: nc.gpsimd.affine_select(out=tri[:, :], in_=tri[:, :], pattern=[[-1, 128]], compare_op=ALU.is_ge,
                                                 fill=0.0, base=0, channel_multiplier=1), reads=[tri], writes=[tri])
    identb = P.sb("e_identb", [128, 128], BF16)
    P.op("dve", lambda: nc.vector.tensor_copy(out=identb[:, :], in_=C.ident[:, :]), reads=[C.ident], writes=[identb])
    onesb = P.sb("e_onesb", [128, 1], BF16)
    P.op("pool", lambda: nc.gpsimd.memset(onesb[:, :], 1.0), writes=[onesb])
    qT = Ring([P.sb("e_qT%d" % i, [128, 4, 128], BF16) for i in range(2)])
    qsq = P.sb("e_qsq", [128, 4, 128], BF16)
    qn2 = P.sb("e_qn2", [128, 4], F32)
    mneg = Ring([P.sb("e_mneg%d" % i, [128, 4], F32) for i in range(2)])
    mpre = Ring([P.sb("e_mpre%d" % i, [128, 4], F32) for i in range(2)])
    Pp = Ring([P.sb("e_Pp%d" % i, [128, 1024], BF16) for i in range(6)])
    PT = Ring([P.sb("e_PT%d" % i, [128, 8, 128], BF16) for i in range(4)])
    dtok = Ring([P.sb("e_dtok%d" % i, [128, 512], BF16) for i in range(2)])
    dT = Ring([P.sb("e_dT%d" % i, [128, 4, 128], BF16) for i in range(2)])
    rden = P.sb("e_rden", [128, 1], F32)

    def loadq(i):
        t = qT.next()
        P.dma("sp", t[:, :, :], S.QM[:, :, i * 128:(i + 1) * 128], reads=[P.db("QM", i // 4)], writes=[t])
        return t

    n_z = sum(4 * ((i + 1 + 7) // 8) + 3 for i in range(T0, NT))
    side_k = -(-len(g.side) // max(1, int(n_z * 0.85)))
    nq = loadq(T0)
    for i in range(T0, NT):
        q = nq
        if i + 1 < NT:
            nq = loadq(i + 1)
        P.op("act", lambda q=q: nc.scalar.activation(out=qsq[0:96, :, :], in_=q[0:96, :, :], func=AF.Square), reads=[q], writes=[qsq])
        ps = g.psf.next()
        for hd in range(4):
            P.mm([lambda hd=hd, ps=ps: nc.tensor.matmul(ps[:, hd:hd + 1], lhsT=qsq[0:96, hd, :], rhs=onesb[0:96, :],
                                                       start=True, stop=True)], reads=[qsq, onesb], writes=[ps])
        P.op("dve", lambda ps=ps: nc.vector.tensor_mul(out=qn2[:, :], in0=ps[:, 0:4], in1=kx[:, :]), reads=[ps, kx], writes=[qn2])
        mn = mneg.next()
        mp = mpre.next()
        P.op("act", lambda mn=mn: nc.scalar.activation(out=mn[:, :], in_=qn2[:, :], func=AF.Sqrt), reads=[qn2], writes=[mn])
        P.op("dve", lambda mn=mn: nc.vector.tensor_scalar_mul(out=mn[:, :], in0=mn[:, :], scalar1=-1.0), reads=[mn], writes=[mn])
        P.op("dve", lambda mn=mn, mp=mp: nc.vector.tensor_scalar(out=mp[:, :], in0=mn[:, :], scalar1=flg[:, 1:2], scalar2=None,
                                                                op0=ALU.add), reads=[mn, flg], writes=[mp])
        nkt = i + 1
        ngr = (nkt + 7) // 8
        items = [(hd, gi) for hd in range(4) for gi in range(ngr)]
        dt_ = dtok.next()
        st = {}
        acc = {}

        def stage_s(hd, gi):
            k0 = gi * 8
            k1 = min(nkt, k0 + 8)
            pp = Pp.next()
            st[(hd, gi)] = [pp, None]
            for half in range(2):
                a0 = k0 + half * 4
                a1 = min(k1, a0 + 4)
                if a1 <= a0:
                    continue
                w = (a1 - a0) * 128
                ps = g.psf.next()
                kb = [kbufs[c] for c in range(a0 // 4, (a1 - 1) // 4 + 1)]
                P.mm([lambda ps=ps, a0=a0, w=w, hd=hd: nc.tensor.matmul(
                    ps[:, 0:w], lhsT=q[0:96, hd, :], rhs=KT[0:96, hd, a0 * 128:a0 * 128 + w], start=True, stop=True)],
                    reads=[q] + kb, writes=[ps])
                bias_t = mp if a0 < T0 else mn
                o0 = (a0 - k0) * 128
                P.op("act", lambda ps=ps, pp=pp, w=w, o0=o0, bias_t=bias_t, hd=hd: nc.scalar.activation(
                    out=pp[:, o0:o0 + w], in_=ps[:, 0:w], func=AF.Exp, bias=bias_t[:, hd:hd + 1], scale=1.0),
                    reads=[ps, bias_t], writes=[pp])
            if k1 == nkt:
                o0 = (nkt - 1 - k0) * 128
                P.op("pool", lambda pp=pp, o0=o0: nc.gpsimd.tensor_mul(out=pp[:, o0:o0 + 128], in0=pp[:, o0:o0 + 128],
                                                                      in1=tri[:, :]), reads=[pp, tri], writes=[pp])

        def stage_t(hd, gi):
            k0 = gi * 8
            k1 = min(nkt, k0 + 8)
            pp = st[(hd, gi)][0]
            pb = g.psb.next()
            P.mm([lambda j=j, pb=pb, pp=pp: nc.tensor.transpose(out=pb[:, j * 128:(j + 1) * 128],
                                                                in_=pp[:, j * 128:(j + 1) * 128], identity=identb[:, :])
                  for j in range(k1 - k0)], reads=[pp, identb], writes=[pb])
            pt = PT.next()
            st[(hd, gi)][1] = pt
            w = (k1 - k0) * 128
            P.op("dve", lambda pb=pb, pt=pt, w=w: nc.vector.tensor_copy(
                out=pt[:, :, :].rearrange("p a b -> p (a b)")[:, 0:w], in_=pb[:, 0:w]), reads=[pb], writes=[pt])

        def stage_v(hd, gi):
            k0 = gi * 8
            k1 = min(nkt, k0 + 8)
            pt = st[(hd, gi)][1]
            if gi == 0:
                acc[hd] = g.pacc.next()
            pa = acc[hd]
            vb = [vbufs[t] for t in range(k0, k1)]
            fns = [lambda j=j, pa=pa, pt=pt, k0=k0, hd=hd: nc.tensor.matmul(
                pa[:, 0:129], lhsT=pt[:, j, :], rhs=VA[:, k0 + j, hd * 129:(hd + 1) * 129],
                start=(k0 + j == 0), stop=(k0 + j == nkt - 1)) for j in range(k1 - k0)]
            P.mm(fns, reads=[pt] + vb, writes=[pa])
            if k1 == nkt:
                P.op("dve", lambda pa=pa: nc.vector.reciprocal(out=rden[:, :], in_=pa[:, 128:129]), reads=[pa], writes=[rden])
                P.op("act", lambda pa=pa, hd=hd: nc.scalar.mul(out=dt_[:, hd * 128:(hd + 1) * 128], in_=pa[:, 0:128], mul=rden[:, 0:1]),
                     reads=[pa, rden], writes=[dt_])
            del st[(hd, gi)]

        n_it = len(items)
        for z in range(n_it + 3):
            P.pump(g.side, side_k)
            if z < n_it:
                stage_s(*items[z])
            if 0 <= z - 2 < n_it:
                stage_t(*items[z - 2])
            if 0 <= z - 3 < n_it:
                stage_v(*items[z - 3])
        pb = g.psb.next()
        P.mm([lambda j=j, pb=pb: nc.tensor.transpose(out=pb[:, j * 128:(j + 1) * 128], in_=dt_[:, j * 128:(j + 1) * 128],
                                                     identity=identb[:, :]) for j in range(4)], reads=[dt_, identb], writes=[pb])
        d_ = dT.next()
        P.op("dve", lambda pb=pb, d_=d_: nc.vector.tensor_copy(out=d_[:, :, :].rearrange("p a b -> p (a b)"), in_=pb[:, 0:512]),
             reads=[pb], writes=[d_])
        P.dma("sp", S.MIXT[:, 4:8, i * 128:(i + 1) * 128], d_[:, :, :], reads=[d_], writes=[P.db("MIXT_D", i)])


def phase_router(g):
    P, nc, C, I, S, cfg, R = g.P, g.nc, g.C, g.I, g.S, g.cfg, g.R
    CTX, OWN, NTT, NST = cfg.CTX, cfg.OWN, cfg.NTT, cfg.NST
    t0 = CTX - OWN
    rw = P.sb("r_rw", [128, 8, 8], F32)
    P.dma("sp", rw[:, :, :], I.router_w.rearrange("(k p) e -> p k e", p=128), writes=[rw])
    rb = P.sb("r_rb", [128, 8], F32)
    P.dma("sp", rb[:, :], I.router_b.partition_broadcast(128), writes=[rb])
    wbase = P.sb("r_wbase", [128, 7], F32)
    P.dma("sp", wbase[:, :], I.widx_base[:, :], writes=[wbase])
    tpos = P.sb("r_tpos", [128, NST], F32)
    P.dma("sp", tpos[:, :], I.tilepos.partition_broadcast(128), writes=[tpos])
    ut = P.sb("r_ut", [128, 128], F32)
    P.op("pool", lambda: nc.gpsimd.memset(ut[:, :], 1.0), writes=[ut])
    P.op("pool", lambda: nc.gpsimd.affine_select(out=ut[:, :], in_=ut[:, :], pattern=[[1, 128]], compare_op=ALU.is_ge,
                                                 fill=0.0, base=0, channel_multiplier=-1), reads=[ut], writes=[ut])
    zt = P.sb("r_zt", [128, 4, D], BF16)
    P.op("pool", lambda: nc.gpsimd.memset(zt[:, :, :], 0.0), writes=[zt])
    for t in range(NST):
        P.dma("sp", S.XS[t * 512:(t + 1) * 512, :].rearrange("(j p) d -> p j d", p=128), zt[:, :, :], reads=[zt],
              writes=[P.db("XSz", t)])
    xf = Ring([P.sb("r_xf%d" % i, [128, 8, 512], F32) for i in range(2)])
    lg = P.sb("r_lg", [128, 8], F32)
    top8 = P.sb("r_top8", [128, 8], F32)
    nv1 = P.sb("r_nv1", [128, 1], F32)
    ex = P.sb("r_ex", [128, 8], F32)
    den = P.sb("r_den", [128, 1], F32)
    msk_all = P.sb("r_msk", [128, NTT, 8], F32)
    oh1_all = P.sb("r_oh1", [128, NTT, 8], F32)
    oh2_all = P.sb("r_oh2", [128, NTT, 8], F32)
    ex_all = P.sb("r_exa", [128, NTT, 8], F32)
    rank_all = P.sb("r_rank", [128, NTT, 8], F32)
    tmp_all = P.sb("r_tmpa", [128, NTT, 8], F32)
    carry = P.sb("r_carry", [128, 8], F32)
    P.op("pool", lambda: nc.gpsimd.memset(carry[:, :], 0.0), writes=[carry])
    for c in range(OWN // 512):
        a = t0 + c * 512
        x_ = xf.next()
        P.dma("sp", x_[:, :, :], S.XNF[:, :, a:a + 512], reads=[P.db("XNF", a // 512)], writes=[x_])
        for j in range(4):
            jt = c * 4 + j
            ps = g.psf.next()
            P.mm([lambda k=k, j=j, ps=ps: nc.tensor.matmul(ps[:, 0:8], lhsT=x_[:, k, j * 128:(j + 1) * 128], rhs=rw[:, k, :],
                                                          start=(k == 0), stop=(k == 7)) for k in range(8)],
                 reads=[x_, rw], writes=[ps])
            P.op("dve", lambda ps=ps: nc.vector.tensor_add(out=lg[:, :], in0=ps[:, 0:8], in1=rb[:, :]), reads=[ps, rb], writes=[lg])
            P.op("dve", lambda: nc.vector.max(out=top8[:, :], in_=lg[:, :]), reads=[lg], writes=[top8])
            P.op("dve", lambda: nc.vector.tensor_scalar_mul(out=nv1[:, :], in0=top8[:, 0:1], scalar1=-1.0), reads=[top8], writes=[nv1])
            P.op("act", lambda: nc.scalar.activation(out=ex[:, :], in_=lg[:, :], func=AF.Exp, bias=nv1[:, 0:1], scale=1.0),
                 reads=[lg, nv1], writes=[ex])
            P.op("dve", lambda jt=jt: nc.vector.tensor_scalar(out=msk_all[:, jt, :], in0=lg[:, :], scalar1=top8[:, 1:2], scalar2=None,
                                                              op0=ALU.is_ge), reads=[lg, top8], writes=[msk_all])
            P.op("dve", lambda jt=jt: nc.vector.tensor_scalar(out=oh1_all[:, jt, :], in0=lg[:, :], scalar1=top8[:, 0:1], scalar2=None,
                                                              op0=ALU.is_ge), reads=[lg, top8], writes=[oh1_all])
            P.op("dve", lambda jt=jt: nc.vector.tensor_mul(out=ex[:, :], in0=ex[:, :], in1=msk_all[:, jt, :]), reads=[ex, msk_all], writes=[ex])
            P.op("dve", lambda: nc.vector.reduce_sum(out=den[:, :], in_=ex[:, :], axis=AX.X), reads=[ex], writes=[den])
            P.op("dve", lambda: nc.vector.reciprocal(out=den[:, :], in_=den[:, :]), reads=[den], writes=[den])
            P.op("dve", lambda jt=jt: nc.vector.tensor_scalar_mul(out=ex_all[:, jt, :], in0=ex[:, :], scalar1=den[:, 0:1]),
                 reads=[ex, den], writes=[ex_all])
            psc = g.psf.next()
            P.mm([lambda jt=jt, psc=psc: nc.tensor.matmul(psc[:, 0:8], lhsT=ut[:, :], rhs=msk_all[:, jt, :], start=True, stop=True)],
                 reads=[ut, msk_all], writes=[psc])
            P.mm([lambda jt=jt, psc=psc: nc.tensor.matmul(psc[:, 8:16], lhsT=C.ones[:, :], rhs=msk_all[:, jt, :], start=True, stop=True)],
                 reads=[C.ones, msk_all], writes=[psc])
            P.op("dve", lambda jt=jt, psc=psc: nc.vector.tensor_add(out=rank_all[:, jt, :], in0=psc[:, 0:8], in1=carry[:, :]),
                 reads=[psc, carry], writes=[rank_all])
            P.op("dve", lambda psc=psc: nc.vector.tensor_add(out=carry[:, :], in0=carry[:, :], in1=psc[:, 8:16]),
                 reads=[psc, carry], writes=[carry])
    rem = P.sb("r_rem", [128, 8], F32)
    pad = P.sb("r_pad", [128, 8], F32)
    end = P.sb("r_end", [128, 8], F32)
    offm1 = P.sb("r_offm1", [128, 8], F32)
    dv = lambda fn, rd, wr: P.op("dve", fn, reads=rd, writes=wr)
    M = OWN // 512
    cmp2 = P.sb("r_cmp2", [128, 8, M], F32)
    dv(lambda: nc.vector.tensor_tensor(out=cmp2[:, :, :], in0=carry[:, :].unsqueeze(2).broadcast_to([128, 8, M]),
                                       in1=tpos[:, 0:M].unsqueeze(1).broadcast_to([128, 8, M]), op=ALU.is_gt), [carry, tpos], [cmp2])
    dv(lambda: nc.vector.reduce_sum(out=pad[:, :], in_=cmp2[:, :, :], axis=AX.X), [cmp2], [pad])
    dv(lambda: nc.vector.tensor_scalar_mul(out=pad[:, :], in0=pad[:, :], scalar1=512.0), [pad], [pad])
    dv(lambda: nc.vector.tensor_copy(out=end[:, 0:1], in_=pad[:, 0:1]), [pad], [end])
    for e in range(1, 8):
        dv(lambda e=e: nc.vector.tensor_add(out=end[:, e:e + 1], in0=end[:, e - 1:e], in1=pad[:, e:e + 1]), [end, pad], [end])
    dv(lambda: nc.vector.tensor_sub(out=offm1[:, :], in0=end[:, :], in1=pad[:, :]), [end, pad], [offm1])
    dv(lambda: nc.vector.tensor_scalar_add(out=offm1[:, :], in0=offm1[:, :], scalar1=-1.0), [offm1], [offm1])
    dv(lambda: nc.vector.tensor_tensor(out=rank_all[:, :, :], in0=rank_all[:, :, :],
                                       in1=offm1[:, :].unsqueeze(1).broadcast_to([128, NTT, 8]), op=ALU.add),
       [rank_all, offm1], [rank_all])
    dv(lambda: nc.vector.tensor_sub(out=oh2_all[:, :, :], in0=msk_all[:, :, :], in1=oh1_all[:, :, :]), [msk_all, oh1_all], [oh2_all])
    s1f = P.sb("r_s1f", [128, NTT], F32)
    s2f = P.sb("r_s2f", [128, NTT], F32)
    for (oh, val, dst) in ((oh1_all, rank_all, s1f), (oh2_all, rank_all, s2f), (oh1_all, ex_all, R.g1), (oh2_all, ex_all, R.g2)):
        dv(lambda oh=oh, val=val: nc.vector.tensor_mul(out=tmp_all[:, :, :], in0=oh[:, :, :], in1=val[:, :, :]), [oh, val], [tmp_all])
        dv(lambda dst=dst: nc.vector.reduce_sum(out=dst[:, :], in_=tmp_all[:, :, :], axis=AX.X), [tmp_all], [dst])
    for sf in (s1f, s2f):
        dv(lambda sf=sf: nc.vector.tensor_scalar(out=sf[:, :], in0=sf[:, :], scalar1=0.0, scalar2=float(cfg.NSLOT - 1),
                                                 op0=ALU.max, op1=ALU.min), [sf], [sf])
    dv(lambda: nc.vector.tensor_copy(out=R.s1i[:, :], in_=s1f[:, :]), [s1f], [R.s1i])
    dv(lambda: nc.vector.tensor_copy(out=R.s2i[:, :], in_=s2f[:, :]), [s2f], [R.s2i])
    cmp = P.sb("r_cmp", [128, NST, 8], F32)
    eid = P.sb("r_eid", [128, NST], F32)
    wf = P.sb("r_wf", [128, NST, 7], F32)
    dv(lambda: nc.vector.tensor_tensor(out=cmp[:, :, :], in0=end[:, :].unsqueeze(1).broadcast_to([128, NST, 8]),
                                       in1=tpos[:, :].unsqueeze(2).broadcast_to([128, NST, 8]), op=ALU.is_le), [end, tpos], [cmp])
    dv(lambda: nc.vector.reduce_sum(out=eid[:, :], in_=cmp[:, :, :], axis=AX.X), [cmp], [eid])
    dv(lambda: nc.vector.tensor_scalar(out=eid[:, :], in0=eid[:, :], scalar1=7.0, scalar2=896.0, op0=ALU.min, op1=ALU.mult), [eid], [eid])
    dv(lambda: nc.vector.tensor_tensor(out=wf[:, :, :], in0=eid[:, :].unsqueeze(2).broadcast_to([128, NST, 7]),
                                       in1=wbase[:, :].unsqueeze(1).broadcast_to([128, NST, 7]), op=ALU.add), [eid, wbase], [wf])
    dv(lambda: nc.vector.tensor_copy(out=R.widx[:, :], in_=wf[:, :, :].rearrange("p a b -> p (a b)")), [wf], [R.widx])


def phase_scatter(g):
    P, nc, C, I, S, cfg, R = g.P, g.nc, g.C, g.I, g.S, g.cfg, g.R
    xt = Ring([P.sb("s_xt%d" % i, [128, D], BF16) for i in range(4)])
    for jt in range(cfg.NTT):
        x_ = xt.next()
        P.dma("sp", x_[:, :], S.XNT[jt * 128:(jt + 1) * 128, :], reads=[P.db("XNT", jt)], writes=[x_])
        for k, si in enumerate((R.s1i, R.s2i)):
            P.idma(out=S.XS[:, :], in_=x_[:, :], out_off=bass.IndirectOffsetOnAxis(ap=si[:, jt:jt + 1], axis=0),
                   bounds=cfg.NSLOT - 1, reads=[x_, si], writes=[P.db("XSs", jt, k)])


def phase_experts(g):
    P, nc, C, I, S, cfg, R = g.P, g.nc, g.C, g.I, g.S, g.cfg, g.R
    NST = cfg.NST
    identb = P.sb("x_identb", [128, 128], BF16)
    P.op("dve", lambda: nc.vector.tensor_copy(out=identb[:, :], in_=C.ident[:, :]), reads=[C.ident], writes=[identb])
    wr = Ring([P.sb("x_w%d" % i, [128, 12288], BF16) for i in range(3)])
    xsr = Ring([P.sb("x_xs%d" % i, [128, 4, D], BF16) for i in range(2)])
    xTr = Ring([P.sb("x_xT%d" % i, [128, 8, 512], BF16) for i in range(2)])
    yar = Ring([P.sb("x_ya%d" % i, [128, 4, D], F32) for i in range(2)])
    t1r = Ring([P.sb("x_t1%d" % i, [128, 512], BF16) for i in range(3)])
    actr = Ring([P.sb("x_act%d" % i, [128, 4, 512], BF16) for i in range(3)])
    work = [(t, fb) for t in range(NST) for fb in range(7)]

    def loadw(t, fb):
        w = wr.next()
        P.idma(out=w[:, :], in_=I.moe_w[:, :], in_off=bass.IndirectOffsetOnAxis(ap=R.widx[:, t * 7 + fb:t * 7 + fb + 1], axis=0),
               bounds=8 * 7 * 128 - 1, reads=[R.widx], writes=[w])
        return w

    def loadx(t):
        xs = xsr.next()
        P.dma("sp", xs[:, :, :], S.XS[t * 512:(t + 1) * 512, :].rearrange("(j p) d -> p j d", p=128), writes=[xs])
        return xs

    def make_xT(xs):
        xT = xTr.next()
        for k2 in range(4):
            pb = g.psb.next()
            P.mm([lambda kk=kk, jj=jj, pb=pb, k2=k2: nc.tensor.transpose(
                out=pb[:, (kk * 4 + jj) * 128:(kk * 4 + jj + 1) * 128],
                in_=xs[:, jj, (2 * k2 + kk) * 128:(2 * k2 + kk + 1) * 128], identity=identb[:, :])
                for kk in range(2) for jj in range(4)], reads=[xs, identb], writes=[pb])
            P.op("dve", lambda pb=pb, k2=k2, xT=xT: nc.vector.tensor_copy(
                out=xT[:, 2 * k2:2 * k2 + 2, :].rearrange("p a b -> p (a b)"), in_=pb[:, 0:1024]), reads=[pb], writes=[xT])
        return xT

    def up_stage(w, xT):
        act = actr.next()
        for j in range(4):
            psg = g.psf.next()
            P.mm([lambda k=k, j=j, psg=psg: nc.tensor.matmul(
                psg[:, :], lhsT=w[:, k * 512 + j * 128:k * 512 + (j + 1) * 128], rhs=xT[:, k, :],
                start=(k == 0), stop=(k == 7)) for k in range(8)], reads=[w, xT], writes=[psg])
            psu = g.psf.next()
            P.mm([lambda k=k, j=j, psu=psu: nc.tensor.matmul(
                psu[:, :], lhsT=w[:, 4096 + k * 512 + j * 128:4096 + k * 512 + (j + 1) * 128], rhs=xT[:, k, :],
                start=(k == 0), stop=(k == 7)) for k in range(8)], reads=[w, xT], writes=[psu])
            t1 = t1r.next()
            P.op("act", lambda psg=psg, t1=t1: nc.scalar.activation(out=t1[:, :], in_=psg[:, :], func=AF.Silu),
                 reads=[psg], writes=[t1])
            P.op("dve", lambda psu=psu, t1=t1, act=act, j=j: nc.vector.tensor_mul(
                out=act[:, j, :], in0=psu[:, :], in1=t1[:, :]), reads=[psu, t1], writes=[act])
        return act

    def down_stage(t, fb, w, act, ya):
        for jj in range(4):
            for dh in range(2):
                pd = g.pacc.next()
                P.mm([lambda j=j, jj=jj, dh=dh, pd=pd: nc.tensor.matmul(
                    pd[:, :], lhsT=act[:, j, jj * 128:(jj + 1) * 128],
                    rhs=w[:, 8192 + j * 1024 + dh * 512:8192 + j * 1024 + (dh + 1) * 512],
                    start=(j == 0), stop=(j == 3)) for j in range(4)], reads=[w, act], writes=[pd])
                if fb == 0:
                    P.op("act", lambda jj=jj, dh=dh, pd=pd: nc.scalar.copy(out=ya[:, jj, dh * 512:(dh + 1) * 512], in_=pd[:, :]),
                         reads=[pd], writes=[ya])
                else:
                    P.op("dve", lambda jj=jj, dh=dh, pd=pd: nc.vector.tensor_add(
                        out=ya[:, jj, dh * 512:(dh + 1) * 512], in0=pd[:, :], in1=ya[:, jj, dh * 512:(dh + 1) * 512]),
                        reads=[pd, ya], writes=[ya])
        if fb == 6:
            P.dma("sp", S.Y[t * 512:(t + 1) * 512, :].rearrange("(j p) d -> p j d", p=128), ya[:, :, :], reads=[ya],
                  writes=[P.db("Y", t)])

    wq = [loadw(*work[0]), loadw(*work[1])]
    nx = loadx(0)
    prev = None
    xT = None
    ya = None
    for wi, (t, fb) in enumerate(work):
        w = wq.pop(0)
        if fb == 0:
            xs = nx
            if t + 1 < NST:
                nx = loadx(t + 1)
            xT = make_xT(xs)
            ya = yar.next()
        act = up_stage(w, xT)
        if prev is not None:
            down_stage(*prev)
        if wi + 2 < len(work):
            wq.append(loadw(*work[wi + 2]))
        prev = (t, fb, w, act, ya)
    down_stage(*prev)


def phase_combine(g):
    P, nc, C, I, S, cfg, R = g.P, g.nc, g.C, g.I, g.S, g.cfg, g.R
    CTX, OWN = cfg.CTX, cfg.OWN
    t0 = CTX - OWN
    hT = Ring([P.sb("k_hT%d" % i, [128, 8, 512], F32) for i in range(2)])
    y1r = Ring([P.sb("k_y1%d" % i, [128, D], F32) for i in range(3)])
    y2r = Ring([P.sb("k_y2%d" % i, [128, D], F32) for i in range(3)])
    ycr = Ring([P.sb("k_yc%d" % i, [128, D], F32) for i in range(2)])

    def gather(jt):
        y1, y2 = y1r.next(), y2r.next()
        P.idma(out=y1[:, :], in_=S.Y[:, :], in_off=bass.IndirectOffsetOnAxis(ap=R.s1i[:, jt:jt + 1], axis=0),
               bounds=cfg.NSLOT - 1, reads=[R.s1i], writes=[y1])
        P.idma(out=y2[:, :], in_=S.Y[:, :], in_off=bass.IndirectOffsetOnAxis(ap=R.s2i[:, jt:jt + 1], axis=0),
               bounds=cfg.NSLOT - 1, reads=[R.s2i], writes=[y2])
        return y1, y2

    def loadh(c):
        a = t0 + c * 512
        h = hT.next()
        P.dma("sp", h[:, :, :], S.HT[:, :, a:a + 512], reads=[P.db("HT", a // 512)], writes=[h])
        return h

    nh = loadh(0)
    ny = gather(0)
    for c in range(OWN // 512):
        a = t0 + c * 512
        h = nh
        if c + 1 < OWN // 512:
            nh = loadh(c + 1)
        for jj in range(4):
            jt = c * 4 + jj
            y1, y2 = ny
            if jt + 1 < cfg.NTT:
                ny = gather(jt + 1)
            yc = ycr.next()
            P.op("act", lambda y1=y1, yc=yc, jt=jt: nc.scalar.mul(out=yc[:, :], in_=y1[:, :], mul=R.g1[:, jt:jt + 1]),
                 reads=[y1, R.g1], writes=[yc])
            P.op("dve", lambda y2=y2, yc=yc, jt=jt: nc.vector.scalar_tensor_tensor(
                out=yc[:, :], in0=y2[:, :], scalar=R.g2[:, jt:jt + 1], in1=yc[:, :], op0=ALU.mult, op1=ALU.add),
                reads=[y2, yc, R.g2], writes=[yc])
            for half in range(2):
                ps = g.psf.next()
                P.mm([lambda k=k, half=half, ps=ps, yc=yc: nc.tensor.transpose(
                    out=ps[:, k * 128:(k + 1) * 128], in_=yc[:, (half * 4 + k) * 128:(half * 4 + k + 1) * 128],
                    identity=C.ident[:, :]) for k in range(4)], reads=[yc, C.ident], writes=[ps])
                P.op("dve", lambda half=half, ps=ps, jj=jj, h=h: nc.vector.tensor_tensor(
                    out=h[:, half * 4:half * 4 + 4, jj * 128:(jj + 1) * 128],
                    in0=h[:, half * 4:half * 4 + 4, jj * 128:(jj + 1) * 128],
                    in1=ps[:, :].rearrange("p (a b) -> p a b", a=4), op=ALU.add), reads=[ps, h], writes=[h])
        P.dma("sp", S.HT[:, :, a:a + 512], h[:, :, :], reads=[h], writes=[P.db("HT", a // 512)])


_NC_CACHE = {}


def _tables(CTX, OWN, early):
    NBLK = CTX // 256
    NT = CTX // 128
    pad = (CTX - OWN) if early else 0
    t = np.arange(CTX) - pad
    tpos = np.maximum(t, 0)
    invc = np.stack([1.0 / np.minimum(tpos + 1, w) for w in (2, 4, 8, 16)]).astype(np.float32)
    sl = (2.0 ** (-8.0 * np.arange(1, 9) / 8)).astype(np.float32)
    ev = np.exp(sl[None, :] * (np.arange(128)[:, None] - 127.0)).astype(np.float32)
    rowt = (sl[None, :] * (127.0 - np.arange(128)[:, None])).astype(np.float32)
    alib = (sl[:, None] * 128.0 * np.arange(NT)[None, :]).astype(np.float32)
    pastb = np.full((NBLK, NBLK), NEG, np.float32)
    pastb2 = np.full((NBLK, NBLK), NEG, np.float32)
    for ob in range(NBLK):
        for n in range(NBLK):
            if n < ob and n >= pad // 256:
                pastb[ob, n] = 0.0
                pastb2[ob, n] = 0.0
        pastb2[ob, ob] = 0.0
    inv_freq = (10000.0 ** (-np.arange(16, dtype=np.float32) / 16.0)).astype(np.float32)
    ang = tpos.astype(np.float32)[None, :] * inv_freq[:, None]
    cos = np.cos(ang).astype(np.float32)
    sin = np.sin(ang).astype(np.float32)
    rope = np.stack([np.concatenate([cos, cos], 0), np.concatenate([-sin, sin], 0)], 1).astype(np.float32)
    flags = np.array([0.0 if early else 1.0, NEG if early else 0.0, 0.0, 0.0], np.float32)
    return dict(invc=invc, ev_tab=ev, rowt=rowt, alib=alib, pastb=pastb, pastb2=pastb2, rope=np.ascontiguousarray(rope),
                l1flags=flags)


def _moe_layout(wg, wu, wd):
    wg = np.asarray(wg, np.float32).reshape(8, 8, 128, 7, 512).transpose(0, 3, 2, 1, 4).reshape(8, 7, 128, 4096)
    wu = np.asarray(wu, np.float32).reshape(8, 8, 128, 7, 512).transpose(0, 3, 2, 1, 4).reshape(8, 7, 128, 4096)
    wd = np.asarray(wd, np.float32).reshape(8, 7, 4, 128, 1024).transpose(0, 1, 3, 2, 4).reshape(8, 7, 128, 4096)
    return np.ascontiguousarray(np.concatenate([wg, wu, wd], axis=3).reshape(8 * 7 * 128, 12288))


def _shared(inp):
    f = lambda a: np.ascontiguousarray(np.asarray(a, dtype=np.float32))
    vecs = np.zeros((128, NV), np.float32)

    def putv(name, v):
        v = np.asarray(v, np.float32).reshape(-1, 128).T
        vecs[:, VOFF[name]:VOFF[name] + v.shape[1]] = v
    putv("ev_norm_mix", inp["ev_norm_mix"][0])
    putv("ev_norm_ffn", inp["ev_norm_ffn"][0])
    putv("pool_scale", inp["pool_scale"][0])
    putv("od_norm_mix", inp["od_norm_mix"][0])
    putv("od_norm_ffn", inp["od_norm_ffn"][0])
    putv("ple_norm0", inp["ple_norm"][0])
    putv("ple_norm1", inp["ple_norm"][1])
    putv("final_norm", inp["final_norm"])
    putv("mla_q_norm", inp["mla_q_norm"][0])
    putv("mla_kv_norm", inp["mla_kv_norm"][0])
    cw = np.asarray(inp["conv_w"][0], np.float32)
    cwl = cw.reshape(4, 8, 128).transpose(2, 1, 0).reshape(128, 32)
    vecs[:, VOFF["conv_w"]:VOFF["conv_w"] + 32] = cwl
    w_in = np.asarray(inp["od_w_in"][0], np.float32)
    z64 = np.zeros((D, 64), np.float32)
    kr = w_in[:, 2440:2472]
    w1 = np.concatenate([w_in[:, 0:2440], z64, kr, z64, kr[:, 16:32], kr[:, 0:16]], axis=1)
    wq = np.asarray(inp["mla_w_uq"][0], np.float32)
    parts = []
    for h in range(4):
        base = wq[:, h * 96:(h + 1) * 96]
        parts.append(base)
        parts.append(np.concatenate([np.zeros((256, 64), np.float32), base[:, 80:96], base[:, 64:80]], axis=1))
    wuq = np.concatenate(parts, axis=1)
    bif = np.concatenate([np.asarray(inp["gate_b_i"][0], np.float32), np.asarray(inp["gate_b_f"][0], np.float32)])
    return {
        "vecs": vecs, "ev_w_in": f(inp["ev_w_in"][0]), "pool_w": f(inp["pool_w"][0]), "ev_w_out": f(inp["ev_w_out"][0]),
        "ffn_w_gate": f(inp["ffn_w_gate"]), "ffn_w_up": f(inp["ffn_w_up"]), "ffn_w_down": f(inp["ffn_w_down"]),
        "ple_w_gate": f(inp["ple_w_gate"]), "ple_w_proj": f(inp["ple_w_proj"]),
        "w1": f(w1), "wuq": f(wuq), "mla_w_ukv": f(inp["mla_w_ukv"][0]), "bif": f(bif),
        "od_w_out": f(inp["od_w_out"][0]), "router_w": f(inp["router_w"][0]), "router_b": f(inp["router_b"][0]),
        "moe_w": _moe_layout(inp["moe_w_gate"][0], inp["moe_w_up"][0], inp["moe_w_down"][0]),
        "widx_base": np.ascontiguousarray((np.arange(7)[None, :] * 128 + np.arange(128)[:, None]).astype(np.float32)),
        "hnorm": f(inp["mlstm_norm"][0]),
    }


def run_module(inp, seq, cfg_kw=None):
    x = np.asarray(inp["x"], np.float32)
    p = np.asarray(inp["p"], np.float32)
    B = x.shape[0]
    CTX, OWN = seq, seq // 2
    key = (CTX, OWN)
    if key not in _NC_CACHE:
        _NC_CACHE[key] = build(Cfg(CTX=CTX, OWN=OWN, **(cfg_kw or {})))
    nc = _NC_CACHE[key]
    shared = _shared(inp)
    shared["tilepos"] = (np.arange((2 * OWN) // 512 + 7) * 512.0).astype(np.float32)
    tabs = [_tables(CTX, OWN, early=False), _tables(CTX, OWN, early=True)]
    maps = []
    for c in range(8):
        b, half = (c // 2) % B, c % 2
        m = dict(shared)
        if half == 0:
            xc = np.zeros((CTX, D), np.float32)
            xc[CTX - OWN:] = x[b, 0:OWN]
            p0 = np.zeros((CTX, 256), np.float32)
            p0[CTX - OWN:] = p[0, b, 0:OWN]
            p1 = p[1, b, 0:OWN]
            m.update(tabs[1])
        else:
            xc = x[b, 0:CTX]
            p0 = p[0, b, 0:CTX]
            p1 = p[1, b, OWN:CTX]
            m.update(tabs[0])
        m["xc"] = np.ascontiguousarray(xc)
        m["p0c"] = np.ascontiguousarray(p0)
        m["p1c"] = np.ascontiguousarray(p1)
        maps.append(m)
    res = run_bass_kernel_spmd(nc, maps, core_ids=list(range(8)))
    out = np.zeros((B, seq, D), np.float32)
    for c in range(8):
        b, half = c // 2, c % 2
        if b < B:
            out[b, half * OWN:(half + 1) * OWN] = res.results[c]["out"]
    return out, res


def kernel(**inputs):
    out, _ = run_module(inputs, 8192)
    return out
```

```python
import os
import numpy as np
from contextlib import ExitStack
import concourse.bass as bass
import concourse.mybir as mybir
from concourse.bass_utils import run_bass_kernel_spmd

F32 = mybir.dt.float32
BF16 = mybir.dt.bfloat16
I32 = mybir.dt.int32
AF = mybir.ActivationFunctionType
ALU = mybir.AluOpType
AX = mybir.AxisListType

D = 1024
NEG = -1.0e30


class Buf:
    __slots__ = ("name", "w", "r")

    def __init__(self, name=""):
        self.name = name
        self.w = None
        self.r = {}


class T:
    def __init__(self, t, buf):
        self.t = t
        self.b = buf

    def __getitem__(self, idx):
        return self.t[idx]


class Prog:
    NDMASEM = 12

    def __init__(self, nc, es):
        self.nc = nc
        self.es = es
        self.eng = {"pe": nc.tensor, "act": nc.scalar, "dve": nc.vector, "pool": nc.gpsimd, "sp": nc.sync}
        self.sems = {}
        self.cnt = {}
        for e in ("pe", "act", "dve", "pool"):
            self.sems[e] = es.enter_context(nc.semaphore("sem_" + e))
            self.cnt[e] = 0
        self.dq = {}
        for q in ("sp", "pool"):
            ss = []
            for i in range(self.NDMASEM):
                k = "dma_%s_%d" % (q, i)
                self.sems[k] = es.enter_context(nc.semaphore(k))
                ss.append(k)
            self.dq[q] = [ss, 0]
        self.waited = {e: {} for e in self.eng}
        self.n_inst = 0
        self.dbufs = {}
        self.scope = None
        self.nuniq = 0
        self.deferred = None

    def sb(self, name, shape, dtype):
        es = self.scope if self.scope is not None else self.es
        self.nuniq += 1
        t = es.enter_context(self.nc.sbuf_tensor("sb%d_%s" % (self.nuniq, name), list(shape), dtype))
        return T(t, Buf(name))

    def begin_phase(self):
        self.scope = ExitStack()

    def end_phase(self):
        self.barrier()
        self.scope.close()
        self.scope = None

    def barrier(self):
        toks = [(e, self.cnt[e]) for e in self.cnt if self.cnt[e] > 0]
        for q in self.dq:
            ss, j = self.dq[q]
            for i, k in enumerate(ss):
                n = (j - i + self.NDMASEM - 1) // self.NDMASEM if j > i else 0
                if n > 0:
                    toks.append((k, 16 * n))
        for e in self.eng:
            for tok in toks:
                self._wait(e, tok)

    def ps(self, name, shape, dtype=F32):
        t = self.es.enter_context(self.nc.psum_tensor(name, list(shape), dtype))
        return T(t, Buf(name))

    def dram(self, name, shape, dtype, kind="Internal"):
        return self.nc.dram_tensor(name, list(shape), dtype, kind=kind).ap()

    def db(self, *key):
        b = self.dbufs.get(key)
        if b is None:
            b = Buf(str(key))
            self.dbufs[key] = b
        return b

    def _wait(self, eng, tok):
        k, v = tok
        if self.waited[eng].get(k, 0) >= v:
            return
        self.eng[eng].wait_ge(self.sems[k], v)
        self.waited[eng][k] = v
        self.n_inst += 1

    def _deps(self, eng, reads, writes):
        toks = {}

        def add(tok):
            if tok is None:
                return
            k, v = tok
            if eng == "pe" and k == "pe":
                return
            if toks.get(k, 0) < v:
                toks[k] = v
        for b in reads:
            add(b.w)
        for b in writes:
            add(b.w)
            for k, v in b.r.items():
                add((k, v))
        for k, v in toks.items():
            self._wait(eng, (k, v))

    @staticmethod
    def _mark(tok, reads, writes):
        k, v = tok
        for b in reads:
            if b.r.get(k, 0) < v:
                b.r[k] = v
        for b in writes:
            b.w = tok
            b.r = {}

    @staticmethod
    def _bufs(lst):
        out = []
        for x in lst:
            if x is None:
                continue
            out.append(x.b if isinstance(x, T) else x)
        return out

    def pump(self, thunks, n):
        while n > 0 and thunks:
            f, a, kw = thunks.pop(0)
            f(*a, **kw)
            n -= 1

    def op(self, eng, fn, reads=(), writes=()):
        if self.deferred is not None:
            self.deferred.append((self.op, (eng, fn, list(reads), list(writes)), {}))
            return None
        reads = self._bufs(reads)
        writes = self._bufs(writes)
        self._deps(eng, reads, writes)
        inst = fn()
        self.cnt[eng] += 1
        inst.then_inc(self.sems[eng], 1)
        self._mark((eng, self.cnt[eng]), reads, writes)
        self.n_inst += 1
        return inst

    def mm(self, fns, reads, writes):
        if self.deferred is not None:
            self.deferred.append((self.mm, (list(fns), list(reads), list(writes)), {}))
            return
        reads = self._bufs(reads)
        writes = self._bufs(writes)
        self._deps("pe", reads, writes)
        inst = None
        for f in fns:
            inst = f()
            self.n_inst += 1
        self.cnt["pe"] += 1
        inst.then_inc(self.sems["pe"], 1)
        self._mark(("pe", self.cnt["pe"]), reads, writes)

    def dma(self, q, out, in_, reads=(), writes=(), **kw):
        if self.deferred is not None:
            self.deferred.append((self.dma, (q, out, in_, list(reads), list(writes)), dict(kw)))
            return
        reads = self._bufs(reads)
        writes = self._bufs(writes)
        ss, j = self.dq[q]
        k = ss[j % self.NDMASEM]
        v = 16 * (j // self.NDMASEM + 1)
        self.dq[q][1] = j + 1
        if v > 16:
            self._wait(q, (k, v - 16))
        self._deps(q, reads, writes)
        self.eng[q].dma_start(out=out, in_=in_, **kw).then_inc(self.sems[k], 16)
        self._mark((k, v), reads, writes)
        self.n_inst += 1

    def idma(self, out, in_, out_off=None, in_off=None, bounds=None, reads=(), writes=()):
        q = "pool"
        reads = self._bufs(reads)
        writes = self._bufs(writes)
        ss, j = self.dq[q]
        k = ss[j % self.NDMASEM]
        v = 16 * (j // self.NDMASEM + 1)
        self.dq[q][1] = j + 1
        if v > 16:
            self._wait(q, (k, v - 16))
        self._deps(q, reads, writes)
        self.eng[q].indirect_dma_start(out=out, out_offset=out_off, in_=in_, in_offset=in_off, bounds_check=None,
                                       oob_is_err=False).then_inc(self.sems[k], 16)
        self._mark((k, v), reads, writes)
        self.n_inst += 1

    def finish(self):
        for q in self.dq:
            ss, j = self.dq[q]
            for i, k in enumerate(ss):
                n = (j - i + self.NDMASEM - 1) // self.NDMASEM if j > i else 0
                if n > 0:
                    self._wait("sp", (k, 16 * n))


class Ring:
    def __init__(self, tiles):
        self.tiles = tiles
        self.i = 0

    def next(self):
        t = self.tiles[self.i % len(self.tiles)]
        self.i += 1
        return t


class Cfg:
    def __init__(self, CTX=8192, OWN=4096, stop_after=None, debug=False):
        self.CTX = CTX
        self.OWN = OWN
        self.NBLK = CTX // 256
        self.NTT = OWN // 128
        self.NST = (2 * OWN) // 512 + 7
        self.NSLOT = self.NST * 512
        self.stop_after = stop_after
        self.debug = debug


def _vec_layout():
    names = [("ev_norm_mix", 8), ("ev_norm_ffn", 8), ("pool_scale", 4), ("od_norm_mix", 8), ("od_norm_ffn", 8),
             ("ple_norm0", 8), ("ple_norm1", 8), ("final_norm", 8), ("mla_q_norm", 2), ("mla_kv_norm", 1),
             ("conv_w", 32)]
    off = {}
    o = 0
    for n, c in names:
        off[n] = o
        o += c
    return off, o


VOFF, NV = _vec_layout()


class Ctx:
    pass


def build(cfg):
    nc = bass.Bass("TRN2", target_bir_lowering=False)
    es = ExitStack()
    P = Prog(nc, es)
    CTX, OWN, NBLK = cfg.CTX, cfg.OWN, cfg.NBLK
    NCH = CTX // 512
    g = Ctx()
    g.P, g.nc, g.cfg = P, nc, cfg

    def ein(name, shape, dt=F32):
        return nc.dram_tensor(name, list(shape), dt, kind="ExternalInput").ap()

    def eout(name, shape, dt=F32):
        return nc.dram_tensor(name, list(shape), dt, kind="ExternalOutput").ap()

    I = Ctx()
    g.I = I
    I.xc = ein("xc", [CTX, D])
    I.vecs = ein("vecs", [128, NV])
    I.ev_w_in = ein("ev_w_in", [D, 2048])
    I.pool_w = ein("pool_w", [4, 128, 128])
    I.invc = ein("invc", [4, CTX])
    I.ev_tab = ein("ev_tab", [128, 8])
    I.rowt = ein("rowt", [128, 8])
    I.pastb2 = ein("pastb2", [NBLK, NBLK])
    I.pastb = ein("pastb", [NBLK, NBLK])
    I.alib = ein("alib", [8, CTX // 128])
    I.ev_w_out = ein("ev_w_out", [D, D])
    I.ffn_w_gate = ein("ffn_w_gate", [1, D, 2816])
    I.ffn_w_up = ein("ffn_w_up", [1, D, 2816])
    I.ffn_w_down = ein("ffn_w_down", [1, 2816, D])
    I.ple_w_gate = ein("ple_w_gate", [2, D, D])
    I.ple_w_proj = ein("ple_w_proj", [2, 256, D])
    I.p0c = ein("p0c", [CTX, 256])
    I.p1c = ein("p1c", [OWN, 256])
    I.w1 = ein("w1", [D, 2632])
    I.wuq = ein("wuq", [256, 8 * 96])
    I.mla_w_ukv = ein("mla_w_ukv", [128, 768])
    I.rope = ein("rope", [32, 2, CTX])
    I.bif = ein("bif", [8])
    I.od_w_out = ein("od_w_out", [D, D])
    I.router_w = ein("router_w", [D, 8])
    I.router_b = ein("router_b", [8])
    I.moe_w = ein("moe_w", [8 * 7 * 128, 12288])
    I.widx_base = ein("widx_base", [128, 7])
    I.tilepos = ein("tilepos", [cfg.NST])
    I.hnorm = ein("hnorm", [512])
    I.l1flags = ein("l1flags", [4])
    g.OUT = eout("out", [OWN, D])
    S = Ctx()
    g.S = S
    dbg = cfg.debug
    kind = "ExternalOutput" if dbg else "Internal"
    S.HT = P.dram("HT", [128, 8, CTX], F32, kind)
    S.QT = P.dram("QT", [128, 4, CTX], BF16, kind)
    S.KT = P.dram("KT", [128, 4, CTX], BF16, kind)
    S.VA = P.dram("VA", [CTX // 128, 128, 8 * 65], BF16, kind)
    S.MIXT = P.dram("MIXT", [128, 8, CTX], BF16, kind)
    S.KM = P.dram("KM", [128, 4, NBLK], F32, kind)
    S.KMAX = P.dram("KMAX", [128, 4], F32, kind)
    S.QKC = P.dram("QKC", [128, 8, CTX], BF16, kind)
    S.VC = P.dram("VC", [CTX // 128, 128, 4 * 129], BF16, kind)
    S.OS = P.dram("OS", [CTX // 128, 128, 512], BF16, kind)
    S.IF = P.dram("IF", [CTX // 128, 128, 8], F32, kind)
    S.QM = P.dram("QM", [128, 4, CTX], BF16, kind)
    S.KMT = P.dram("KMT", [128, 4, CTX], BF16, kind)
    S.VM = P.dram("VM", [CTX // 128, 128, 4 * 129], BF16, kind)
    S.KMAX1 = P.dram("KMAX1", [128, 4], F32, kind)
    S.XNT = P.dram("XNT", [OWN, D], BF16, kind)
    S.XS = P.dram("XS", [cfg.NSLOT, D], BF16, kind)
    S.Y = P.dram("Y", [cfg.NSLOT, D], F32, kind)
    S.XN = P.dram("XN", [128, 8, CTX], BF16, kind)
    S.XNF = P.dram("XNF", [128, 8, CTX], F32, kind)

    C = Ctx()
    g.C = C
    C.vecs = P.sb("vecs", [128, NV], F32)
    P.dma("sp", C.vecs[:, :], I.vecs[:, :], writes=[C.vecs])
    C.ones = P.sb("ones", [128, 128], F32)
    P.op("pool", lambda: nc.gpsimd.memset(C.ones[:, :], 1.0), writes=[C.ones])
    C.onesb = P.sb("onesb", [128, 128], BF16)
    P.op("pool", lambda: nc.gpsimd.memset(C.onesb[:, :], 1.0), writes=[C.onesb])
    C.eps = P.sb("eps", [128, 1], F32)
    P.op("pool", lambda: nc.gpsimd.memset(C.eps[:, :], 1e-6), writes=[C.eps])
    C.ident = P.sb("ident", [128, 128], F32)
    P.op("pool", lambda: nc.gpsimd.memset(C.ident[:, :], 1.0), writes=[C.ident])
    P.op("pool", lambda: nc.gpsimd.affine_select(out=C.ident[:, :], in_=C.ident[:, :], pattern=[[-1, 128]],
                                                 compare_op=ALU.is_equal, fill=0.0, base=0, channel_multiplier=1),
         reads=[C.ident], writes=[C.ident])
    C.blk2 = P.sb("blk2", [128, 128], F32)
    P.op("pool", lambda: nc.gpsimd.memset(C.blk2[:, :], 0.0), writes=[C.blk2])
    P.op("pool", lambda: nc.gpsimd.memset(C.blk2[0:64, 0:64], 1.0), reads=[C.blk2], writes=[C.blk2])
    P.op("pool", lambda: nc.gpsimd.memset(C.blk2[64:128, 64:128], 1.0), reads=[C.blk2], writes=[C.blk2])

    g.psf = Ring([P.ps("psf%d" % i, [128, 512], F32) for i in range(4)])
    g.pacc = Ring([P.ps("pacc%d" % i, [128, 512], F32) for i in range(2)])
    g.psb = Ring([P.ps("psb%d" % i, [128, 1024], BF16) for i in range(2)])
    STOP = int(os.environ.get('KSTOP', '99'))

    P.begin_phase()
    phase_l0_a(g)
    P.end_phase()
    if STOP >= 2:
        P.begin_phase()
        phase_l0_b(g)
        P.end_phase()
    if STOP >= 3:
        P.begin_phase()
        phase_resid_norm(g, I.ev_w_out, VOFF["ev_norm_ffn"], 0, CTX)
        P.end_phase()
        P.begin_phase()
        phase_mlp(g, I.ffn_w_gate, I.ffn_w_up, I.ffn_w_down, 1, 2816, 0, CTX, None)
        P.end_phase()
        P.begin_phase()
        phase_ple(g, 0, I.p0c, VOFF["ple_norm0"], 0, CTX, final=False)
        P.end_phase()
    if STOP >= 4:
        P.begin_phase()
        phase_l1_in(g)
        P.end_phase()
    if STOP >= 6:
        full_psf, full_psb = g.psf, g.psb
        P.begin_phase()
        g.psf, g.psb = Ring(full_psf.tiles[3:4]), Ring(full_psb.tiles[1:2])
        P.deferred = []
        phase_mlstm(g)
        g.side = P.deferred
        P.deferred = None
        g.psf, g.psb = Ring(full_psf.tiles[0:3]), Ring(full_psb.tiles[0:1])
        phase_mla(g)
        P.pump(g.side, len(g.side))
        P.end_phase()
        g.psf, g.psb = full_psf, full_psb
    if STOP >= 7:
        T0 = CTX - OWN
        P.begin_phase()
        phase_resid_norm(g, I.od_w_out, VOFF["od_norm_ffn"], T0, OWN, want_f32=True, mixd=True)
        P.end_phase()
        R = Ctx()
        g.R = R
        NTT, NST = cfg.NTT, cfg.NST
        R.s1i = P.sb("R_s1i", [128, NTT], I32)
        R.s2i = P.sb("R_s2i", [128, NTT], I32)
        R.g1 = P.sb("R_g1", [128, NTT], F32)
        R.g2 = P.sb("R_g2", [128, NTT], F32)
        R.widx = P.sb("R_widx", [128, NST * 7], I32)
        P.begin_phase()
        phase_router(g)
        P.end_phase()
        P.begin_phase()
        phase_scatter(g)
        P.end_phase()
        P.begin_phase()
        phase_experts(g)
        P.end_phase()
        P.begin_phase()
        phase_combine(g)
        P.end_phase()
        P.begin_phase()
        phase_ple(g, 1, I.p1c, VOFF["ple_norm1"], T0, OWN, final=True)
        P.end_phase()

    P.finish()
    es.close()
    return nc


def rmsnorm_fm(g, hT, xn, gcol, n, sqr, rstd, tmp, nk=8, dim=D):
    P, nc, C = g.P, g.nc, g.C
    ps = g.psf.next()
    for k in range(nk):
        sq = sqr.next()
        P.op("act", lambda k=k, sq=sq: nc.scalar.activation(out=sq[:, :n], in_=hT[:, k, :n], func=AF.Square),
             reads=[hT], writes=[sq])
        P.mm([lambda k=k, sq=sq: nc.tensor.matmul(ps[:, :n], lhsT=C.onesb[:, :], rhs=sq[:, :n], start=(k == 0),
                                                  stop=(k == nk - 1))], reads=[C.onesb, sq], writes=[ps])
    P.op("act", lambda: nc.scalar.activation(out=tmp[:, :n], in_=ps[:, :n], func=AF.Sqrt, bias=C.eps[:, 0:1],
                                             scale=1.0 / dim), reads=[ps, C.eps], writes=[tmp])
    P.op("dve", lambda: nc.vector.reciprocal(out=rstd[:, :n], in_=tmp[:, :n]), reads=[tmp], writes=[rstd])
    for k in range(nk):
        P.op("dve", lambda k=k: nc.vector.scalar_tensor_tensor(
            out=xn[:, k, :n], in0=hT[:, k, :n], scalar=C.vecs[:, gcol + k:gcol + k + 1], in1=rstd[:, :n],
            op0=ALU.mult, op1=ALU.mult), reads=[hT, rstd, C.vecs], writes=[xn])


def phase_l0_a(g):
    P, nc, C, I, S, cfg = g.P, g.nc, g.C, g.I, g.S, g.cfg
    CTX, NBLK = cfg.CTX, cfg.NBLK
    NCH = CTX // 512
    win = P.sb("a_win", [128, 8, 2048], BF16)
    P.dma("pool", win[:, :, :], I.ev_w_in.rearrange("(k p) f -> p k f", p=128), writes=[win])
    pw = P.sb("a_pw", [128, 4, 128], BF16)
    P.dma("pool", pw[:, :, :], I.pool_w.rearrange("g c d -> c g d"), writes=[pw])
    evt = P.sb("a_evt", [128, 8], F32)
    P.dma("sp", evt[:, :], I.ev_tab[:, :], writes=[evt])
    xtok = Ring([P.sb("a_xtok%d" % i, [128, 4, D], F32) for i in range(2)])
    hT = Ring([P.sb("a_hT%d" % i, [128, 8, 512], F32) for i in range(2)])
    sq = Ring([P.sb("a_sq%d" % i, [128, 512], BF16) for i in range(2)])
    xnr = Ring([P.sb("a_xn%d" % i, [128, 8, 512], BF16) for i in range(2)])
    rstd = P.sb("a_rstd", [128, 512], F32)
    tmp = P.sb("a_tmp", [128, 512], F32)
    qT = Ring([P.sb("a_qT%d" % i, [128, 4, 512], BF16) for i in range(2)])
    kT = Ring([P.sb("a_kT%d" % i, [128, 4, 512], BF16) for i in range(2)])
    ksq = P.sb("a_ksq", [128, 512], F32)
    km = P.sb("a_km", [128, 4, NBLK], F32)
    kmx = P.sb("a_kmx", [128, 4], F32)
    kmx1 = P.sb("a_kmx1", [128, 1], F32)
    P.op("pool", lambda: nc.gpsimd.memset(kmx[:, :], 0.0), writes=[kmx])
    ub = P.sb("a_ub", [128, 4, 528], F32)
    P.op("pool", lambda: nc.gpsimd.memset(ub[:, :, :], 0.0), writes=[ub])
    s_a = P.sb("a_sa", [128, 528], F32)
    s_b = P.sb("a_sb", [128, 528], F32)
    invc = Ring([P.sb("a_invc%d" % i, [128, 4, 512], F32) for i in range(2)])
    pm = P.sb("a_pm", [128, 512], BF16)
    bout = Ring([P.sb("a_bout%d" % i, [128, 4, 512], BF16) for i in range(2)])
    vaug = Ring([P.sb("a_vaug%d" % i, [128, 8, 65], BF16) for i in range(2)])
    vc = VOFF
    STG = int(os.environ.get('KSTAGE', '99'))

    def load(c):
        t = xtok.next()
        P.dma("sp", t[:, :, :], I.xc[c * 512:(c + 1) * 512, :].rearrange("(j p) d -> p j d", p=128), writes=[t])
        iv = invc.next()
        for gi in range(4):
            P.dma("sp", iv[:, gi, :], I.invc[gi:gi + 1, c * 512:(c + 1) * 512].partition_broadcast(128),
                  writes=[iv])
        return t, iv

    nxt = load(0)
    for c in range(NCH):
        xt, iv = nxt
        if c + 1 < NCH:
            nxt = load(c + 1)
        h = hT.next()
        xn = xnr.next()
        for k in range(8):
            ps = g.psf.next()
            P.mm([lambda j=j, k=k: nc.tensor.transpose(out=ps[:, j * 128:(j + 1) * 128],
                                                       in_=xt[:, j, k * 128:(k + 1) * 128], identity=C.ident[:, :])
                  for j in range(4)], reads=[xt, C.ident], writes=[ps])
            P.op("dve", lambda k=k, ps=ps: nc.vector.tensor_copy(out=h[:, k, :], in_=ps[:, :]), reads=[ps], writes=[h])
        P.dma("sp", S.HT[:, :, c * 512:(c + 1) * 512], h[:, :, :], reads=[h], writes=[P.db("HT", c)])
        if STG < 2:
            continue
        rmsnorm_fm(g, h, xn, vc["ev_norm_mix"], 512, sq, rstd, tmp)
        if STG < 3:
            continue
        q = qT.next()
        for fc in range(4):
            ps = g.psf.next()
            P.mm([lambda k=k, fc=fc: nc.tensor.matmul(ps[:, :], lhsT=win[:, k, fc * 128:(fc + 1) * 128],
                                                      rhs=xn[:, k, :], start=(k == 0), stop=(k == 7))
                  for k in range(8)], reads=[win, xn], writes=[ps])
            P.op("act", lambda fc=fc, ps=ps: nc.scalar.mul(out=q[:, fc, :], in_=ps[:, :], mul=0.125),
                 reads=[ps], writes=[q])
        P.dma("sp", S.QT[:, :, c * 512:(c + 1) * 512], q[:, :, :], reads=[q], writes=[P.db("QT", c)])
        if STG < 4:
            continue
        kk = kT.next()
        for fc in range(4):
            ps = g.psf.next()
            P.mm([lambda k=k, fc=fc: nc.tensor.matmul(ps[:, :], lhsT=win[:, k, 512 + fc * 128:512 + (fc + 1) * 128],
                                                      rhs=xn[:, k, :], start=(k == 0), stop=(k == 7))
                  for k in range(8)], reads=[win, xn], writes=[ps])
            P.op("act", lambda fc=fc, ps=ps: nc.scalar.copy(out=kk[:, fc, :], in_=ps[:, :]), reads=[ps], writes=[kk])
            SUB = int(os.environ.get('KSUB', '99'))
            if SUB < 2:
                continue
            for bb in range(2):
                P.op("dve", lambda fc=fc, ps=ps, bb=bb: nc.vector.reduce_sum(
                    out=km[:, fc, 2 * c + bb:2 * c + bb + 1], in_=kk[:, fc, bb * 256:(bb + 1) * 256], axis=AX.X),
                    reads=[kk], writes=[km])
            if SUB < 3:
                continue
            P.op("act", lambda ps=ps: nc.scalar.activation(out=ksq[:, :], in_=ps[:, :], func=AF.Square),
                 reads=[ps], writes=[ksq])
            if SUB < 4:
                continue
            ps2 = g.psf.next()
            P.mm([lambda: nc.tensor.matmul(ps2[:, :], lhsT=C.blk2[:, :], rhs=ksq[:, :], start=True, stop=True)],
                 reads=[C.blk2, ksq], writes=[ps2])
            if SUB < 5:
                continue
            P.op("dve", lambda ps2=ps2: nc.vector.reduce_max(out=kmx1[:, :], in_=ps2[:, :], axis=AX.X),
                 reads=[ps2], writes=[kmx1])
            if SUB < 6:
                continue
            P.op("dve", lambda fc=fc: nc.vector.tensor_max(out=kmx[:, fc:fc + 1], in0=kmx[:, fc:fc + 1], in1=kmx1[:, :]),
                 reads=[kmx, kmx1], writes=[kmx])
        P.dma("sp", S.KT[:, :, c * 512:(c + 1) * 512], kk[:, :, :], reads=[kk], writes=[P.db("KT", c)])
        if STG < 5:
            continue
        for j in range(4):
            ps = g.psf.next()
            P.mm([lambda k=k, j=j: nc.tensor.matmul(ps[:, :], lhsT=xn[:, k, j * 128:(j + 1) * 128],
                                                    rhs=win[:, k, 1024:1536], start=(k == 0), stop=(k == 7))
                  for k in range(8)], reads=[win, xn], writes=[ps])
            va = vaug.next()
            par = j % 2
            P.op("dve", lambda ps=ps, va=va, par=par: nc.vector.tensor_tensor(
                out=va[:, :, 0:64], in0=ps[:, :].rearrange("p (h d) -> p h d", h=8),
                in1=evt[:, :].unsqueeze(2).broadcast_to([128, 8, 64]), op=ALU.mult),
                reads=[ps, evt], writes=[va])
            P.op("act", lambda va=va, par=par: nc.scalar.copy(out=va[:, :, 64:65], in_=evt[:, :].unsqueeze(2)),
                 reads=[evt, va], writes=[va])
            P.dma("sp", S.VA[c * 4 + j, :, :], va[:, :, :].rearrange("p h d -> p (h d)"), reads=[va],
                  writes=[P.db("VA", c * 4 + j)])
        if STG < 6:
            continue
        bo = bout.next()
        for gi in range(4):
            ps = g.psf.next()
            P.mm([lambda k=k, gi=gi: nc.tensor.matmul(ps[:, :], lhsT=win[:, k, 1536 + gi * 128:1536 + (gi + 1) * 128],
                                                      rhs=xn[:, k, :], start=(k == 0), stop=(k == 7))
                  for k in range(8)], reads=[win, xn], writes=[ps])
            P.op("act", lambda gi=gi, ps=ps: nc.scalar.copy(out=ub[:, gi, 16:528], in_=ps[:, :]), reads=[ps], writes=[ub])
            src = None
            cur, oth = s_a, s_b
            for st in range(gi + 1):
                sh = 1 << st
                if st == 0:
                    P.op("dve", lambda gi=gi, cur=cur, sh=sh: nc.vector.tensor_add(
                        out=cur[:, sh:528], in0=ub[:, gi, sh:528], in1=ub[:, gi, 0:528 - sh]),
                        reads=[ub], writes=[cur])
                else:
                    v = 2 * sh - 1
                    P.op("dve", lambda cur=cur, oth=oth, sh=sh, v=v: nc.vector.tensor_add(
                        out=oth[:, v:528], in0=cur[:, v:528], in1=cur[:, v - sh:528 - sh]),
                        reads=[cur], writes=[oth])
                    cur, oth = oth, cur
            P.op("dve", lambda gi=gi, cur=cur, oth=oth: nc.vector.tensor_mul(
                out=oth[:, 16:528], in0=cur[:, 16:528], in1=iv[:, gi, :]), reads=[cur, iv], writes=[oth])
            P.op("dve", lambda gi=gi, oth=oth: nc.vector.tensor_sub(
                out=pm[:, :], in0=oth[:, 16:528], in1=ub[:, gi, 16:528]), reads=[oth, ub], writes=[pm])
            P.op("pool", lambda gi=gi: nc.gpsimd.tensor_copy(out=ub[:, gi, 0:16], in_=ub[:, gi, 512:528]),
                 reads=[ub], writes=[ub])
            ps3 = g.psf.next()
            P.mm([lambda gi=gi, ps3=ps3: nc.tensor.matmul(ps3[:, :], lhsT=pw[:, gi, :], rhs=pm[:, :], start=True, stop=True)],
                 reads=[pw, pm], writes=[ps3])
            P.op("act", lambda gi=gi, ps3=ps3: nc.scalar.mul(out=bo[:, gi, :], in_=ps3[:, :],
                                                             mul=C.vecs[:, vc["pool_scale"] + gi:vc["pool_scale"] + gi + 1]),
                 reads=[ps3, C.vecs], writes=[bo])
        P.dma("sp", S.MIXT[:, 4:8, c * 512:(c + 1) * 512], bo[:, :, :], reads=[bo], writes=[P.db("MIXT_B", c)])
    P.op("dve", lambda: nc.vector.tensor_scalar_mul(out=km[:, :, :], in0=km[:, :, :], scalar1=1.0 / 256.0),
         reads=[km], writes=[km])
    P.dma("sp", S.KM[:, :, :], km[:, :, :], reads=[km], writes=[P.db("KM")])
    P.dma("sp", S.KMAX[:, :], kmx[:, :], reads=[kmx], writes=[P.db("KMAX")])


def phase_l0_b(g):
    P, nc, C, I, S, cfg = g.P, g.nc, g.C, g.I, g.S, g.cfg
    CTX, NBLK = cfg.CTX, cfg.NBLK
    NT = CTX // 128
    NB8 = max(NBLK, 8)
    KT = P.sb("b_KT", [128, 4, CTX], BF16)
    NCH = CTX // 512
    kbufs = [Buf("KTc%d" % c) for c in range(NCH)]
    for c in range(NCH):
        P.dma("sp", KT[:, :, c * 512:(c + 1) * 512], S.KT[:, :, c * 512:(c + 1) * 512], reads=[P.db("KT", c)],
              writes=[kbufs[c]])
    VA = P.sb("b_VA", [128, NT, 520], BF16)
    vbufs = [Buf("VAt%d" % t) for t in range(NT)]
    for t in range(NT):
        P.dma("sp", VA[:, t, :], S.VA[t, :, :], reads=[P.db("VA", t)], writes=[vbufs[t]])
    kmf = P.sb("b_kmf", [128, 4, NBLK], F32)
    P.dma("sp", kmf[:, :, :], S.KM[:, :, :], reads=[P.db("KM")], writes=[kmf])
    kmb = P.sb("b_kmb", [128, 4, 2, NBLK], BF16)
    P.op("dve", lambda: nc.vector.memset(kmb[:, :, :, :], 0.0), writes=[kmb])
    P.op("dve", lambda: nc.vector.tensor_copy(out=kmb[0:64, :, 0, :], in_=kmf[0:64, :, :]), reads=[kmf, kmb], writes=[kmb])
    P.op("dve", lambda: nc.vector.tensor_copy(out=kmb[64:128, :, 1, :], in_=kmf[64:128, :, :]), reads=[kmf, kmb], writes=[kmb])
    kmx = P.sb("b_kmx", [128, 4], F32)
    P.dma("sp", kmx[:, :], S.KMAX[:, :], reads=[P.db("KMAX")], writes=[kmx])
    pastb = P.sb("b_pastb", [128, NBLK, NBLK], F32)
    P.dma("sp", pastb[:, :, :].rearrange("p a b -> p (a b)"),
          I.pastb.rearrange("a b -> (a b)").partition_broadcast(128),
          writes=[pastb])
    alib = P.sb("b_alib", [128, 8, NT], F32)
    P.dma("sp", alib[:, :, :].rearrange("p a b -> p (a b)"),
          I.alib.rearrange("a b -> (a b)").partition_broadcast(128),
          writes=[alib])
    pastb2 = P.sb("b_pastb2", [128, NBLK, NBLK], F32)
    P.dma("sp", pastb2[:, :, :].rearrange("p a b -> p (a b)"),
          I.pastb2.rearrange("a b -> (a b)").partition_broadcast(128), writes=[pastb2])
    rowt = P.sb("b_rowt", [128, 8], F32)
    P.dma("sp", rowt[:, :], I.rowt[:, :], writes=[rowt])
    rowc = P.sb("b_rowc", [128, 8], F32)
    e0 = P.sb("b_e0", [128, 2, 128], F32)
    P.op("pool", lambda: nc.gpsimd.memset(e0[:, :, :], 0.0), writes=[e0])
    P.op("pool", lambda: nc.gpsimd.memset(e0[0:1, 0, :], 1.0), reads=[e0], writes=[e0])
    P.op("pool", lambda: nc.gpsimd.memset(e0[64:65, 1, :], 1.0), reads=[e0], writes=[e0])
    hsel = P.sb("b_hsel", [128, 2], BF16)
    P.op("pool", lambda: nc.gpsimd.memset(hsel[:, :], 0.0), writes=[hsel])
    P.op("pool", lambda: nc.gpsimd.memset(hsel[0:64, 0:1], 1.0), reads=[hsel], writes=[hsel])
    P.op("pool", lambda: nc.gpsimd.memset(hsel[64:128, 1:2], 1.0), reads=[hsel], writes=[hsel])
    tri = P.sb("b_tri", [128, 128], BF16)
    P.op("pool", lambda: nc.gpsimd.memset(tri[:, :], 1.0), writes=[tri])
    P.op("pool", lambda: nc.gpsimd.affine_select(out=tri[:, :], in_=tri[:, :], pattern=[[-1, 128]],
                                                 compare_op=ALU.is_ge, fill=0.0, base=0, channel_multiplier=1),
         reads=[tri], writes=[tri])
    identb = P.sb("b_identb", [128, 128], BF16)
    P.op("dve", lambda: nc.vector.tensor_copy(out=identb[:, :], in_=C.ident[:, :]), reads=[C.ident], writes=[identb])
    kx = P.sb("b_kx", [128, 8], F32)
    ps = g.psf.next()
    for par in range(2):
        P.mm([lambda par=par: nc.tensor.matmul(ps[:, par * 4:par * 4 + 4], lhsT=e0[:, par, :], rhs=kmx[:, :],
                                               start=True, stop=True)], reads=[e0, kmx], writes=[ps])
    P.op("dve", lambda: nc.vector.tensor_copy(out=kx[:, :].rearrange("p (hp par) -> p par hp", par=2),
                                              in_=ps[:, 0:8].rearrange("p (par hp) -> p par hp", par=2)),
         reads=[ps], writes=[kx])

    qT = Ring([P.sb("b_qT%d" % i, [128, 4, 128], BF16) for i in range(2)])
    qsq = P.sb("b_qsq", [128, 4, 128], BF16)
    gs = P.sb("b_gs", [128, 8, NB8], F32)
    P.op("pool", lambda: nc.gpsimd.memset(gs[:, :, :], -3.0e30), writes=[gs])
    top8 = P.sb("b_top8", [128, 8, 8], F32)
    sel = P.sb("b_sel", [128, 8, NBLK], F32)
    bq = Ring([P.sb("b_bq%d" % i, [128, 8, 2 * NBLK], F32) for i in range(2)])
    fq = Ring([P.sb("b_fq%d" % i, [128, 8, 2 * NBLK], F32) for i in range(2)])
    mb = Ring([P.sb("b_mb%d" % i, [128, 8], F32) for i in range(2)])
    nmb = Ring([P.sb("b_nmb%d" % i, [128, 8], F32) for i in range(2)])
    qn2 = P.sb("b_qn2", [128, 8], F32)
    Pp = Ring([P.sb("b_Pp%d" % i, [128, 1024], BF16) for i in range(6)])
    PT = Ring([P.sb("b_PT%d" % i, [128, 8, 128], BF16) for i in range(4)])
    atok = Ring([P.sb("b_atok%d" % i, [128, 512], BF16) for i in range(2)])
    aT = Ring([P.sb("b_aT%d" % i, [128, 4, 128], BF16) for i in range(2)])
    rden = P.sb("b_rden", [128, 1], F32)

    def loadq(i):
        t = qT.next()
        P.dma("sp", t[:, :, :], S.QT[:, :, i * 128:(i + 1) * 128], reads=[P.db("QT", i // 4)], writes=[t])
        return t

    nq = loadq(0)
    NTL = min(NT, int(os.environ.get('KNT', '9999')))
    for i in range(NTL):
        q = nq
        if i + 1 < NT:
            nq = loadq(i + 1)
        ob, par = i // 2, i % 2
        P.op("act", lambda q=q: nc.scalar.activation(out=qsq[:, :, :], in_=q[:, :, :], func=AF.Square),
             reads=[q], writes=[qsq])
        ps = g.psf.next()
        for hp in range(4):
            P.mm([lambda hp=hp: nc.tensor.matmul(ps[:, 2 * hp:2 * hp + 2], lhsT=qsq[:, hp, :], rhs=hsel[:, :],
                                                 start=True, stop=True)], reads=[qsq, hsel], writes=[ps])
        m_ = mb.next()
        P.op("dve", lambda ps=ps: nc.vector.tensor_mul(out=qn2[:, :], in0=ps[:, 0:8], in1=kx[:, :]),
             reads=[ps, kx], writes=[qn2])
        P.op("act", lambda m_=m_: nc.scalar.activation(out=m_[:, :], in_=qn2[:, :], func=AF.Sqrt),
             reads=[qn2], writes=[m_])
        b_ = bq.next()
        if ob > 0:
            if int(os.environ.get('KGS', '99')) < 1:
                continue
            psg = g.psf.next()
            for hp in range(4):
                P.mm([lambda hp=hp: nc.tensor.matmul(
                    psg[:, hp * 2 * NBLK:(hp + 1) * 2 * NBLK], lhsT=q[:, hp, :],
                    rhs=kmb[:, hp, :, :].rearrange("p a b -> p (a b)"), start=True, stop=True)],
                    reads=[q, kmb], writes=[psg])
            GS = int(os.environ.get('KGS', '99'))
            if GS < 2:
                continue
            P.op("dve", lambda psg=psg, ob=ob: nc.vector.tensor_tensor(
                out=gs[:, :, 0:NBLK], in0=psg[:, 0:8 * NBLK].rearrange("p (h n) -> p h n", h=8),
                in1=pastb[:, ob, :].unsqueeze(1).broadcast_to([128, 8, NBLK]), op=ALU.add),
                reads=[psg, pastb], writes=[gs])
            if GS < 3:
                continue
            for h in range(8):
                P.op("dve", lambda h=h: nc.vector.max(out=top8[:, h, :], in_=gs[:, h, :]), reads=[gs], writes=[top8])
            if GS < 4:
                continue
            P.op("dve", lambda: nc.vector.tensor_tensor(
                out=sel[:, :, :], in0=gs[:, :, 0:NBLK], in1=top8[:, :, 2:3].broadcast_to([128, 8, NBLK]), op=ALU.is_ge),
                reads=[gs, top8], writes=[sel])
            P.op("dve", lambda: nc.vector.tensor_scalar(out=sel[:, :, :], in0=sel[:, :, :], scalar1=-1.0, scalar2=1.0e30,
                                                        op0=ALU.add, op1=ALU.mult), reads=[sel], writes=[sel])
            P.op("dve", lambda ob=ob: nc.vector.memset(sel[:, :, ob:ob + 1], 0.0), reads=[sel], writes=[sel])
        else:
            P.op("dve", lambda: nc.vector.memset(sel[:, :, :], 0.0), reads=[sel], writes=[sel])
        P.op("dve", lambda ob=ob: nc.vector.tensor_tensor(
            out=sel[:, :, :], in0=sel[:, :, :], in1=pastb2[:, ob, :].unsqueeze(1).broadcast_to([128, 8, NBLK]),
            op=ALU.add), reads=[sel, pastb2], writes=[sel])
        nm_ = nmb.next()
        P.op("dve", lambda m_=m_, nm_=nm_: nc.vector.tensor_scalar_mul(out=nm_[:, :], in0=m_[:, :], scalar1=-1.0),
             reads=[m_], writes=[nm_])
        P.op("dve", lambda i=i: nc.vector.tensor_sub(out=rowc[:, :], in0=rowt[:, :], in1=alib[:, :, i]),
             reads=[rowt, alib], writes=[rowc])
        P.op("dve", lambda b_=b_: nc.vector.tensor_tensor(
            out=b_[:, :, :].rearrange("p h (n two) -> p h n two", two=2),
            in0=alib[:, :, :].rearrange("p h (n two) -> p h n two", two=2),
            in1=sel[:, :, :].unsqueeze(3).broadcast_to([128, 8, NBLK, 2]), op=ALU.add),
            reads=[sel, alib], writes=[b_])
        P.op("dve", lambda b_=b_: nc.vector.tensor_tensor(
            out=b_[:, :, :], in0=b_[:, :, :], in1=rowc[:, :].unsqueeze(2).broadcast_to([128, 8, 2 * NBLK]), op=ALU.add),
            reads=[b_, rowc], writes=[b_])
        nkt = i + 1
        f_ = fq.next()
        P.op("act", lambda b_=b_, f_=f_, nkt=nkt: nc.scalar.activation(out=f_[:, :, 0:nkt], in_=b_[:, :, 0:nkt], func=AF.Exp),
             reads=[b_], writes=[f_])
        ngr = (nkt + 7) // 8
        items = [(h, gi) for h in range(8) for gi in range(ngr)]
        at = atok.next()
        st = {}

        def stage_s(h, gi):
            hp, p2 = h // 2, h % 2
            k0 = gi * 8
            k1 = min(nkt, k0 + 8)
            pp = Pp.next()
            st[(h, gi)] = [pp, None]
            for half in range(2):
                a0 = k0 + half * 4
                a1 = min(k1, a0 + 4)
                if a1 <= a0:
                    continue
                w = (a1 - a0) * 128
                ps = g.psf.next()
                kb = [kbufs[c] for c in range(a0 // 4, (a1 - 1) // 4 + 1)]
                P.mm([lambda ps=ps, a0=a0, w=w, hp=hp, p2=p2: nc.tensor.matmul(
                    ps[:, 0:w], lhsT=q[64 * p2:64 * p2 + 64, hp, :],
                    rhs=KT[64 * p2:64 * p2 + 64, hp, a0 * 128:a0 * 128 + w], start=True, stop=True)],
                    reads=[q] + kb, writes=[ps])
                o0 = (a0 - k0) * 128
                P.op("act", lambda ps=ps, pp=pp, o0=o0, w=w: nc.scalar.activation(
                    out=pp[:, o0:o0 + w], in_=ps[:, 0:w], func=AF.Exp, bias=nm_[:, h:h + 1], scale=1.0),
                    reads=[ps, nm_], writes=[pp])
            nk = k1 - k0
            P.op("dve", lambda pp=pp, nk=nk, k0=k0: nc.vector.tensor_tensor(
                out=pp[:, 0:nk * 128].rearrange("p (a b) -> p a b", b=128),
                in0=pp[:, 0:nk * 128].rearrange("p (a b) -> p a b", b=128),
                in1=f_[:, h, k0:k0 + nk].unsqueeze(2).broadcast_to([128, nk, 128]), op=ALU.mult),
                reads=[pp, f_], writes=[pp])
            if k1 == nkt:
                o0 = (nkt - 1 - k0) * 128
                P.op("pool", lambda pp=pp, o0=o0: nc.gpsimd.tensor_mul(out=pp[:, o0:o0 + 128], in0=pp[:, o0:o0 + 128],
                                                                      in1=tri[:, :]), reads=[pp, tri], writes=[pp])

        def stage_t(h, gi):
            k0 = gi * 8
            k1 = min(nkt, k0 + 8)
            pp = st[(h, gi)][0]
            pb = g.psb.next()
            P.mm([lambda j=j, pb=pb, pp=pp: nc.tensor.transpose(out=pb[:, j * 128:(j + 1) * 128],
                                                                in_=pp[:, j * 128:(j + 1) * 128], identity=identb[:, :])
                  for j in range(k1 - k0)], reads=[pp, identb], writes=[pb])
            pt = PT.next()
            st[(h, gi)][1] = pt
            w = (k1 - k0) * 128
            if (h + gi) % 2 == 0:
                P.op("dve", lambda pb=pb, pt=pt, w=w: nc.vector.tensor_copy(
                    out=pt[:, :, :].rearrange("p a b -> p (a b)")[:, 0:w], in_=pb[:, 0:w]), reads=[pb], writes=[pt])
            else:
                P.op("act", lambda pb=pb, pt=pt, w=w: nc.scalar.copy(
                    out=pt[:, :, :].rearrange("p a b -> p (a b)")[:, 0:w], in_=pb[:, 0:w]), reads=[pb], writes=[pt])

        acc = {}

        def stage_v(h, gi):
            k0 = gi * 8
            k1 = min(nkt, k0 + 8)
            pt = st[(h, gi)][1]
            if gi == 0:
                acc[h] = g.pacc.next()
            pa = acc[h]
            vb = [vbufs[t] for t in range(k0, k1)]
            fns = [lambda j=j, pa=pa, pt=pt, k0=k0, h=h: nc.tensor.matmul(
                pa[:, 0:65], lhsT=pt[:, j, :], rhs=VA[:, k0 + j, h * 65:(h + 1) * 65],
                start=(k0 + j == 0), stop=(k0 + j == nkt - 1)) for j in range(k1 - k0)]
            P.mm(fns, reads=[pt] + vb, writes=[pa])
            if k1 == nkt:
                P.op("dve", lambda pa=pa: nc.vector.reciprocal(out=rden[:, :], in_=pa[:, 64:65]), reads=[pa], writes=[rden])
                P.op("act", lambda pa=pa, h=h: nc.scalar.mul(out=at[:, h * 64:(h + 1) * 64], in_=pa[:, 0:64], mul=rden[:, 0:1]),
                     reads=[pa, rden], writes=[at])
            del st[(h, gi)]

        n_it = len(items)
        for z in range(n_it + 3):
            if z < n_it:
                stage_s(*items[z])
            if 0 <= z - 2 < n_it:
                stage_t(*items[z - 2])
            if 0 <= z - 3 < n_it:
                stage_v(*items[z - 3])
        pb = g.psb.next()
        P.mm([lambda j=j, pb=pb: nc.tensor.transpose(out=pb[:, j * 128:(j + 1) * 128], in_=at[:, j * 128:(j + 1) * 128],
                                                     identity=identb[:, :]) for j in range(4)],
             reads=[at, identb], writes=[pb])
        a_ = aT.next()
        P.op("dve", lambda pb=pb, a_=a_: nc.vector.tensor_copy(out=a_[:, :, :].rearrange("p a b -> p (a b)"), in_=pb[:, 0:512]),
             reads=[pb], writes=[a_])
        P.dma("sp", S.MIXT[:, 0:4, i * 128:(i + 1) * 128], a_[:, :, :], reads=[a_], writes=[P.db("MIXT_A", i)])


def phase_resid_norm(g, w_out, gcol, t0, n_tok, want_f32=False, mixd=False):
    P, nc, C, I, S, cfg = g.P, g.nc, g.C, g.I, g.S, g.cfg
    wo = P.sb("c_wo", [128, 8, D], BF16)
    P.dma("pool", wo[:, :, :], w_out.rearrange("(k p) f -> p k f", p=128), writes=[wo])
    hT = Ring([P.sb("c_hT%d" % i, [128, 8, 512], F32) for i in range(2)])
    mx = Ring([P.sb("c_mx%d" % i, [128, 8, 512], BF16) for i in range(2)])
    xn = Ring([P.sb("c_xn%d" % i, [128, 8, 512], BF16) for i in range(2)])
    xnf = Ring([P.sb("c_xnf%d" % i, [128, 8, 512], F32) for i in range(2)]) if want_f32 else None
    sq = Ring([P.sb("c_sq%d" % i, [128, 512], BF16) for i in range(2)])
    rstd = P.sb("c_rstd", [128, 512], F32)
    tmp = P.sb("c_tmp", [128, 512], F32)
    nch = n_tok // 512
    if want_f32:
        identb = P.sb("c_identb", [128, 128], BF16)
        P.op("dve", lambda: nc.vector.tensor_copy(out=identb[:, :], in_=C.ident[:, :]), reads=[C.ident], writes=[identb])
        xtm = Ring([P.sb("c_xtm%d" % i, [128, D], BF16) for i in range(3)])

    def load(c):
        a = t0 + c * 512
        h = hT.next()
        P.dma("sp", h[:, :, :], S.HT[:, :, a:a + 512], reads=[P.db("HT", a // 512)], writes=[h])
        m = mx.next()
        rd = [P.db("MIXT_B", a // 512)] + [P.db("MIXT_A", a // 128 + j) for j in range(4)]
        if mixd:
            rd += [P.db("MIXT_D", a // 128 + j) for j in range(4)]
        P.dma("sp", m[:, :, :], S.MIXT[:, :, a:a + 512], reads=rd, writes=[m])
        return h, m

    nxt = load(0)
    for c in range(nch):
        h, m = nxt
        if c + 1 < nch:
            nxt = load(c + 1)
        a = t0 + c * 512
        for d in range(8):
            ps = g.psf.next()
            P.mm([lambda k=k, d=d, ps=ps: nc.tensor.matmul(ps[:, :], lhsT=wo[:, k, d * 128:(d + 1) * 128], rhs=m[:, k, :],
                                                          start=(k == 0), stop=(k == 7)) for k in range(8)],
                 reads=[wo, m], writes=[ps])
            P.op("dve", lambda d=d, ps=ps: nc.vector.tensor_add(out=h[:, d, :], in0=ps[:, :], in1=h[:, d, :]),
                 reads=[ps, h], writes=[h])
        P.dma("sp", S.HT[:, :, a:a + 512], h[:, :, :], reads=[h], writes=[P.db("HT", a // 512)])
        x_ = xn.next()
        if want_f32:
            xf = xnf.next()
            rmsnorm_fm(g, h, xf, gcol, 512, sq, rstd, tmp)
            P.op("act", lambda xf=xf, x_=x_: nc.scalar.copy(out=x_[:, :, :], in_=xf[:, :, :]), reads=[xf], writes=[x_])
            P.dma("sp", S.XNF[:, :, a:a + 512], xf[:, :, :], reads=[xf], writes=[P.db("XNF", a // 512)])
            for jj in range(4):
                pb = g.psb.next()
                P.mm([lambda k=k, jj=jj, pb=pb, x_=x_: nc.tensor.transpose(
                    out=pb[:, k * 128:(k + 1) * 128], in_=x_[:, k, jj * 128:(jj + 1) * 128], identity=identb[:, :])
                    for k in range(8)], reads=[x_, identb], writes=[pb])
                xt_ = xtm.next()
                P.op("act", lambda pb=pb, xt_=xt_: nc.scalar.copy(out=xt_[:, :], in_=pb[:, 0:1024]), reads=[pb], writes=[xt_])
                r0 = a - t0 + jj * 128
                P.dma("sp", S.XNT[r0:r0 + 128, :], xt_[:, :], reads=[xt_], writes=[P.db("XNT", r0 // 128)])
        else:
            rmsnorm_fm(g, h, x_, gcol, 512, sq, rstd, tmp)
        P.dma("sp", S.XN[:, :, a:a + 512], x_[:, :, :], reads=[x_], writes=[P.db("XN", a // 512)])


def phase_mlp(g, wg_all, wu_all, wd_all, n_exp, F, t0, n_tok, gates):
    P, nc, C, I, S, cfg = g.P, g.nc, g.C, g.I, g.S, g.cfg
    TS = min(2048, n_tok)
    NTC = TS // 512
    yacc = P.sb("m_yacc", [128, 8, TS], F32)
    xn = P.sb("m_xn", [128, 8, TS], BF16)
    wgr = Ring([P.sb("m_wg%d" % i, [128, 8, 512], BF16) for i in range(2)])
    wur = Ring([P.sb("m_wu%d" % i, [128, 8, 512], BF16) for i in range(2)])
    wdr = Ring([P.sb("m_wd%d" % i, [128, 4, D], BF16) for i in range(2)])
    t1r = Ring([P.sb("m_t1%d" % i, [128, 512], BF16) for i in range(3)])
    actr = Ring([P.sb("m_act%d" % i, [128, 4, 512], BF16) for i in range(2)])
    ger = Ring([P.sb("m_ge%d" % i, [128, TS], BF16) for i in range(2)]) if gates is not None else None
    fbs = []
    f = 0
    while f < F:
        fbs.append((f, min(512, F - f)))
        f += 512
    work = [(e, f0, fw) for e in range(n_exp) for (f0, fw) in fbs]

    def loadw(e, f0, fw):
        wg, wu, wd = wgr.next(), wur.next(), wdr.next()
        P.dma("pool", wg[:, :, 0:fw], wg_all[e, :, f0:f0 + fw].rearrange("(k p) f -> p k f", p=128), writes=[wg])
        P.dma("pool", wu[:, :, 0:fw], wu_all[e, :, f0:f0 + fw].rearrange("(k p) f -> p k f", p=128), writes=[wu])
        P.dma("pool", wd[:, 0:fw // 128, :], wd_all[e, f0:f0 + fw, :].rearrange("(j p) d -> p j d", p=128), writes=[wd])
        return wg, wu, wd

    for sc in range(n_tok // TS):
        a = t0 + sc * TS
        for c in range(NTC):
            P.dma("sp", yacc[:, :, c * 512:(c + 1) * 512], S.HT[:, :, a + c * 512:a + (c + 1) * 512],
                  reads=[P.db("HT", (a + c * 512) // 512)], writes=[yacc])
            P.dma("sp", xn[:, :, c * 512:(c + 1) * 512], S.XN[:, :, a + c * 512:a + (c + 1) * 512],
                  reads=[P.db("XN", (a + c * 512) // 512)], writes=[xn])
        nw = loadw(*work[0])
        ge = None
        for wi, (e, f0, fw) in enumerate(work):
            wg, wu, wd = nw
            if wi + 1 < len(work):
                nw = loadw(*work[wi + 1])
            nj = fw // 128
            if gates is not None and (wi == 0 or work[wi - 1][0] != e):
                ge = ger.next()
                o = a - t0
                P.dma("sp", ge[:, :], gates[e, :, o:o + TS], reads=[P.db("GATES")], writes=[ge])

            def up_stage(tc):
                act = actr.next()
                for j in range(nj):
                    psg = g.psf.next()
                    P.mm([lambda k=k, j=j, psg=psg: nc.tensor.matmul(
                        psg[:, :], lhsT=wg[:, k, j * 128:(j + 1) * 128], rhs=xn[:, k, tc * 512:(tc + 1) * 512],
                        start=(k == 0), stop=(k == 7)) for k in range(8)], reads=[wg, xn], writes=[psg])
                    psu = g.psf.next()
                    P.mm([lambda k=k, j=j, psu=psu: nc.tensor.matmul(
                        psu[:, :], lhsT=wu[:, k, j * 128:(j + 1) * 128], rhs=xn[:, k, tc * 512:(tc + 1) * 512],
                        start=(k == 0), stop=(k == 7)) for k in range(8)], reads=[wu, xn], writes=[psu])
                    t1 = t1r.next()
                    P.op("act", lambda psg=psg, t1=t1: nc.scalar.activation(out=t1[:, :], in_=psg[:, :], func=AF.Silu),
                         reads=[psg], writes=[t1])
                    P.op("dve", lambda psu=psu, t1=t1, act=act, j=j: nc.vector.tensor_mul(
                        out=act[:, j, :], in0=psu[:, :], in1=t1[:, :]), reads=[psu, t1], writes=[act])
                    if ge is not None:
                        P.op("pool", lambda act=act, j=j, ge=ge: nc.gpsimd.tensor_mul(
                            out=act[:, j, :], in0=act[:, j, :], in1=ge[:, tc * 512:(tc + 1) * 512]),
                            reads=[act, ge], writes=[act])
                return act

            def down_stage(tc, act):
                for d in range(8):
                    pd = g.pacc.next()
                    P.mm([lambda j=j, d=d, pd=pd: nc.tensor.matmul(
                        pd[:, :], lhsT=wd[:, j, d * 128:(d + 1) * 128], rhs=act[:, j, :],
                        start=(j == 0), stop=(j == nj - 1)) for j in range(nj)], reads=[wd, act], writes=[pd])
                    P.op("dve", lambda d=d, pd=pd: nc.vector.tensor_add(
                        out=yacc[:, d, tc * 512:(tc + 1) * 512], in0=pd[:, :], in1=yacc[:, d, tc * 512:(tc + 1) * 512]),
                        reads=[pd, yacc], writes=[yacc])

            prev = None
            for tc in range(NTC):
                act = up_stage(tc)
                if prev is not None:
                    down_stage(*prev)
                prev = (tc, act)
            down_stage(*prev)
        for c in range(NTC):
            P.dma("sp", S.HT[:, :, a + c * 512:a + (c + 1) * 512], yacc[:, :, c * 512:(c + 1) * 512],
                  reads=[yacc], writes=[P.db("HT", (a + c * 512) // 512)])


def phase_ple(g, layer, p_in, gcol, t0, n_tok, final):
    P, nc, C, I, S, cfg = g.P, g.nc, g.C, g.I, g.S, g.cfg
    wgt = P.sb("p_wg", [128, 8, D], BF16)
    P.dma("pool", wgt[:, :, :], I.ple_w_gate[layer].rearrange("(k p) f -> p k f", p=128), writes=[wgt])
    wpj = P.sb("p_wp", [128, 2, D], BF16)
    P.dma("pool", wpj[:, :, :], I.ple_w_proj[layer].rearrange("(k p) f -> p k f", p=128), writes=[wpj])
    hT = Ring([P.sb("p_hT%d" % i, [128, 8, 512], F32) for i in range(2)])
    pt = Ring([P.sb("p_pt%d" % i, [128, 4, 256], F32) for i in range(2)])
    pT = P.sb("p_pT", [128, 2, 512], BF16)
    xnr = Ring([P.sb("p_xn%d" % i, [128, 8, 512], BF16) for i in range(2)])
    sq = Ring([P.sb("p_sq%d" % i, [128, 512], BF16) for i in range(2)])
    rstd = P.sb("p_rstd", [128, 512], F32)
    tmp = P.sb("p_tmp", [128, 512], F32)
    sg = Ring([P.sb("p_sg%d" % i, [128, 512], F32) for i in range(2)])
    if final:
        xo = P.sb("p_xo", [128, 8, 512], F32)
        ot = Ring([P.sb("p_ot%d" % i, [128, D], F32) for i in range(2)])
    nch = n_tok // 512

    def load(c):
        a = t0 + c * 512
        h = hT.next()
        P.dma("sp", h[:, :, :], S.HT[:, :, a:a + 512], reads=[P.db("HT", a // 512)], writes=[h])
        p_ = pt.next()
        P.dma("sp", p_[:, :, :], p_in[c * 512:(c + 1) * 512, :].rearrange("(j p) d -> p j d", p=128), writes=[p_])
        return h, p_

    nxt = load(0)
    for c in range(nch):
        h, p_ = nxt
        if c + 1 < nch:
            nxt = load(c + 1)
        a = t0 + c * 512
        for k in range(2):
            ps = g.psf.next()
            P.mm([lambda j=j, k=k, ps=ps: nc.tensor.transpose(out=ps[:, j * 128:(j + 1) * 128],
                                                             in_=p_[:, j, k * 128:(k + 1) * 128], identity=C.ident[:, :])
                  for j in range(4)], reads=[p_, C.ident], writes=[ps])
            P.op("act", lambda k=k, ps=ps: nc.scalar.copy(out=pT[:, k, :], in_=ps[:, :]), reads=[ps], writes=[pT])
        xn = xnr.next()
        rmsnorm_fm(g, h, xn, gcol, 512, sq, rstd, tmp)
        for d in range(8):
            ps = g.psf.next()
            P.mm([lambda k=k, d=d, ps=ps: nc.tensor.matmul(ps[:, :], lhsT=wgt[:, k, d * 128:(d + 1) * 128], rhs=xn[:, k, :],
                                                          start=(k == 0), stop=(k == 7)) for k in range(8)],
                 reads=[wgt, xn], writes=[ps])
            s_ = sg.next()
            P.op("act", lambda ps=ps, s_=s_: nc.scalar.activation(out=s_[:, :], in_=ps[:, :], func=AF.Sigmoid),
                 reads=[ps], writes=[s_])
            ps2 = g.pacc.next()
            P.mm([lambda k=k, d=d, ps2=ps2: nc.tensor.matmul(ps2[:, :], lhsT=wpj[:, k, d * 128:(d + 1) * 128], rhs=pT[:, k, :],
                                                            start=(k == 0), stop=(k == 1)) for k in range(2)],
                 reads=[wpj, pT], writes=[ps2])
            P.op("dve", lambda ps2=ps2, s_=s_: nc.vector.tensor_mul(out=s_[:, :], in0=ps2[:, :], in1=s_[:, :]),
                 reads=[ps2, s_], writes=[s_])
            P.op("dve", lambda d=d, s_=s_: nc.vector.tensor_add(out=h[:, d, :], in0=h[:, d, :], in1=s_[:, :]),
                 reads=[s_, h], writes=[h])
        if not final:
            P.dma("sp", S.HT[:, :, a:a + 512], h[:, :, :], reads=[h], writes=[P.db("HT", a // 512)])
        else:
            rmsnorm_fm(g, h, xo, VOFF["final_norm"], 512, sq, rstd, tmp)
            for j in range(4):
                o_ = ot.next()
                for k in range(8):
                    ps = g.psf.next()
                    P.mm([lambda j=j, k=k, ps=ps: nc.tensor.transpose(out=ps[:, 0:128], in_=xo[:, k, j * 128:(j + 1) * 128],
                                                                     identity=C.ident[:, :])], reads=[xo, C.ident], writes=[ps])
                    P.op("act" if k % 2 else "dve",
                         (lambda k=k, ps=ps, o_=o_: nc.scalar.copy(out=o_[:, k * 128:(k + 1) * 128], in_=ps[:, 0:128])) if k % 2 else
                         (lambda k=k, ps=ps, o_=o_: nc.vector.tensor_copy(out=o_[:, k * 128:(k + 1) * 128], in_=ps[:, 0:128])),
                         reads=[ps], writes=[o_])
                r0 = c * 512 + j * 128
                P.dma("sp", g.OUT[r0:r0 + 128, :], o_[:, :], reads=[o_], writes=[P.db("OUT", c * 4 + j)])


def phase_l1_in(g):
    P, nc, C, I, S, cfg = g.P, g.nc, g.C, g.I, g.S, g.cfg
    CTX, OWN = cfg.CTX, cfg.OWN
    NCH = CTX // 512
    vc = VOFF
    W1 = 2632
    win = P.sb("d_win", [128, 8, W1], BF16)
    P.dma("pool", win[:, :, :], I.w1.rearrange("(k p) f -> p k f", p=128), writes=[win])
    wuq = P.sb("d_wuq", [128, 2, 8, 96], BF16)
    P.dma("pool", wuq[:, :, :, :].rearrange("p k a b -> p k (a b)"), I.wuq.rearrange("(k p) f -> p k f", p=128), writes=[wuq])
    wukv = P.sb("d_wukv", [128, 768], BF16)
    P.dma("pool", wukv[:, :], I.mla_w_ukv[:, :], writes=[wukv])
    ropet = Ring([P.sb("d_rope%d" % i, [128, 2, 512], F32) for i in range(2)])
    bif = P.sb("d_bif", [128, 8], F32)
    P.dma("sp", bif[:, :], I.bif.partition_broadcast(128), writes=[bif])
    hT = Ring([P.sb("d_hT%d" % i, [128, 8, 512], F32) for i in range(2)])
    xnr = Ring([P.sb("d_xn%d" % i, [128, 8, 512], BF16) for i in range(2)])
    xn = None
    sq = Ring([P.sb("d_sq%d" % i, [128, 512], BF16) for i in range(2)])
    rstd = P.sb("d_rstd", [128, 512], F32)
    tmp = P.sb("d_tmp", [128, 512], F32)
    uqk = P.sb("d_uqk", [128, 8, 515], F32)
    P.op("pool", lambda: nc.gpsimd.memset(uqk[:, :, :], 0.0), writes=[uqk])
    cacc = P.sb("d_cacc", [128, 512], F32)
    qkc = Ring([P.sb("d_qkc%d" % i, [128, 8, 512], BF16) for i in range(2)])
    vt = Ring([P.sb("d_vt%d" % i, [128, 4, 129], BF16) for i in range(2)])
    ot = Ring([P.sb("d_ot%d" % i, [128, 512], BF16) for i in range(2)])
    ift = Ring([P.sb("d_ift%d" % i, [128, 8], F32) for i in range(2)])
    cq = P.sb("d_cq", [128, 2, 512], F32)
    cqn = P.sb("d_cqn", [128, 2, 512], BF16)
    ckv = P.sb("d_ckv", [128, 1, 512], F32)
    ckvn = P.sb("d_ckvn", [128, 1, 512], BF16)
    qm = Ring([P.sb("d_qm%d" % i, [128, 4, 512], BF16) for i in range(2)])
    kmt = Ring([P.sb("d_kmt%d" % i, [128, 4, 512], BF16) for i in range(2)])
    for t_ in qm.tiles + kmt.tiles:
        P.op("pool", lambda t_=t_: nc.gpsimd.memset(t_[:, :, :], 0.0), writes=[t_])
    kr = P.sb("d_kr", [128, 512], F32)
    kr2 = P.sb("d_kr2", [128, 512], F32)
    ksq = P.sb("d_ksq", [128, 512], F32)
    kmx = P.sb("d_kmx", [128, 4], F32)
    kmx1 = P.sb("d_kmx1", [128, 1], F32)
    P.op("pool", lambda: nc.gpsimd.memset(kmx[:, :], 0.0), writes=[kmx])
    vm = Ring([P.sb("d_vm%d" % i, [128, 4, 129], BF16) for i in range(2)])
    cw = vc["conv_w"]

    def load(c):
        h = hT.next()
        P.dma("sp", h[:, :, :], S.HT[:, :, c * 512:(c + 1) * 512], reads=[P.db("HT", c)], writes=[h])
        r_ = ropet.next()
        P.dma("sp", r_[64:96, :, :], I.rope[:, :, c * 512:(c + 1) * 512], writes=[r_])
        return h, r_

    def fm_mm(ps, col0, ncols, rhs_tile=None, nk=8, w=None):
        w = win if w is None else w
        P.mm([lambda k=k: nc.tensor.matmul(ps[0:ncols, :], lhsT=w[:, k, col0:col0 + ncols], rhs=xn[:, k, :],
                                           start=(k == 0), stop=(k == nk - 1)) for k in range(nk)], reads=[w, xn], writes=[ps])

    nxt = load(0)
    for c in range(NCH):
        h, rp = nxt
        if c + 1 < NCH:
            nxt = load(c + 1)
        xn = xnr.next()
        own_c = c >= (CTX - OWN) // 512
        rmsnorm_fm(g, h, xn, vc["od_norm_mix"], 512, sq, rstd, tmp)
        qk = qkc.next()
        for f in range(8):
            ps = g.psf.next()
            fm_mm(ps, f * 128, 128)
            P.op("act", lambda f=f, ps=ps: nc.scalar.copy(out=uqk[:, f, 3:515], in_=ps[:, :]), reads=[ps], writes=[uqk])
            P.op("dve", lambda f=f: nc.vector.tensor_scalar_mul(out=cacc[:, :], in0=uqk[:, f, 0:512],
                                                                scalar1=C.vecs[:, cw + f * 4:cw + f * 4 + 1]),
                 reads=[uqk, C.vecs], writes=[cacc])
            for j in range(1, 4):
                P.op("dve", lambda f=f, j=j: nc.vector.scalar_tensor_tensor(
                    out=cacc[:, :], in0=uqk[:, f, j:j + 512], scalar=C.vecs[:, cw + f * 4 + j:cw + f * 4 + j + 1],
                    in1=cacc[:, :], op0=ALU.mult, op1=ALU.add), reads=[uqk, cacc, C.vecs], writes=[cacc])
            P.op("pool", lambda f=f: nc.gpsimd.tensor_copy(out=uqk[:, f, 0:3], in_=uqk[:, f, 512:515]), reads=[uqk], writes=[uqk])
            if f < 4:
                P.op("act", lambda f=f, qk=qk: nc.scalar.activation(out=qk[:, f, :], in_=cacc[:, :], func=AF.Silu),
                     reads=[cacc], writes=[qk])
            else:
                P.op("act", lambda f=f: nc.scalar.activation(out=tmp[:, :], in_=cacc[:, :], func=AF.Silu),
                     reads=[cacc], writes=[tmp])
                P.op("dve", lambda f=f, qk=qk: nc.vector.tensor_scalar_mul(out=qk[:, f, :], in0=tmp[:, :], scalar1=128.0 ** -0.5),
                     reads=[tmp], writes=[qk])
        P.dma("sp", S.QKC[:, :, c * 512:(c + 1) * 512], qk[:, :, :], reads=[qk], writes=[P.db("QKC", c)])
        for j in range(4):
            ps = g.psf.next()
            P.mm([lambda k=k, j=j, ps=ps: nc.tensor.matmul(ps[:, :], lhsT=xn[:, k, j * 128:(j + 1) * 128],
                                                          rhs=win[:, k, 1024:1536], start=(k == 0), stop=(k == 7))
                  for k in range(8)], reads=[win, xn], writes=[ps])
            v_ = vt.next()
            P.op("act", lambda ps=ps, v_=v_: nc.scalar.copy(out=v_[:, :, 0:128], in_=ps[:, :].rearrange("p (h d) -> p h d", h=4)),
                 reads=[ps], writes=[v_])
            P.op("pool", lambda v_=v_: nc.gpsimd.memset(v_[:, :, 128:129], 1.0), reads=[v_], writes=[v_])
            P.dma("sp", S.VC[c * 4 + j, :, :], v_[:, :, :].rearrange("p h d -> p (h d)"), reads=[v_], writes=[P.db("VC", c * 4 + j)])
            if own_c:
                ps = g.psf.next()
                P.mm([lambda k=k, j=j, ps=ps: nc.tensor.matmul(ps[:, :], lhsT=xn[:, k, j * 128:(j + 1) * 128],
                                                              rhs=win[:, k, 1536:2048], start=(k == 0), stop=(k == 7))
                      for k in range(8)], reads=[win, xn], writes=[ps])
                o_ = ot.next()
                P.op("act", lambda ps=ps, o_=o_: nc.scalar.activation(out=o_[:, :], in_=ps[:, :], func=AF.Sigmoid),
                     reads=[ps], writes=[o_])
                P.dma("sp", S.OS[c * 4 + j, :, :], o_[:, :], reads=[o_], writes=[P.db("OS", c * 4 + j)])
            ps = g.psf.next()
            P.mm([lambda k=k, j=j, ps=ps: nc.tensor.matmul(ps[:, 0:8], lhsT=xn[:, k, j * 128:(j + 1) * 128],
                                                          rhs=win[:, k, 2048:2056], start=(k == 0), stop=(k == 7))
                  for k in range(8)], reads=[win, xn], writes=[ps])
            i_ = ift.next()
            P.op("dve", lambda ps=ps, i_=i_: nc.vector.tensor_add(out=i_[:, :], in0=ps[:, 0:8], in1=bif[:, :]),
                 reads=[ps, bif], writes=[i_])
            P.dma("sp", S.IF[c * 4 + j, :, :], i_[:, :], reads=[i_], writes=[P.db("IF", c * 4 + j)])
        if own_c:
            for k2 in range(2):
                ps = g.psf.next()
                fm_mm(ps, 2056 + k2 * 128, 128)
                P.op("act", lambda k2=k2, ps=ps: nc.scalar.copy(out=cq[:, k2, :], in_=ps[:, :]), reads=[ps], writes=[cq])
            rmsnorm_fm(g, cq, cqn, vc["mla_q_norm"], 512, sq, rstd, tmp, nk=2, dim=256)
        ps = g.psf.next()
        fm_mm(ps, 2312, 128)
        P.op("act", lambda ps=ps: nc.scalar.copy(out=ckv[:, 0, :], in_=ps[:, :]), reads=[ps], writes=[ckv])
        rmsnorm_fm(g, ckv, ckvn, vc["mla_kv_norm"], 512, sq, rstd, tmp, nk=1, dim=128)

        def rope_rows(dst, psA, psB, scale):
            P.op("dve", lambda: nc.vector.tensor_mul(out=kr[64:96, :], in0=psA[64:96, :], in1=rp[64:96, 0, :]),
                 reads=[psA, rp], writes=[kr])
            P.op("dve", lambda: nc.vector.tensor_mul(out=kr2[64:96, :], in0=psB[64:96, :], in1=rp[64:96, 1, :]),
                 reads=[psB, rp], writes=[kr2])
            P.op("dve", lambda: nc.vector.scalar_tensor_tensor(out=dst, in0=kr[64:96, :], scalar=scale, in1=kr2[64:96, :],
                                                               op0=ALU.mult, op1=ALU.add), reads=[kr, kr2], writes=[])
        if own_c:
            q_ = qm.next()
            for hh in range(4):
                psA = g.psf.next()
                P.mm([lambda k=k, hh=hh, psA=psA: nc.tensor.matmul(psA[0:96, :], lhsT=wuq[:, k, hh * 2, :], rhs=cqn[:, k, :],
                                                                  start=(k == 0), stop=(k == 1)) for k in range(2)],
                     reads=[wuq, cqn], writes=[psA])
                psB = g.psf.next()
                P.mm([lambda k=k, hh=hh, psB=psB: nc.tensor.matmul(psB[0:96, :], lhsT=wuq[:, k, hh * 2 + 1, :], rhs=cqn[:, k, :],
                                                                  start=(k == 0), stop=(k == 1)) for k in range(2)],
                     reads=[wuq, cqn], writes=[psB])
                sc = 96.0 ** -0.5
                P.op("act", lambda hh=hh, psA=psA, q_=q_: nc.scalar.mul(out=q_[0:64, hh, :], in_=psA[0:64, :], mul=sc),
                     reads=[psA], writes=[q_])
                P.op("dve", lambda psA=psA: nc.vector.tensor_mul(out=kr[64:96, :], in0=psA[64:96, :], in1=rp[64:96, 0, :]),
                     reads=[psA, rp], writes=[kr])
                P.op("dve", lambda psB=psB: nc.vector.tensor_mul(out=kr2[64:96, :], in0=psB[64:96, :], in1=rp[64:96, 1, :]),
                     reads=[psB, rp], writes=[kr2])
                P.op("dve", lambda: nc.vector.tensor_add(out=kr[64:96, :], in0=kr[64:96, :], in1=kr2[64:96, :]),
                     reads=[kr, kr2], writes=[kr])
                P.op("act", lambda hh=hh, q_=q_: nc.scalar.mul(out=q_[64:96, hh, :], in_=kr[64:96, :], mul=sc),
                     reads=[kr], writes=[q_])
            P.dma("sp", S.QM[:, :, c * 512:(c + 1) * 512], q_[:, :, :], reads=[q_], writes=[P.db("QM", c)])
        k_ = kmt.next()
        psA = g.psf.next()
        fm_mm(psA, 2440, 96)
        psB = g.psf.next()
        fm_mm(psB, 2536, 96)
        P.op("dve", lambda psA=psA: nc.vector.tensor_mul(out=kr[64:96, :], in0=psA[64:96, :], in1=rp[64:96, 0, :]),
             reads=[psA, rp], writes=[kr])
        P.op("dve", lambda psB=psB: nc.vector.tensor_mul(out=kr2[64:96, :], in0=psB[64:96, :], in1=rp[64:96, 1, :]),
             reads=[psB, rp], writes=[kr2])
        P.op("dve", lambda: nc.vector.tensor_add(out=kr[64:96, :], in0=kr[64:96, :], in1=kr2[64:96, :]),
             reads=[kr, kr2], writes=[kr])
        for hh in range(4):
            P.op("act", lambda hh=hh, k_=k_: nc.scalar.copy(out=k_[64:96, hh, :], in_=kr[64:96, :]), reads=[kr], writes=[k_])
            ps = g.psf.next()
            P.mm([lambda hh=hh, ps=ps: nc.tensor.matmul(ps[0:64, :], lhsT=wukv[:, hh * 192:hh * 192 + 64], rhs=ckvn[:, 0, :],
                                                       start=True, stop=True)], reads=[wukv, ckvn], writes=[ps])
            P.op("act", lambda hh=hh, ps=ps, k_=k_: nc.scalar.copy(out=k_[0:64, hh, :], in_=ps[0:64, :]), reads=[ps], writes=[k_])
            P.op("act", lambda hh=hh, k_=k_: nc.scalar.activation(out=ksq[0:96, :], in_=k_[0:96, hh, :], func=AF.Square),
                 reads=[k_], writes=[ksq])
            ps2 = g.psf.next()
            P.mm([lambda ps2=ps2: nc.tensor.matmul(ps2[:, :], lhsT=C.ones[0:96, :], rhs=ksq[0:96, :], start=True, stop=True)],
                 reads=[C.ones, ksq], writes=[ps2])
            P.op("dve", lambda ps2=ps2: nc.vector.reduce_max(out=kmx1[:, :], in_=ps2[:, :], axis=AX.X), reads=[ps2], writes=[kmx1])
            P.op("dve", lambda hh=hh: nc.vector.tensor_max(out=kmx[:, hh:hh + 1], in0=kmx[:, hh:hh + 1], in1=kmx1[:, :]),
                 reads=[kmx, kmx1], writes=[kmx])
        P.dma("sp", S.KMT[:, :, c * 512:(c + 1) * 512], k_[:, :, :], reads=[k_], writes=[P.db("KMT", c)])
        for j in range(4):
            ps = g.psf.next()
            P.mm([lambda j=j, ps=ps, hh=hh: nc.tensor.matmul(ps[:, hh * 128:(hh + 1) * 128], lhsT=ckvn[:, 0, j * 128:(j + 1) * 128],
                                                            rhs=wukv[:, hh * 192 + 64:hh * 192 + 192], start=True, stop=True)
                  for hh in range(4)], reads=[wukv, ckvn], writes=[ps])
            v_ = vm.next()
            P.op("act", lambda ps=ps, v_=v_: nc.scalar.copy(out=v_[:, :, 0:128], in_=ps[:, :].rearrange("p (h d) -> p h d", h=4)),
                 reads=[ps], writes=[v_])
            P.op("pool", lambda v_=v_: nc.gpsimd.memset(v_[:, :, 128:129], 1.0), reads=[v_], writes=[v_])
            P.dma("sp", S.VM[c * 4 + j, :, :], v_[:, :, :].rearrange("p h d -> p (h d)"), reads=[v_], writes=[P.db("VM", c * 4 + j)])
    P.dma("sp", S.KMAX1[:, :], kmx[:, :], reads=[kmx], writes=[P.db("KMAX1")])


def phase_mlstm(g):
    P, nc, C, I, S, cfg = g.P, g.nc, g.C, g.I, g.S, g.cfg
    CTX, OWN = cfg.CTX, cfg.OWN
    NT = CTX // 128
    T0 = (CTX - OWN) // 128
    ut = P.sb("l_ut", [128, 128], F32)
    P.op("pool", lambda: nc.gpsimd.memset(ut[:, :], 1.0), writes=[ut])
    P.op("pool", lambda: nc.gpsimd.affine_select(out=ut[:, :], in_=ut[:, :], pattern=[[1, 128]], compare_op=ALU.is_ge,
                                                 fill=0.0, base=0, channel_multiplier=-1), reads=[ut], writes=[ut])
    maskb = P.sb("l_maskb", [128, 128], F32)
    P.op("pool", lambda: nc.gpsimd.memset(maskb[:, :], 0.0), writes=[maskb])
    P.op("pool", lambda: nc.gpsimd.affine_select(out=maskb[:, :], in_=maskb[:, :], pattern=[[-1, 128]], compare_op=ALU.is_ge,
                                                 fill=NEG, base=0, channel_multiplier=1), reads=[maskb], writes=[maskb])
    identb = P.sb("l_identb", [128, 128], BF16)
    P.op("dve", lambda: nc.vector.tensor_copy(out=identb[:, :], in_=C.ident[:, :]), reads=[C.ident], writes=[identb])
    hn = P.sb("l_hn", [128, 512], F32)
    P.dma("sp", hn[:, :], I.hnorm.partition_broadcast(128), writes=[hn])
    flg = P.sb("l_flg", [128, 4], F32)
    P.dma("sp", flg[:, :], I.l1flags.partition_broadcast(128), writes=[flg])
    CT = P.sb("l_CT", [128, 4, 129], F32)
    CTb = P.sb("l_CTb", [128, 4, 129], BF16)
    mprev = P.sb("l_mprev", [128, 4], F32)
    P.op("pool", lambda: nc.gpsimd.memset(CT[:, :, :], 0.0), writes=[CT])
    P.op("pool", lambda: nc.gpsimd.memset(CTb[:, :, :], 0.0), writes=[CTb])
    P.op("pool", lambda: nc.gpsimd.memset(mprev[:, :], 0.0), writes=[mprev])
    qk = Ring([P.sb("l_qk%d" % i, [128, 8, 128], BF16) for i in range(2)])
    va = Ring([P.sb("l_va%d" % i, [128, 4, 129], BF16) for i in range(2)])
    os_ = Ring([P.sb("l_os%d" % i, [128, 512], BF16) for i in range(2)])
    ift = Ring([P.sb("l_if%d" % i, [128, 8], F32) for i in range(2)])
    sm = {n: P.sb("l_" + n, [128, 4], F32) for n in
          ("e", "logf", "b", "bend", "cc", "rowmax", "bm", "mt", "nmt", "wint", "gg", "gmax", "mnew", "nmnew", "dec", "ws",
           "emt", "den", "rden")}
    diag4 = P.sb("l_diag4", [128, 4, 128], F32)
    intra = P.sb("l_intra", [128, 4, 128], F32)
    Dm = P.sb("l_D", [128, 4, 128], F32)
    am = P.sb("l_a", [128, 4, 128], BF16)
    aT = P.sb("l_aT", [128, 4, 128], BF16)
    kw = P.sb("l_kw", [128, 4, 128], BF16)
    atmp = P.sb("l_atmp", [128, 129], F32)
    nd = P.sb("l_nd", [128, 129], F32)
    hsq = P.sb("l_hsq", [128, 128], F32)
    ss = P.sb("l_ss", [128, 4], F32)
    hh_ = P.sb("l_hh", [128, 4, 128], F32)
    cout = Ring([P.sb("l_cout%d" % i, [128, 512], BF16) for i in range(2)])
    coT = Ring([P.sb("l_coT%d" % i, [128, 4, 128], BF16) for i in range(2)])

    def load(c):
        q_ = qk.next()
        P.dma("sp", q_[:, :, :], S.QKC[:, :, c * 128:(c + 1) * 128], reads=[P.db("QKC", c // 4)], writes=[q_])
        v_ = va.next()
        P.dma("sp", v_[:, :, :].rearrange("p h d -> p (h d)"), S.VC[c, :, :], reads=[P.db("VC", c)], writes=[v_])
        o_ = os_.next()
        P.dma("sp", o_[:, :], S.OS[c, :, :], reads=[P.db("OS", c)], writes=[o_])
        i_ = ift.next()
        P.dma("sp", i_[:, :], S.IF[c, :, :], reads=[P.db("IF", c)], writes=[i_])
        return q_, v_, o_, i_

    def dv(fn, reads, writes):
        P.op("dve", fn, reads=reads, writes=writes)

    def ac(fn, reads, writes):
        P.op("act", fn, reads=reads, writes=writes)

    def chunk(c, q_, v_, o_, i_):
        if c == T0 and T0 > 0:
            dv(lambda: nc.vector.tensor_scalar_mul(out=CT[:, :, :], in0=CT[:, :, :], scalar1=flg[:, 0:1]), [CT, flg], [CT])
            dv(lambda: nc.vector.tensor_scalar_mul(out=CTb[:, :, :], in0=CTb[:, :, :], scalar1=flg[:, 0:1]), [CTb, flg], [CTb])
            dv(lambda: nc.vector.tensor_scalar_mul(out=mprev[:, :], in0=mprev[:, :], scalar1=flg[:, 0:1]), [mprev, flg], [mprev])
        ipre = i_[:, 0:4]
        ac(lambda: nc.scalar.activation(out=sm["e"][:, :], in_=i_[:, 4:8], func=AF.Exp, scale=-1.0), [i_], [sm["e"]])
        ac(lambda: nc.scalar.activation(out=sm["logf"][:, :], in_=sm["e"][:, :], func=AF.Ln, bias=1.0), [sm["e"]], [sm["logf"]])
        dv(lambda: nc.vector.tensor_scalar_mul(out=sm["logf"][:, :], in0=sm["logf"][:, :], scalar1=-1.0), [sm["logf"]], [sm["logf"]])
        ps = g.psf.next()
        P.mm([lambda: nc.tensor.matmul(ps[:, 0:4], lhsT=ut[:, :], rhs=sm["logf"][:, :], start=True, stop=True)],
             reads=[ut, sm["logf"]], writes=[ps])
        P.mm([lambda: nc.tensor.matmul(ps[:, 4:8], lhsT=C.ones[:, :], rhs=sm["logf"][:, :], start=True, stop=True)],
             reads=[C.ones, sm["logf"]], writes=[ps])
        dv(lambda: nc.vector.tensor_copy(out=sm["b"][:, :], in_=ps[:, 0:4]), [ps], [sm["b"]])
        dv(lambda: nc.vector.tensor_copy(out=sm["bend"][:, :], in_=ps[:, 4:8]), [ps], [sm["bend"]])
        own = c >= T0
        if own:
            dv(lambda: nc.vector.tensor_sub(out=sm["cc"][:, :], in0=ipre, in1=sm["b"][:, :]), [i_, sm["b"]], [sm["cc"]])
            dv(lambda: nc.vector.tensor_tensor(out=diag4[:, :, :], in0=C.ident[:, :].unsqueeze(1).broadcast_to([128, 4, 128]),
                                               in1=sm["cc"][:, :].unsqueeze(2).broadcast_to([128, 4, 128]), op=ALU.mult),
               [C.ident, sm["cc"]], [diag4])
            psr = g.psf.next()
            P.mm([lambda: nc.tensor.matmul(psr[:, :], lhsT=C.ones[:, :], rhs=diag4[:, :, :].rearrange("p a b -> p (a b)"),
                                           start=True, stop=True)], reads=[C.ones, diag4], writes=[psr])
            dv(lambda: nc.vector.tensor_tensor(out=intra[:, :, :], in0=psr[:, :].rearrange("p (a b) -> p a b", a=4),
                                               in1=maskb[:, :].unsqueeze(1).broadcast_to([128, 4, 128]), op=ALU.add),
               [psr, maskb], [intra])
            dv(lambda: nc.vector.tensor_tensor(out=intra[:, :, :], in0=intra[:, :, :],
                                               in1=sm["b"][:, :].unsqueeze(2).broadcast_to([128, 4, 128]), op=ALU.add),
               [intra, sm["b"]], [intra])
            dv(lambda: nc.vector.reduce_max(out=sm["rowmax"][:, :], in_=intra[:, :, :], axis=AX.X), [intra], [sm["rowmax"]])
            dv(lambda: nc.vector.tensor_add(out=sm["bm"][:, :], in0=sm["b"][:, :], in1=mprev[:, :]), [sm["b"], mprev], [sm["bm"]])
            dv(lambda: nc.vector.tensor_max(out=sm["mt"][:, :], in0=sm["bm"][:, :], in1=sm["rowmax"][:, :]),
               [sm["bm"], sm["rowmax"]], [sm["mt"]])
            dv(lambda: nc.vector.tensor_scalar_mul(out=sm["nmt"][:, :], in0=sm["mt"][:, :], scalar1=-1.0), [sm["mt"]], [sm["nmt"]])
            for hd in range(4):
                ac(lambda hd=hd: nc.scalar.activation(out=Dm[:, hd, :], in_=intra[:, hd, :], func=AF.Exp,
                                                      bias=sm["nmt"][:, hd:hd + 1], scale=1.0), [intra, sm["nmt"]], [Dm])
            dv(lambda: nc.vector.tensor_sub(out=sm["wint"][:, :], in0=sm["bm"][:, :], in1=sm["mt"][:, :]), [sm["bm"], sm["mt"]], [sm["wint"]])
            ac(lambda: nc.scalar.activation(out=sm["wint"][:, :], in_=sm["wint"][:, :], func=AF.Exp), [sm["wint"]], [sm["wint"]])
            ac(lambda: nc.scalar.activation(out=sm["emt"][:, :], in_=sm["mt"][:, :], func=AF.Exp, scale=-1.0), [sm["mt"]], [sm["emt"]])
            psq = g.psf.next()
            for hd in range(4):
                P.mm([lambda hd=hd: nc.tensor.matmul(psq[:, hd * 128:(hd + 1) * 128], lhsT=q_[:, hd, :], rhs=q_[:, 4 + hd, :],
                                                     start=True, stop=True)], reads=[q_], writes=[psq])
            dv(lambda: nc.vector.tensor_tensor(out=am[:, :, :], in0=psq[:, :].rearrange("p (a b) -> p a b", a=4), in1=Dm[:, :, :],
                                               op=ALU.mult), [psq, Dm], [am])
            pb = g.psb.next()
            P.mm([lambda hd=hd: nc.tensor.transpose(out=pb[:, hd * 128:(hd + 1) * 128], in_=am[:, hd, :], identity=identb[:, :])
                  for hd in range(4)], reads=[am, identb], writes=[pb])
            dv(lambda: nc.vector.tensor_copy(out=aT[:, :, :].rearrange("p a b -> p (a b)"), in_=pb[:, 0:512]), [pb], [aT])
            co = cout.next()
            dv(lambda: nc.vector.memset(ss[:, :], 0.0), [ss], [ss])
            for hd in range(4):
                pA = g.psf.next()
                P.mm([lambda hd=hd, pA=pA: nc.tensor.matmul(pA[:, 0:129], lhsT=aT[:, hd, :], rhs=v_[:, hd, :],
                                                           start=True, stop=True)], reads=[aT, v_], writes=[pA])
                pB = g.psf.next()
                P.mm([lambda hd=hd, pB=pB: nc.tensor.matmul(pB[:, 256:385], lhsT=q_[:, hd, :], rhs=CTb[:, hd, :],
                                                           start=True, stop=True)], reads=[q_, CTb], writes=[pB])
                ac(lambda pA=pA: nc.scalar.copy(out=atmp[:, :], in_=pA[:, 0:129]), [pA], [atmp])
                dv(lambda pB=pB, hd=hd: nc.vector.scalar_tensor_tensor(out=nd[:, :], in0=pB[:, 256:385], scalar=sm["wint"][:, hd:hd + 1],
                                                                       in1=atmp[:, :], op0=ALU.mult, op1=ALU.add),
                   [pB, sm["wint"], atmp], [nd])
                ac(lambda hd=hd: nc.scalar.activation(out=sm["den"][:, hd:hd + 1], in_=nd[:, 128:129], func=AF.Abs),
                   [nd], [sm["den"]])
                dv(lambda hd=hd: nc.vector.tensor_max(out=sm["den"][:, hd:hd + 1], in0=sm["den"][:, hd:hd + 1],
                                                      in1=sm["emt"][:, hd:hd + 1]), [sm["den"], sm["emt"]], [sm["den"]])
                dv(lambda hd=hd: nc.vector.reciprocal(out=sm["rden"][:, hd:hd + 1], in_=sm["den"][:, hd:hd + 1]),
                   [sm["den"]], [sm["rden"]])
                dv(lambda hd=hd: nc.vector.tensor_scalar_mul(out=hh_[:, hd, :], in0=nd[:, 0:128], scalar1=sm["rden"][:, hd:hd + 1]),
                   [nd, sm["rden"]], [hh_])
                ac(lambda hd=hd: nc.scalar.activation(out=hsq[:, :], in_=hh_[:, hd, :], func=AF.Square, accum_out=ss[:, hd:hd + 1]),
                   [hh_], [hsq, ss])
            ac(lambda: nc.scalar.activation(out=ss[:, :], in_=ss[:, :], func=AF.Sqrt, bias=C.eps[:, 0:1], scale=1.0 / 128.0),
               [ss, C.eps], [ss])
            dv(lambda: nc.vector.reciprocal(out=ss[:, :], in_=ss[:, :]), [ss], [ss])
            dv(lambda: nc.vector.tensor_tensor(out=hh_[:, :, :], in0=hh_[:, :, :],
                                               in1=ss[:, :].unsqueeze(2).broadcast_to([128, 4, 128]), op=ALU.mult), [hh_, ss], [hh_])
            dv(lambda: nc.vector.tensor_mul(out=hh_[:, :, :].rearrange("p a b -> p (a b)"),
                                            in0=hh_[:, :, :].rearrange("p a b -> p (a b)"), in1=hn[:, :]), [hh_, hn], [hh_])
            dv(lambda co=co: nc.vector.tensor_mul(out=co[:, :], in0=hh_[:, :, :].rearrange("p a b -> p (a b)"), in1=o_[:, :]),
               [hh_, o_], [co])
            pb2 = g.psb.next()
            P.mm([lambda hd=hd, pb2=pb2, co=co: nc.tensor.transpose(out=pb2[:, hd * 128:(hd + 1) * 128],
                                                                   in_=co[:, hd * 128:(hd + 1) * 128], identity=identb[:, :])
                  for hd in range(4)], reads=[co, identb], writes=[pb2])
            ct_ = coT.next()
            dv(lambda pb2=pb2, ct_=ct_: nc.vector.tensor_copy(out=ct_[:, :, :].rearrange("p a b -> p (a b)"), in_=pb2[:, 0:512]),
               [pb2], [ct_])
            P.dma("sp", S.MIXT[:, 0:4, c * 128:(c + 1) * 128], ct_[:, :, :], reads=[ct_], writes=[P.db("MIXT_A", c)])
        dv(lambda: nc.vector.tensor_sub(out=sm["gg"][:, :], in0=sm["bend"][:, :], in1=sm["b"][:, :]), [sm["bend"], sm["b"]], [sm["gg"]])
        dv(lambda: nc.vector.tensor_add(out=sm["gg"][:, :], in0=sm["gg"][:, :], in1=ipre), [sm["gg"], i_], [sm["gg"]])
        dv(lambda: nc.vector.tensor_tensor(out=diag4[:, :, :], in0=C.ident[:, :].unsqueeze(1).broadcast_to([128, 4, 128]),
                                           in1=sm["gg"][:, :].unsqueeze(2).broadcast_to([128, 4, 128]), op=ALU.mult),
           [C.ident, sm["gg"]], [diag4])
        psg = g.psf.next()
        P.mm([lambda: nc.tensor.matmul(psg[:, :], lhsT=C.ones[:, :], rhs=diag4[:, :, :].rearrange("p a b -> p (a b)"),
                                       start=True, stop=True)], reads=[C.ones, diag4], writes=[psg])
        dv(lambda: nc.vector.reduce_max(out=sm["gmax"][:, :], in_=psg[:, :].rearrange("p (a b) -> p a b", a=4), axis=AX.X),
           [psg], [sm["gmax"]])
        dv(lambda: nc.vector.tensor_add(out=sm["mnew"][:, :], in0=sm["bend"][:, :], in1=mprev[:, :]), [sm["bend"], mprev], [sm["mnew"]])
        dv(lambda: nc.vector.tensor_sub(out=sm["dec"][:, :], in0=sm["mnew"][:, :], in1=sm["mnew"][:, :]), [sm["mnew"]], [sm["dec"]])
        dv(lambda: nc.vector.tensor_copy(out=sm["dec"][:, :], in_=sm["mnew"][:, :]), [sm["mnew"]], [sm["dec"]])
        dv(lambda: nc.vector.tensor_max(out=sm["mnew"][:, :], in0=sm["mnew"][:, :], in1=sm["gmax"][:, :]),
           [sm["mnew"], sm["gmax"]], [sm["mnew"]])
        dv(lambda: nc.vector.tensor_sub(out=sm["dec"][:, :], in0=sm["dec"][:, :], in1=sm["mnew"][:, :]), [sm["dec"], sm["mnew"]], [sm["dec"]])
        ac(lambda: nc.scalar.activation(out=sm["dec"][:, :], in_=sm["dec"][:, :], func=AF.Exp), [sm["dec"]], [sm["dec"]])
        dv(lambda: nc.vector.tensor_sub(out=sm["ws"][:, :], in0=sm["gg"][:, :], in1=sm["mnew"][:, :]), [sm["gg"], sm["mnew"]], [sm["ws"]])
        ac(lambda: nc.scalar.activation(out=sm["ws"][:, :], in_=sm["ws"][:, :], func=AF.Exp), [sm["ws"]], [sm["ws"]])
        pbk = g.psb.next()
        P.mm([lambda hd=hd: nc.tensor.transpose(out=pbk[:, hd * 128:(hd + 1) * 128], in_=q_[:, 4 + hd, :], identity=identb[:, :])
              for hd in range(4)], reads=[q_, identb], writes=[pbk])
        dv(lambda: nc.vector.tensor_tensor(out=kw[:, :, :], in0=pbk[:, 0:512].rearrange("p (a b) -> p a b", a=4),
                                           in1=sm["ws"][:, :].unsqueeze(2).broadcast_to([128, 4, 128]), op=ALU.mult),
           [pbk, sm["ws"]], [kw])
        for hd in range(4):
            pU = g.psf.next()
            P.mm([lambda hd=hd, pU=pU: nc.tensor.matmul(pU[:, 0:129], lhsT=kw[:, hd, :], rhs=v_[:, hd, :], start=True, stop=True)],
                 reads=[kw, v_], writes=[pU])
            dv(lambda hd=hd, pU=pU: nc.vector.scalar_tensor_tensor(out=CT[:, hd, :], in0=CT[:, hd, :], scalar=sm["dec"][:, hd:hd + 1],
                                                                   in1=pU[:, 0:129], op0=ALU.mult, op1=ALU.add),
               [CT, sm["dec"], pU], [CT])
        ac(lambda: nc.scalar.copy(out=CTb[:, :, :], in_=CT[:, :, :]), [CT], [CTb])
        dv(lambda: nc.vector.tensor_copy(out=mprev[:, :], in_=sm["mnew"][:, :]), [sm["mnew"]], [mprev])

    nxt = load(0)
    for c in range(NT):
        cur = nxt
        if c + 1 < NT:
            nxt = load(c + 1)
        chunk(c, *cur)


def phase_mla(g):
    P, nc, C, I, S, cfg = g.P, g.nc, g.C, g.I, g.S, g.cfg
    CTX, OWN = cfg.CTX, cfg.OWN
    NT = CTX // 128
    T0 = (CTX - OWN) // 128
    NCH = CTX // 512
    KT = P.sb("e_KT", [128, 4, CTX], BF16)
    kbufs = [Buf("eKT%d" % c) for c in range(NCH)]
    for c in range(NCH):
        P.dma("sp", KT[:, :, c * 512:(c + 1) * 512], S.KMT[:, :, c * 512:(c + 1) * 512], reads=[P.db("KMT", c)], writes=[kbufs[c]])
    VA = P.sb("e_VA", [128, NT, 516], BF16)
    vbufs = [Buf("eVA%d" % t) for t in range(NT)]
    for t in range(NT):
        P.dma("sp", VA[:, t, :], S.VM[t, :, :], reads=[P.db("VM", t)], writes=[vbufs[t]])
    kx = P.sb("e_kx", [128, 4], F32)
    P.dma("sp", kx[:, :], S.KMAX1[:, :], reads=[P.db("KMAX1")], writes=[kx])
    flg = P.sb("e_flg", [128, 4], F32)
    P.dma("sp", flg[:, :], I.l1flags.partition_broadcast(128), writes=[flg])
    tri = P.sb("e_tri", [128, 128], BF16)
    P.op("pool", lambda: nc.gpsimd.memset(tri[:, :], 1.0), writes=[tri])
    P.op("pool", lambda: nc.gpsimd.affine_select(out=tri[:, :], in_=tri[:, :], pattern=[[-1, 128]], compare_op=ALU.is_ge,
                                                 fill=0.0, base=0, channel_multiplier=1), reads=[tri], writes=[tri])
    identb = P.sb("e_identb", [128, 128], BF16)
    P.op("dve", lambda: nc.vector.tensor_copy(out=identb[:, :], in_=C.ident[:, :]), reads=[C.ident], writes=[identb])
    onesb = P.sb("e_onesb", [128, 1], BF16)
    P.op("pool", lambda: nc.gpsimd.memset(onesb[:, :], 1.0), writes=[onesb])
    qT = Ring([P.sb("e_qT%d" % i, [128, 4, 128], BF16) for i in range(2)])
    qsq = P.sb("e_qsq", [128, 4, 128], BF16)
    qn2 = P.sb("e_qn2", [128, 4], F32)
    mneg = Ring([P.sb("e_mneg%d" % i, [128, 4], F32) for i in range(2)])
    mpre = Ring([P.sb("e_mpre%d" % i, [128, 4], F32) for i in range(2)])
    Pp = Ring([P.sb("e_Pp%d" % i, [128, 1024], BF16) for i in range(6)])
    PT = Ring([P.sb("e_PT%d" % i, [128, 8, 128], BF16) for i in range(4)])
    dtok = Ring([P.sb("e_dtok%d" % i, [128, 512], BF16) for i in range(2)])
    dT = Ring([P.sb("e_dT%d" % i, [128, 4, 128], BF16) for i in range(2)])
    rden = P.sb("e_rden", [128, 1], F32)

    def loadq(i):
        t = qT.next()
        P.dma("sp", t[:, :, :], S.QM[:, :, i * 128:(i + 1) * 128], reads=[P.db("QM", i // 4)], writes=[t])
        return t

    n_z = sum(4 * ((i + 1 + 7) // 8) + 3 for i in range(T0, NT))
    side_k = -(-len(g.side) // max(1, int(n_z * 0.85)))
    nq = loadq(T0)
    for i in range(T0, NT):
        q = nq
        if i + 1 < NT:
            nq = loadq(i + 1)
        P.op("act", lambda q=q: nc.scalar.activation(out=qsq[0:96, :, :], in_=q[0:96, :, :], func=AF.Square), reads=[q], writes=[qsq])
        ps = g.psf.next()
        for hd in range(4):
            P.mm([lambda hd=hd, ps=ps: nc.tensor.matmul(ps[:, hd:hd + 1], lhsT=qsq[0:96, hd, :], rhs=onesb[0:96, :],
                                                       start=True, stop=True)], reads=[qsq, onesb], writes=[ps])
        P.op("dve", lambda ps=ps: nc.vector.tensor_mul(out=qn2[:, :], in0=ps[:, 0:4], in1=kx[:, :]), reads=[ps, kx], writes=[qn2])
        mn = mneg.next()
        mp = mpre.next()
        P.op("act", lambda mn=mn: nc.scalar.activation(out=mn[:, :], in_=qn2[:, :], func=AF.Sqrt), reads=[qn2], writes=[mn])
        P.op("dve", lambda mn=mn: nc.vector.tensor_scalar_mul(out=mn[:, :], in0=mn[:, :], scalar1=-1.0), reads=[mn], writes=[mn])
        P.op("dve", lambda mn=mn, mp=mp: nc.vector.tensor_scalar(out=mp[:, :], in0=mn[:, :], scalar1=flg[:, 1:2], scalar2=None,
                                                                op0=ALU.add), reads=[mn, flg], writes=[mp])
        nkt = i + 1
        ngr = (nkt + 7) // 8
        items = [(hd, gi) for hd in range(4) for gi in range(ngr)]
        dt_ = dtok.next()
        st = {}
        acc = {}

        def stage_s(hd, gi):
            k0 = gi * 8
            k1 = min(nkt, k0 + 8)
            pp = Pp.next()
            st[(hd, gi)] = [pp, None]
            for half in range(2):
                a0 = k0 + half * 4
                a1 = min(k1, a0 + 4)
                if a1 <= a0:
                    continue
                w = (a1 - a0) * 128
                ps = g.psf.next()
                kb = [kbufs[c] for c in range(a0 // 4, (a1 - 1) // 4 + 1)]
                P.mm([lambda ps=ps, a0=a0, w=w, hd=hd: nc.tensor.matmul(
                    ps[:, 0:w], lhsT=q[0:96, hd, :], rhs=KT[0:96, hd, a0 * 128:a0 * 128 + w], start=True, stop=True)],
                    reads=[q] + kb, writes=[ps])
                bias_t = mp if a0 < T0 else mn
                o0 = (a0 - k0) * 128
                P.op("act", lambda ps=ps, pp=pp, w=w, o0=o0, bias_t=bias_t, hd=hd: nc.scalar.activation(
                    out=pp[:, o0:o0 + w], in_=ps[:, 0:w], func=AF.Exp, bias=bias_t[:, hd:hd + 1], scale=1.0),
                    reads=[ps, bias_t], writes=[pp])
            if k1 == nkt:
                o0 = (nkt - 1 - k0) * 128
                P.op("pool", lambda pp=pp, o0=o0: nc.gpsimd.tensor_mul(out=pp[:, o0:o0 + 128], in0=pp[:, o0:o0 + 128],
                                                                      in1=tri[:, :]), reads=[pp, tri], writes=[pp])

        def stage_t(hd, gi):
            k0 = gi * 8
            k1 = min(nkt, k0 + 8)
            pp = st[(hd, gi)][0]
            pb = g.psb.next()
            P.mm([lambda j=j, pb=pb, pp=pp: nc.tensor.transpose(out=pb[:, j * 128:(j + 1) * 128],
                                                                in_=pp[:, j * 128:(j + 1) * 128], identity=identb[:, :])
                  for j in range(k1 - k0)], reads=[pp, identb], writes=[pb])
            pt = PT.next()
            st[(hd, gi)][1] = pt
            w = (k1 - k0) * 128
            P.op("dve", lambda pb=pb, pt=pt, w=w: nc.vector.tensor_copy(
                out=pt[:, :, :].rearrange("p a b -> p (a b)")[:, 0:w], in_=pb[:, 0:w]), reads=[pb], writes=[pt])

        def stage_v(hd, gi):
            k0 = gi * 8
            k1 = min(nkt, k0 + 8)
            pt = st[(hd, gi)][1]
            if gi == 0:
                acc[hd] = g.pacc.next()
            pa = acc[hd]
            vb = [vbufs[t] for t in range(k0, k1)]
            fns = [lambda j=j, pa=pa, pt=pt, k0=k0, hd=hd: nc.tensor.matmul(
                pa[:, 0:129], lhsT=pt[:, j, :], rhs=VA[:, k0 + j, hd * 129:(hd + 1) * 129],
                start=(k0 + j == 0), stop=(k0 + j == nkt - 1)) for j in range(k1 - k0)]
            P.mm(fns, reads=[pt] + vb, writes=[pa])
            if k1 == nkt:
                P.op("dve", lambda pa=pa: nc.vector.reciprocal(out=rden[:, :], in_=pa[:, 128:129]), reads=[pa], writes=[rden])
                P.op("act", lambda pa=pa, hd=hd: nc.scalar.mul(out=dt_[:, hd * 128:(hd + 1) * 128], in_=pa[:, 0:128], mul=rden[:, 0:1]),
                     reads=[pa, rden], writes=[dt_])
            del st[(hd, gi)]

        n_it = len(items)
        for z in range(n_it + 3):
            P.pump(g.side, side_k)
            if z < n_it:
                stage_s(*items[z])
            if 0 <= z - 2 < n_it:
                stage_t(*items[z - 2])
            if 0 <= z - 3 < n_it:
                stage_v(*items[z - 3])
        pb = g.psb.next()
        P.mm([lambda j=j, pb=pb: nc.tensor.transpose(out=pb[:, j * 128:(j + 1) * 128], in_=dt_[:, j * 128:(j + 1) * 128],
                                                     identity=identb[:, :]) for j in range(4)], reads=[dt_, identb], writes=[pb])
        d_ = dT.next()
        P.op("dve", lambda pb=pb, d_=d_: nc.vector.tensor_copy(out=d_[:, :, :].rearrange("p a b -> p (a b)"), in_=pb[:, 0:512]),
             reads=[pb], writes=[d_])
        P.dma("sp", S.MIXT[:, 4:8, i * 128:(i + 1) * 128], d_[:, :, :], reads=[d_], writes=[P.db("MIXT_D", i)])


def phase_router(g):
    P, nc, C, I, S, cfg, R = g.P, g.nc, g.C, g.I, g.S, g.cfg, g.R
    CTX, OWN, NTT, NST = cfg.CTX, cfg.OWN, cfg.NTT, cfg.NST
    t0 = CTX - OWN
    rw = P.sb("r_rw", [128, 8, 8], F32)
    P.dma("sp", rw[:, :, :], I.router_w.rearrange("(k p) e -> p k e", p=128), writes=[rw])
    rb = P.sb("r_rb", [128, 8], F32)
    P.dma("sp", rb[:, :], I.router_b.partition_broadcast(128), writes=[rb])
    wbase = P.sb("r_wbase", [128, 7], F32)
    P.dma("sp", wbase[:, :], I.widx_base[:, :], writes=[wbase])
    tpos = P.sb("r_tpos", [128, NST], F32)
    P.dma("sp", tpos[:, :], I.tilepos.partition_broadcast(128), writes=[tpos])
    ut = P.sb("r_ut", [128, 128], F32)
    P.op("pool", lambda: nc.gpsimd.memset(ut[:, :], 1.0), writes=[ut])
    P.op("pool", lambda: nc.gpsimd.affine_select(out=ut[:, :], in_=ut[:, :], pattern=[[1, 128]], compare_op=ALU.is_ge,
                                                 fill=0.0, base=0, channel_multiplier=-1), reads=[ut], writes=[ut])
    zt = P.sb("r_zt", [128, 4, D], BF16)
    P.op("pool", lambda: nc.gpsimd.memset(zt[:, :, :], 0.0), writes=[zt])
    for t in range(NST):
        P.dma("sp", S.XS[t * 512:(t + 1) * 512, :].rearrange("(j p) d -> p j d", p=128), zt[:, :, :], reads=[zt],
              writes=[P.db("XSz", t)])
    xf = Ring([P.sb("r_xf%d" % i, [128, 8, 512], F32) for i in range(2)])
    lg = P.sb("r_lg", [128, 8], F32)
    top8 = P.sb("r_top8", [128, 8], F32)
    nv1 = P.sb("r_nv1", [128, 1], F32)
    ex = P.sb("r_ex", [128, 8], F32)
    den = P.sb("r_den", [128, 1], F32)
    msk_all = P.sb("r_msk", [128, NTT, 8], F32)
    oh1_all = P.sb("r_oh1", [128, NTT, 8], F32)
    oh2_all = P.sb("r_oh2", [128, NTT, 8], F32)
    ex_all = P.sb("r_exa", [128, NTT, 8], F32)
    rank_all = P.sb("r_rank", [128, NTT, 8], F32)
    tmp_all = P.sb("r_tmpa", [128, NTT, 8], F32)
    carry = P.sb("r_carry", [128, 8], F32)
    P.op("pool", lambda: nc.gpsimd.memset(carry[:, :], 0.0), writes=[carry])
    for c in range(OWN // 512):
        a = t0 + c * 512
        x_ = xf.next()
        P.dma("sp", x_[:, :, :], S.XNF[:, :, a:a + 512], reads=[P.db("XNF", a // 512)], writes=[x_])
        for j in range(4):
            jt = c * 4 + j
            ps = g.psf.next()
            P.mm([lambda k=k, j=j, ps=ps: nc.tensor.matmul(ps[:, 0:8], lhsT=x_[:, k, j * 128:(j + 1) * 128], rhs=rw[:, k, :],
                                                          start=(k == 0), stop=(k == 7)) for k in range(8)],
                 reads=[x_, rw], writes=[ps])
            P.op("dve", lambda ps=ps: nc.vector.tensor_add(out=lg[:, :], in0=ps[:, 0:8], in1=rb[:, :]), reads=[ps, rb], writes=[lg])
            P.op("dve", lambda: nc.vector.max(out=top8[:, :], in_=lg[:, :]), reads=[lg], writes=[top8])
            P.op("dve", lambda: nc.vector.tensor_scalar_mul(out=nv1[:, :], in0=top8[:, 0:1], scalar1=-1.0), reads=[top8], writes=[nv1])
            P.op("act", lambda: nc.scalar.activation(out=ex[:, :], in_=lg[:, :], func=AF.Exp, bias=nv1[:, 0:1], scale=1.0),
                 reads=[lg, nv1], writes=[ex])
            P.op("dve", lambda jt=jt: nc.vector.tensor_scalar(out=msk_all[:, jt, :], in0=lg[:, :], scalar1=top8[:, 1:2], scalar2=None,
                                                              op0=ALU.is_ge), reads=[lg, top8], writes=[msk_all])
            P.op("dve", lambda jt=jt: nc.vector.tensor_scalar(out=oh1_all[:, jt, :], in0=lg[:, :], scalar1=top8[:, 0:1], scalar2=None,
                                                              op0=ALU.is_ge), reads=[lg, top8], writes=[oh1_all])
            P.op("dve", lambda jt=jt: nc.vector.tensor_mul(out=ex[:, :], in0=ex[:, :], in1=msk_all[:, jt, :]), reads=[ex, msk_all], writes=[ex])
            P.op("dve", lambda: nc.vector.reduce_sum(out=den[:, :], in_=ex[:, :], axis=AX.X), reads=[ex], writes=[den])
            P.op("dve", lambda: nc.vector.reciprocal(out=den[:, :], in_=den[:, :]), reads=[den], writes=[den])
            P.op("dve", lambda jt=jt: nc.vector.tensor_scalar_mul(out=ex_all[:, jt, :], in0=ex[:, :], scalar1=den[:, 0:1]),
                 reads=[ex, den], writes=[ex_all])
            psc = g.psf.next()
            P.mm([lambda jt=jt, psc=psc: nc.tensor.matmul(psc[:, 0:8], lhsT=ut[:, :], rhs=msk_all[:, jt, :], start=True, stop=True)],
                 reads=[ut, msk_all], writes=[psc])
            P.mm([lambda jt=jt, psc=psc: nc.tensor.matmul(psc[:, 8:16], lhsT=C.ones[:, :], rhs=msk_all[:, jt, :], start=True, stop=True)],
                 reads=[C.ones, msk_all], writes=[psc])
            P.op("dve", lambda jt=jt, psc=psc: nc.vector.tensor_add(out=rank_all[:, jt, :], in0=psc[:, 0:8], in1=carry[:, :]),
                 reads=[psc, carry], writes=[rank_all])
            P.op("dve", lambda psc=psc: nc.vector.tensor_add(out=carry[:, :], in0=carry[:, :], in1=psc[:, 8:16]),
                 reads=[psc, carry], writes=[carry])
    rem = P.sb("r_rem", [128, 8], F32)
    pad = P.sb("r_pad", [128, 8], F32)
    end = P.sb("r_end", [128, 8], F32)
    offm1 = P.sb("r_offm1", [128, 8], F32)
    dv = lambda fn, rd, wr: P.op("dve", fn, reads=rd, writes=wr)
    M = OWN // 512
    cmp2 = P.sb("r_cmp2", [128, 8, M], F32)
    dv(lambda: nc.vector.tensor_tensor(out=cmp2[:, :, :], in0=carry[:, :].unsqueeze(2).broadcast_to([128, 8, M]),
                                       in1=tpos[:, 0:M].unsqueeze(1).broadcast_to([128, 8, M]), op=ALU.is_gt), [carry, tpos], [cmp2])
    dv(lambda: nc.vector.reduce_sum(out=pad[:, :], in_=cmp2[:, :, :], axis=AX.X), [cmp2], [pad])
    dv(lambda: nc.vector.tensor_scalar_mul(out=pad[:, :], in0=pad[:, :], scalar1=512.0), [pad], [pad])
    dv(lambda: nc.vector.tensor_copy(out=end[:, 0:1], in_=pad[:, 0:1]), [pad], [end])
    for e in range(1, 8):
        dv(lambda e=e: nc.vector.tensor_add(out=end[:, e:e + 1], in0=end[:, e - 1:e], in1=pad[:, e:e + 1]), [end, pad], [end])
    dv(lambda: nc.vector.tensor_sub(out=offm1[:, :], in0=end[:, :], in1=pad[:, :]), [end, pad], [offm1])
    dv(lambda: nc.vector.tensor_scalar_add(out=offm1[:, :], in0=offm1[:, :], scalar1=-1.0), [offm1], [offm1])
    dv(lambda: nc.vector.tensor_tensor(out=rank_all[:, :, :], in0=rank_all[:, :, :],
                                       in1=offm1[:, :].unsqueeze(1).broadcast_to([128, NTT, 8]), op=ALU.add),
       [rank_all, offm1], [rank_all])
    dv(lambda: nc.vector.tensor_sub(out=oh2_all[:, :, :], in0=msk_all[:, :, :], in1=oh1_all[:, :, :]), [msk_all, oh1_all], [oh2_all])
    s1f = P.sb("r_s1f", [128, NTT], F32)
    s2f = P.sb("r_s2f", [128, NTT], F32)
    for (oh, val, dst) in ((oh1_all, rank_all, s1f), (oh2_all, rank_all, s2f), (oh1_all, ex_all, R.g1), (oh2_all, ex_all, R.g2)):
        dv(lambda oh=oh, val=val: nc.vector.tensor_mul(out=tmp_all[:, :, :], in0=oh[:, :, :], in1=val[:, :, :]), [oh, val], [tmp_all])
        dv(lambda dst=dst: nc.vector.reduce_sum(out=dst[:, :], in_=tmp_all[:, :, :], axis=AX.X), [tmp_all], [dst])
    for sf in (s1f, s2f):
        dv(lambda sf=sf: nc.vector.tensor_scalar(out=sf[:, :], in0=sf[:, :], scalar1=0.0, scalar2=float(cfg.NSLOT - 1),
                                                 op0=ALU.max, op1=ALU.min), [sf], [sf])
    dv(lambda: nc.vector.tensor_copy(out=R.s1i[:, :], in_=s1f[:, :]), [s1f], [R.s1i])
    dv(lambda: nc.vector.tensor_copy(out=R.s2i[:, :], in_=s2f[:, :]), [s2f], [R.s2i])
    cmp = P.sb("r_cmp", [128, NST, 8], F32)
    eid = P.sb("r_eid", [128, NST], F32)
    wf = P.sb("r_wf", [128, NST, 7], F32)
    dv(lambda: nc.vector.tensor_tensor(out=cmp[:, :, :], in0=end[:, :].unsqueeze(1).broadcast_to([128, NST, 8]),
                                       in1=tpos[:, :].unsqueeze(2).broadcast_to([128, NST, 8]), op=ALU.is_le), [end, tpos], [cmp])
    dv(lambda: nc.vector.reduce_sum(out=eid[:, :], in_=cmp[:, :, :], axis=AX.X), [cmp], [eid])
    dv(lambda: nc.vector.tensor_scalar(out=eid[:, :], in0=eid[:, :], scalar1=7.0, scalar2=896.0, op0=ALU.min, op1=ALU.mult), [eid], [eid])
    dv(lambda: nc.vector.tensor_tensor(out=wf[:, :, :], in0=eid[:, :].unsqueeze(2).broadcast_to([128, NST, 7]),
                                       in1=wbase[:, :].unsqueeze(1).broadcast_to([128, NST, 7]), op=ALU.add), [eid, wbase], [wf])
    dv(lambda: nc.vector.tensor_copy(out=R.widx[:, :], in_=wf[:, :, :].rearrange("p a b -> p (a b)")), [wf], [R.widx])


def phase_scatter(g):
    P, nc, C, I, S, cfg, R = g.P, g.nc, g.C, g.I, g.S, g.cfg, g.R
    xt = Ring([P.sb("s_xt%d" % i, [128, D], BF16) for i in range(4)])
    for jt in range(cfg.NTT):
        x_ = xt.next()
        P.dma("sp", x_[:, :], S.XNT[jt * 128:(jt + 1) * 128, :], reads=[P.db("XNT", jt)], writes=[x_])
        for k, si in enumerate((R.s1i, R.s2i)):
            P.idma(out=S.XS[:, :], in_=x_[:, :], out_off=bass.IndirectOffsetOnAxis(ap=si[:, jt:jt + 1], axis=0),
                   bounds=cfg.NSLOT - 1, reads=[x_, si], writes=[P.db("XSs", jt, k)])


def phase_experts(g):
    P, nc, C, I, S, cfg, R = g.P, g.nc, g.C, g.I, g.S, g.cfg, g.R
    NST = cfg.NST
    identb = P.sb("x_identb", [128, 128], BF16)
    P.op("dve", lambda: nc.vector.tensor_copy(out=identb[:, :], in_=C.ident[:, :]), reads=[C.ident], writes=[identb])
    wr = Ring([P.sb("x_w%d" % i, [128, 12288], BF16) for i in range(3)])
    xsr = Ring([P.sb("x_xs%d" % i, [128, 4, D], BF16) for i in range(2)])
    xTr = Ring([P.sb("x_xT%d" % i, [128, 8, 512], BF16) for i in range(2)])
    yar = Ring([P.sb("x_ya%d" % i, [128, 4, D], F32) for i in range(2)])
    t1r = Ring([P.sb("x_t1%d" % i, [128, 512], BF16) for i in range(3)])
    actr = Ring([P.sb("x_act%d" % i, [128, 4, 512], BF16) for i in range(3)])
    work = [(t, fb) for t in range(NST) for fb in range(7)]

    def loadw(t, fb):
        w = wr.next()
        P.idma(out=w[:, :], in_=I.moe_w[:, :], in_off=bass.IndirectOffsetOnAxis(ap=R.widx[:, t * 7 + fb:t * 7 + fb + 1], axis=0),
               bounds=8 * 7 * 128 - 1, reads=[R.widx], writes=[w])
        return w

    def loadx(t):
        xs = xsr.next()
        P.dma("sp", xs[:, :, :], S.XS[t * 512:(t + 1) * 512, :].rearrange("(j p) d -> p j d", p=128), writes=[xs])
        return xs

    def make_xT(xs):
        xT = xTr.next()
        for k2 in range(4):
            pb = g.psb.next()
            P.mm([lambda kk=kk, jj=jj, pb=pb, k2=k2: nc.tensor.transpose(
                out=pb[:, (kk * 4 + jj) * 128:(kk * 4 + jj + 1) * 128],
                in_=xs[:, jj, (2 * k2 + kk) * 128:(2 * k2 + kk + 1) * 128], identity=identb[:, :])
                for kk in range(2) for jj in range(4)], reads=[xs, identb], writes=[pb])
            P.op("dve", lambda pb=pb, k2=k2, xT=xT: nc.vector.tensor_copy(
                out=xT[:, 2 * k2:2 * k2 + 2, :].rearrange("p a b -> p (a b)"), in_=pb[:, 0:1024]), reads=[pb], writes=[xT])
        return xT

    def up_stage(w, xT):
        act = actr.next()
        for j in range(4):
            psg = g.psf.next()
            P.mm([lambda k=k, j=j, psg=psg: nc.tensor.matmul(
                psg[:, :], lhsT=w[:, k * 512 + j * 128:k * 512 + (j + 1) * 128], rhs=xT[:, k, :],
                start=(k == 0), stop=(k == 7)) for k in range(8)], reads=[w, xT], writes=[psg])
            psu = g.psf.next()
            P.mm([lambda k=k, j=j, psu=psu: nc.tensor.matmul(
                psu[:, :], lhsT=w[:, 4096 + k * 512 + j * 128:4096 + k * 512 + (j + 1) * 128], rhs=xT[:, k, :],
                start=(k == 0), stop=(k == 7)) for k in range(8)], reads=[w, xT], writes=[psu])
            t1 = t1r.next()
            P.op("act", lambda psg=psg, t1=t1: nc.scalar.activation(out=t1[:, :], in_=psg[:, :], func=AF.Silu),
                 reads=[psg], writes=[t1])
            P.op("dve", lambda psu=psu, t1=t1, act=act, j=j: nc.vector.tensor_mul(
                out=act[:, j, :], in0=psu[:, :], in1=t1[:, :]), reads=[psu, t1], writes=[act])
        return act

    def down_stage(t, fb, w, act, ya):
        for jj in range(4):
            for dh in range(2):
                pd = g.pacc.next()
                P.mm([lambda j=j, jj=jj, dh=dh, pd=pd: nc.tensor.matmul(
                    pd[:, :], lhsT=act[:, j, jj * 128:(jj + 1) * 128],
                    rhs=w[:, 8192 + j * 1024 + dh * 512:8192 + j * 1024 + (dh + 1) * 512],
                    start=(j == 0), stop=(j == 3)) for j in range(4)], reads=[w, act], writes=[pd])
                if fb == 0:
                    P.op("act", lambda jj=jj, dh=dh, pd=pd: nc.scalar.copy(out=ya[:, jj, dh * 512:(dh + 1) * 512], in_=pd[:, :]),
                         reads=[pd], writes=[ya])
                else:
                    P.op("dve", lambda jj=jj, dh=dh, pd=pd: nc.vector.tensor_add(
                        out=ya[:, jj, dh * 512:(dh + 1) * 512], in0=pd[:, :], in1=ya[:, jj, dh * 512:(dh + 1) * 512]),
                        reads=[pd, ya], writes=[ya])
        if fb == 6:
            P.dma("sp", S.Y[t * 512:(t + 1) * 512, :].rearrange("(j p) d -> p j d", p=128), ya[:, :, :], reads=[ya],
                  writes=[P.db("Y", t)])

    wq = [loadw(*work[0]), loadw(*work[1])]
    nx = loadx(0)
    prev = None
    xT = None
    ya = None
    for wi, (t, fb) in enumerate(work):
        w = wq.pop(0)
        if fb == 0:
            xs = nx
            if t + 1 < NST:
                nx = loadx(t + 1)
            xT = make_xT(xs)
            ya = yar.next()
        act = up_stage(w, xT)
        if prev is not None:
            down_stage(*prev)
        if wi + 2 < len(work):
            wq.append(loadw(*work[wi + 2]))
        prev = (t, fb, w, act, ya)
    down_stage(*prev)


def phase_combine(g):
    P, nc, C, I, S, cfg, R = g.P, g.nc, g.C, g.I, g.S, g.cfg, g.R
    CTX, OWN = cfg.CTX, cfg.OWN
    t0 = CTX - OWN
    hT = Ring([P.sb("k_hT%d" % i, [128, 8, 512], F32) for i in range(2)])
    y1r = Ring([P.sb("k_y1%d" % i, [128, D], F32) for i in range(3)])
    y2r = Ring([P.sb("k_y2%d" % i, [128, D], F32) for i in range(3)])
    ycr = Ring([P.sb("k_yc%d" % i, [128, D], F32) for i in range(2)])

    def gather(jt):
        y1, y2 = y1r.next(), y2r.next()
        P.idma(out=y1[:, :], in_=S.Y[:, :], in_off=bass.IndirectOffsetOnAxis(ap=R.s1i[:, jt:jt + 1], axis=0),
               bounds=cfg.NSLOT - 1, reads=[R.s1i], writes=[y1])
        P.idma(out=y2[:, :], in_=S.Y[:, :], in_off=bass.IndirectOffsetOnAxis(ap=R.s2i[:, jt:jt + 1], axis=0),
               bounds=cfg.NSLOT - 1, reads=[R.s2i], writes=[y2])
        return y1, y2

    def loadh(c):
        a = t0 + c * 512
        h = hT.next()
        P.dma("sp", h[:, :, :], S.HT[:, :, a:a + 512], reads=[P.db("HT", a // 512)], writes=[h])
        return h

    nh = loadh(0)
    ny = gather(0)
    for c in range(OWN // 512):
        a = t0 + c * 512
        h = nh
        if c + 1 < OWN // 512:
            nh = loadh(c + 1)
        for jj in range(4):
            jt = c * 4 + jj
            y1, y2 = ny
            if jt + 1 < cfg.NTT:
                ny = gather(jt + 1)
            yc = ycr.next()
            P.op("act", lambda y1=y1, yc=yc, jt=jt: nc.scalar.mul(out=yc[:, :], in_=y1[:, :], mul=R.g1[:, jt:jt + 1]),
                 reads=[y1, R.g1], writes=[yc])
            P.op("dve", lambda y2=y2, yc=yc, jt=jt: nc.vector.scalar_tensor_tensor(
                out=yc[:, :], in0=y2[:, :], scalar=R.g2[:, jt:jt + 1], in1=yc[:, :], op0=ALU.mult, op1=ALU.add),
                reads=[y2, yc, R.g2], writes=[yc])
            for half in range(2):
                ps = g.psf.next()
                P.mm([lambda k=k, half=half, ps=ps, yc=yc: nc.tensor.transpose(
                    out=ps[:, k * 128:(k + 1) * 128], in_=yc[:, (half * 4 + k) * 128:(half * 4 + k + 1) * 128],
                    identity=C.ident[:, :]) for k in range(4)], reads=[yc, C.ident], writes=[ps])
                P.op("dve", lambda half=half, ps=ps, jj=jj, h=h: nc.vector.tensor_tensor(
                    out=h[:, half * 4:half * 4 + 4, jj * 128:(jj + 1) * 128],
                    in0=h[:, half * 4:half * 4 + 4, jj * 128:(jj + 1) * 128],
                    in1=ps[:, :].rearrange("p (a b) -> p a b", a=4), op=ALU.add), reads=[ps, h], writes=[h])
        P.dma("sp", S.HT[:, :, a:a + 512], h[:, :, :], reads=[h], writes=[P.db("HT", a // 512)])


_NC_CACHE = {}


def _tables(CTX, OWN, early):
    NBLK = CTX // 256
    NT = CTX // 128
    pad = (CTX - OWN) if early else 0
    t = np.arange(CTX) - pad
    tpos = np.maximum(t, 0)
    invc = np.stack([1.0 / np.minimum(tpos + 1, w) for w in (2, 4, 8, 16)]).astype(np.float32)
    sl = (2.0 ** (-8.0 * np.arange(1, 9) / 8)).astype(np.float32)
    ev = np.exp(sl[None, :] * (np.arange(128)[:, None] - 127.0)).astype(np.float32)
    rowt = (sl[None, :] * (127.0 - np.arange(128)[:, None])).astype(np.float32)
    alib = (sl[:, None] * 128.0 * np.arange(NT)[None, :]).astype(np.float32)
    pastb = np.full((NBLK, NBLK), NEG, np.float32)
    pastb2 = np.full((NBLK, NBLK), NEG, np.float32)
    for ob in range(NBLK):
        for n in range(NBLK):
            if n < ob and n >= pad // 256:
                pastb[ob, n] = 0.0
                pastb2[ob, n] = 0.0
        pastb2[ob, ob] = 0.0
    inv_freq = (10000.0 ** (-np.arange(16, dtype=np.float32) / 16.0)).astype(np.float32)
    ang = tpos.astype(np.float32)[None, :] * inv_freq[:, None]
    cos = np.cos(ang).astype(np.float32)
    sin = np.sin(ang).astype(np.float32)
    rope = np.stack([np.concatenate([cos, cos], 0), np.concatenate([-sin, sin], 0)], 1).astype(np.float32)
    flags = np.array([0.0 if early else 1.0, NEG if early else 0.0, 0.0, 0.0], np.float32)
    return dict(invc=invc, ev_tab=ev, rowt=rowt, alib=alib, pastb=pastb, pastb2=pastb2, rope=np.ascontiguousarray(rope),
                l1flags=flags)


def _moe_layout(wg, wu, wd):
    wg = np.asarray(wg, np.float32).reshape(8, 8, 128, 7, 512).transpose(0, 3, 2, 1, 4).reshape(8, 7, 128, 4096)
    wu = np.asarray(wu, np.float32).reshape(8, 8, 128, 7, 512).transpose(0, 3, 2, 1, 4).reshape(8, 7, 128, 4096)
    wd = np.asarray(wd, np.float32).reshape(8, 7, 4, 128, 1024).transpose(0, 1, 3, 2, 4).reshape(8, 7, 128, 4096)
    return np.ascontiguousarray(np.concatenate([wg, wu, wd], axis=3).reshape(8 * 7 * 128, 12288))


def _shared(inp):
    f = lambda a: np.ascontiguousarray(np.asarray(a, dtype=np.float32))
    vecs = np.zeros((128, NV), np.float32)

    def putv(name, v):
        v = np.asarray(v, np.float32).reshape(-1, 128).T
        vecs[:, VOFF[name]:VOFF[name] + v.shape[1]] = v
    putv("ev_norm_mix", inp["ev_norm_mix"][0])
    putv("ev_norm_ffn", inp["ev_norm_ffn"][0])
    putv("pool_scale", inp["pool_scale"][0])
    putv("od_norm_mix", inp["od_norm_mix"][0])
    putv("od_norm_ffn", inp["od_norm_ffn"][0])
    putv("ple_norm0", inp["ple_norm"][0])
    putv("ple_norm1", inp["ple_norm"][1])
    putv("final_norm", inp["final_norm"])
    putv("mla_q_norm", inp["mla_q_norm"][0])
    putv("mla_kv_norm", inp["mla_kv_norm"][0])
    cw = np.asarray(inp["conv_w"][0], np.float32)
    cwl = cw.reshape(4, 8, 128).transpose(2, 1, 0).reshape(128, 32)
    vecs[:, VOFF["conv_w"]:VOFF["conv_w"] + 32] = cwl
    w_in = np.asarray(inp["od_w_in"][0], np.float32)
    z64 = np.zeros((D, 64), np.float32)
    kr = w_in[:, 2440:2472]
    w1 = np.concatenate([w_in[:, 0:2440], z64, kr, z64, kr[:, 16:32], kr[:, 0:16]], axis=1)
    wq = np.asarray(inp["mla_w_uq"][0], np.float32)
    parts = []
    for h in range(4):
        base = wq[:, h * 96:(h + 1) * 96]
        parts.append(base)
        parts.append(np.concatenate([np.zeros((256, 64), np.float32), base[:, 80:96], base[:, 64:80]], axis=1))
    wuq = np.concatenate(parts, axis=1)
    bif = np.concatenate([np.asarray(inp["gate_b_i"][0], np.float32), np.asarray(inp["gate_b_f"][0], np.float32)])
    return {
        "vecs": vecs, "ev_w_in": f(inp["ev_w_in"][0]), "pool_w": f(inp["pool_w"][0]), "ev_w_out": f(inp["ev_w_out"][0]),
        "ffn_w_gate": f(inp["ffn_w_gate"]), "ffn_w_up": f(inp["ffn_w_up"]), "ffn_w_down": f(inp["ffn_w_down"]),
        "ple_w_gate": f(inp["ple_w_gate"]), "ple_w_proj": f(inp["ple_w_proj"]),
        "w1": f(w1), "wuq": f(wuq), "mla_w_ukv": f(inp["mla_w_ukv"][0]), "bif": f(bif),
        "od_w_out": f(inp["od_w_out"][0]), "router_w": f(inp["router_w"][0]), "router_b": f(inp["router_b"][0]),
        "moe_w": _moe_layout(inp["moe_w_gate"][0], inp["moe_w_up"][0], inp["moe_w_down"][0]),
        "widx_base": np.ascontiguousarray((np.arange(7)[None, :] * 128 + np.arange(128)[:, None]).astype(np.float32)),
        "hnorm": f(inp["mlstm_norm"][0]),
    }


def run_module(inp, seq, cfg_kw=None):
    x = np.asarray(inp["x"], np.float32)
    p = np.asarray(inp["p"], np.float32)
    B = x.shape[0]
    CTX, OWN = seq, seq // 2
    key = (CTX, OWN)
    if key not in _NC_CACHE:
        _NC_CACHE[key] = build(Cfg(CTX=CTX, OWN=OWN, **(cfg_kw or {})))
    nc = _NC_CACHE[key]
    shared = _shared(inp)
    shared["tilepos"] = (np.arange((2 * OWN) // 512 + 7) * 512.0).astype(np.float32)
    tabs = [_tables(CTX, OWN, early=False), _tables(CTX, OWN, early=True)]
    maps = []
    for c in range(8):
        b, half = (c // 2) % B, c % 2
        m = dict(shared)
        if half == 0:
            xc = np.zeros((CTX, D), np.float32)
            xc[CTX - OWN:] = x[b, 0:OWN]
            p0 = np.zeros((CTX, 256), np.float32)
            p0[CTX - OWN:] = p[0, b, 0:OWN]
            p1 = p[1, b, 0:OWN]
            m.update(tabs[1])
        else:
            xc = x[b, 0:CTX]
            p0 = p[0, b, 0:CTX]
            p1 = p[1, b, OWN:CTX]
            m.update(tabs[0])
        m["xc"] = np.ascontiguousarray(xc)
        m["p0c"] = np.ascontiguousarray(p0)
        m["p1c"] = np.ascontiguousarray(p1)
        maps.append(m)
    res = run_bass_kernel_spmd(nc, maps, core_ids=list(range(8)))
    out = np.zeros((B, seq, D), np.float32)
    for c in range(8):
        b, half = c // 2, c % 2
        if b < B:
            out[b, half * OWN:(half + 1) * OWN] = res.results[c]["out"]
    return out, res


def kernel(**inputs):
    out, _ = run_module(inputs, 8192)
    return out
```
